# Optimizing a Trainium2 kernel written in Bass

```python
import jax, jax.numpy as jnp
from jax import lax
import numpy as np

D_MODEL = 2048
BATCH = 4
SEQ = 2048
DEPTH = 2
DEC_BATCH = 128
DEC_SEQ = 1
PAST_LEN = 16384
PAGE_SIZE = 128

MIX_WIDTH = D_MODEL
CONV_DIM = MIX_WIDTH // 2
CONV_GROUPS = 16
CONV_W = 3
HG_HEADS = 8
HG_DV = (MIX_WIDTH - CONV_DIM) // HG_HEADS
HG_DK = 128
HG_CHUNK = 64
PROJ_SPLITS = (CONV_DIM, CONV_DIM, CONV_DIM,
               HG_HEADS * HG_DK, HG_HEADS * HG_DK, HG_HEADS * HG_DV, HG_HEADS * HG_DV)
PROJ_WIDTH = sum(PROJ_SPLITS)
D_FF = ((8 * D_MODEL // 3 + 255) // 256) * 256
N_EXPERTS = 8
TOP_K = 2
D_FF_EXPERT = 7 * D_MODEL // 2
MOE_BLOCK = 256
N_DENSE = (DEPTH + 1) // 2
N_MOE = DEPTH // 2
EPS = 1e-6
F_MIN = 1e-6

kernel_name = "hymba_conv_hgrn2_adaln_moe_step"


def _rmsnorm(x, g):
    xf = x.astype(jnp.float32)
    y = xf * lax.rsqrt(jnp.mean(xf * xf, axis=-1, keepdims=True) + EPS)
    return (y * g.astype(jnp.float32)).astype(x.dtype)


def _hgrn2_recurrence(q, logf, k, v, s0):
    b, t, h, dk = q.shape
    dv = v.shape[-1]
    L = min(HG_CHUNK, t)
    n = -(-t // L)
    pad = n * L - t

    def chunks(a):
        a = jnp.pad(a.astype(jnp.float32), ((0, 0), (0, pad), (0, 0), (0, 0)))
        return a.reshape(b, n, L, h, a.shape[-1]).transpose(1, 0, 2, 3, 4)

    mask = jnp.tril(jnp.ones((L, L), dtype=bool))[None, :, :, None, None]

    def step(S, xs):
        qc, lfc, kc, vc = xs
        A = jnp.cumsum(lfc, axis=1)
        diff = A[:, :, None] - A[:, None, :]
        decay = jnp.where(mask, jnp.exp(jnp.where(mask, diff, 0.0)), 0.0)
        scores = jnp.einsum('bthd,btshd,bshd->bhts', qc, decay, kc)
        o = (jnp.einsum('bhts,bshv->bthv', scores, vc)
             + jnp.einsum('bthd,bhdv->bthv', qc * jnp.exp(A), S))
        A_last = A[:, -1]
        S = (jnp.exp(A_last)[..., None] * S
             + jnp.einsum('bshd,bshv->bhdv', kc * jnp.exp(A_last[:, None] - A), vc))
        return S, o

    S, o = lax.scan(step, s0.astype(jnp.float32), (chunks(q), chunks(logf), chunks(k), chunks(v)))
    o = o.transpose(1, 0, 2, 3, 4).reshape(b, n * L, h, dv)[:, :t]
    return o, S


def _token_mixer(h, conv_state, hgrn_state, lb, w_in, conv_w, conv_norm, hgrn_norm, w_out):
    b, t, _ = h.shape
    proj = h @ w_in
    idx = list(np.cumsum(PROJ_SPLITS)[:-1])
    hc, bg, cg, q, fz, iv, og = jnp.split(proj, idx, axis=-1)
    u = cg * hc
    ext = jnp.concatenate([conv_state.astype(u.dtype), u], axis=1)
    conv = sum(conv_w[j] * ext[:, j:j + t] for j in range(CONV_W))
    yc = (bg * conv).reshape(b, t, CONV_GROUPS, CONV_DIM // CONV_GROUPS)
    yc = _rmsnorm(yc, conv_norm.reshape(CONV_GROUPS, -1)).reshape(b, t, CONV_DIM)
    new_conv = ext[:, -(CONV_W - 1):]
    z = fz.astype(jnp.float32).reshape(b, t, HG_HEADS, HG_DK)
    lbh = lb.reshape(HG_HEADS, HG_DK)
    sg = jax.nn.sigmoid(z)
    f = lbh + (1.0 - lbh) * sg
    logf = jnp.log(jnp.maximum(f, F_MIN))
    k = (1.0 - lbh) * (1.0 - sg)
    o, new_s = _hgrn2_recurrence(q.reshape(b, t, HG_HEADS, HG_DK), logf, k,
                                 iv.reshape(b, t, HG_HEADS, HG_DV), hgrn_state)
    o = _rmsnorm(o, hgrn_norm.reshape(HG_HEADS, HG_DV)).astype(h.dtype)
    o = (o * jax.nn.silu(og.reshape(b, t, HG_HEADS, HG_DV))).reshape(b, t, HG_HEADS * HG_DV)
    out = jnp.concatenate([yc, o], axis=-1) @ w_out
    return out, new_conv, new_s


def _swiglu(h, w1, w3, w2):
    return (jax.nn.silu(h @ w1) * (h @ w3)) @ w2


def _moe_block(m):
    per = -(-m // N_EXPERTS)
    return min(MOE_BLOCK, max(8, -(-per // 8) * 8))


def _moe_ffn(h2, router_w, router_b, w1, w3, w2):
    n, d = h2.shape
    logits = h2.astype(jnp.float32) @ router_w.astype(jnp.float32) + router_b.astype(jnp.float32)
    top_val, top_idx = lax.top_k(logits, TOP_K)
    gate = jax.nn.softmax(top_val, axis=-1)
    m = n * TOP_K
    e_flat = top_idx.reshape(-1)
    tok_flat = jnp.repeat(jnp.arange(n, dtype=jnp.int32), TOP_K)
    g_flat = gate.reshape(-1)
    blk = _moe_block(m)
    n_blocks = -(-(m + N_EXPERTS * (blk - 1)) // blk)
    P = n_blocks * blk
    order = jnp.argsort(e_flat)
    se = e_flat[order]
    counts = jnp.bincount(e_flat, length=N_EXPERTS)
    starts = jnp.cumsum(counts) - counts
    padded = (counts + blk - 1) // blk * blk
    pends = jnp.cumsum(padded)
    pstarts = pends - padded
    dest = pstarts[se] + jnp.arange(m, dtype=jnp.int32) - starts[se]
    tok_buf = jnp.full((P,), n, dtype=jnp.int32).at[dest].set(tok_flat[order])
    g_buf = jnp.zeros((P,), jnp.float32).at[dest].set(g_flat[order])
    block_e = jnp.minimum(jnp.searchsorted(pends, jnp.arange(n_blocks, dtype=jnp.int32) * blk,
                                           side='right'), N_EXPERTS - 1)
    h_pad = jnp.concatenate([h2, jnp.zeros((1, d), h2.dtype)], axis=0)
    xb = h_pad[tok_buf].reshape(n_blocks, blk, d)

    def expert_block(args):
        xblk, e = args
        return _swiglu(xblk, w1[e], w3[e], w2[e])

    yb = lax.map(expert_block, (xb, block_e)).reshape(P, d)
    y = jnp.zeros((n + 1, d), yb.dtype).at[tok_buf].add(yb * g_buf[:, None].astype(yb.dtype))
    return y[:n]


def _trunk(x, c, state_conv, state_hgrn, lb_all, norm_pre, norm_post, w_mod, b_mod, w_in, conv_w,
           conv_norm, hgrn_norm, w_out, ffn_w1, ffn_w3, ffn_w2, router_w, router_b,
           moe_w1, moe_w3, moe_w2):
    b, t, d = x.shape
    cf = jax.nn.silu(c)
    new_convs, new_hgrns = [], []
    for l in range(DEPTH):
        mod = (cf @ w_mod[l] + b_mod[l]).reshape(b, 6, 1, d)
        sh_a, sc_a, ga_a, sh_f, sc_f, ga_f = [mod[:, i] for i in range(6)]
        h = _rmsnorm(x, norm_pre[l, 0]) * (1 + sc_a) + sh_a
        mixed, nc, ns = _token_mixer(h, state_conv[l], state_hgrn[l], lb_all[l], w_in[l], conv_w[l],
                                     conv_norm[l], hgrn_norm[l], w_out[l])
        x = x + ga_a * _rmsnorm(mixed, norm_post[l, 0])
        new_convs.append(nc)
        new_hgrns.append(ns)
        h = _rmsnorm(x, norm_pre[l, 1]) * (1 + sc_f) + sh_f
        if l % 2 == 0:
            f = _swiglu(h, ffn_w1[l // 2], ffn_w3[l // 2], ffn_w2[l // 2])
        else:
            j = l // 2
            f = _moe_ffn(h.reshape(b * t, d), router_w[j], router_b[j], moe_w1[j], moe_w3[j],
                         moe_w2[j]).reshape(b, t, d)
        x = x + ga_f * _rmsnorm(f, norm_post[l, 1])
    return x, jnp.stack(new_convs), jnp.stack(new_hgrns)


def setup_inputs(seed: int = 0) -> dict:
    key = jax.random.key(seed)
    ks = jax.random.split(key, 26)
    nrm = jax.random.normal
    f32 = jnp.float32
    D = D_MODEL
    return {
        'x_prompt': nrm(ks[0], (BATCH, SEQ, D), f32),
        'x_sample': nrm(ks[1], (DEC_BATCH, DEC_SEQ, D), f32),
        'state_conv': nrm(ks[2], (DEPTH, DEC_BATCH, CONV_W - 1, CONV_DIM), f32),
        'state_hgrn': nrm(ks[3], (DEPTH, DEC_BATCH, HG_HEADS, HG_DK, HG_DV), f32),
        'c_prompt': nrm(ks[4], (BATCH, D), f32),
        'c_sample': nrm(ks[5], (DEC_BATCH, D), f32),
        'norm_pre': 1.0 + 0.02 * nrm(ks[6], (DEPTH, 2, D), f32),
        'norm_post': 1.0 + 0.02 * nrm(ks[7], (DEPTH, 2, D), f32),
        'w_mod': 0.5 * D ** -0.5 * nrm(ks[8], (DEPTH, D, 6 * D), f32),
        'b_mod': 0.02 * nrm(ks[9], (DEPTH, 6 * D), f32),
        'w_in': D ** -0.5 * nrm(ks[10], (DEPTH, D, PROJ_WIDTH), f32),
        'conv_w': CONV_W ** -0.5 * nrm(ks[11], (DEPTH, CONV_W, CONV_DIM), f32),
        'conv_norm': 1.0 + 0.02 * nrm(ks[12], (DEPTH, CONV_DIM), f32),
        'lb_logits': 0.5 * nrm(ks[13], (DEPTH, HG_HEADS * HG_DK), f32),
        'hgrn_norm': 1.0 + 0.02 * nrm(ks[14], (DEPTH, HG_HEADS * HG_DV), f32),
        'w_out': MIX_WIDTH ** -0.5 * nrm(ks[15], (DEPTH, MIX_WIDTH, D), f32),
        'ffn_w1': D ** -0.5 * nrm(ks[16], (N_DENSE, D, D_FF), f32),
        'ffn_w3': D ** -0.5 * nrm(ks[17], (N_DENSE, D, D_FF), f32),
        'ffn_w2': D_FF ** -0.5 * nrm(ks[18], (N_DENSE, D_FF, D), f32),
        'router_w': D ** -0.5 * nrm(ks[19], (N_MOE, D, N_EXPERTS), f32),
        'router_b': 0.01 * nrm(ks[20], (N_MOE, N_EXPERTS), f32),
        'moe_w1': D ** -0.5 * nrm(ks[21], (N_MOE, N_EXPERTS, D, D_FF_EXPERT), f32),
        'moe_w3': D ** -0.5 * nrm(ks[22], (N_MOE, N_EXPERTS, D, D_FF_EXPERT), f32),
        'moe_w2': D_FF_EXPERT ** -0.5 * nrm(ks[23], (N_MOE, N_EXPERTS, D_FF_EXPERT, D), f32),
    }


def reference(x_prompt, x_sample, state_conv, state_hgrn, c_prompt, c_sample, norm_pre, norm_post,
              w_mod, b_mod, w_in, conv_w, conv_norm, lb_logits, hgrn_norm, w_out, ffn_w1, ffn_w3,
              ffn_w2, router_w, router_b, moe_w1, moe_w3, moe_w2):
    p = jax.nn.softmax(lb_logits.astype(jnp.float32), axis=0)
    lb_all = jnp.cumsum(p, axis=0) - p[0:1]
    weights = (lb_all, norm_pre, norm_post, w_mod, b_mod, w_in, conv_w, conv_norm, hgrn_norm, w_out,
               ffn_w1, ffn_w3, ffn_w2, router_w, router_b, moe_w1, moe_w3, moe_w2)
    zero_conv = jnp.zeros((DEPTH, x_prompt.shape[0], CONV_W - 1, CONV_DIM), x_prompt.dtype)
    zero_hgrn = jnp.zeros((DEPTH, x_prompt.shape[0], HG_HEADS, HG_DK, HG_DV), jnp.float32)
    y_prompt, new_conv_prompt, new_hgrn_prompt = _trunk(x_prompt, c_prompt, zero_conv, zero_hgrn, *weights)
    y_sample, new_conv_sample, new_hgrn_sample = _trunk(x_sample, c_sample, state_conv, state_hgrn, *weights)
    return (y_prompt, y_sample, new_conv_prompt, new_hgrn_prompt, new_conv_sample, new_hgrn_sample)
```

```python
import numpy as np
import concourse.bass as bass
import concourse.mybir as mybir
from concourse.bass_utils import run_bass_kernel_spmd

F32, BF16 = mybir.dt.float32, mybir.dt.bfloat16
AF = mybir.ActivationFunctionType
ALU = mybir.AluOpType
AX = mybir.AxisListType

D = 2048
DC = D // 128
DEPTH = 2
CONV_DIM = 1024
HG = 8
NE = 8
EPS = 1e-6
F_MIN = 1e-6
LCH = 32
BIGIDX = 1 << 24


class Cfg:
    def __init__(s, nseq=4, seq=2048, ts=128, dff=5632, dffe=7168):
        s.nseq, s.seq, s.ts, s.dff, s.dffe = nseq, seq, ts, dff, dffe
        s.tp = nseq * seq
        s.T = s.tp + ts
        s.R = nseq + ts


def tiles(T, step=128):
    return [(i, min(step, T - i)) for i in range(0, T, step)]


class Seq:
    def __init__(s, nc, sem):
        s.nc, s.sem, s.val, s.sym, s.dry = nc, sem, 0, None, False

    def _w(s):
        return s.val if s.sym is None else s.sym + s.val

    def op(s, eng, fn, dma=False):
        inc = 16 if dma else 1
        if not s.dry:
            eng.wait_ge(s.sem, s._w())
            fn().then_inc(s.sem, inc)
        s.val += inc

    def grp(s, eng, fns):
        if not s.dry:
            eng.wait_ge(s.sem, s._w())
            ins = None
            for f in fns:
                ins = f()
            ins.then_inc(s.sem, 1)
        s.val += 1

    def fin(s, eng):
        eng.wait_ge(s.sem, s.val)


def build(cfg):
    nc = bass.Bass("TRN2", target_bir_lowering=False)
    c = cfg
    T, TP, TS, R, NSEQ, SEQ = c.T, c.tp, c.ts, c.R, c.nseq, c.seq

    def din(name, shape):
        return nc.dram_tensor(name, list(shape), F32, kind="ExternalInput").ap()

    def dout(name, shape):
        return nc.dram_tensor(name, list(shape), F32, kind="ExternalOutput").ap()

    def dscr(name, shape, dt=F32):
        return nc.dram_tensor(name, list(shape), dt, kind="Internal").ap()

    x_prompt = din("x_prompt", [NSEQ, SEQ, D]); x_sample = din("x_sample", [TS, 1, D])
    state_conv = din("state_conv", [DEPTH, TS, 2, CONV_DIM]); state_hgrn = din("state_hgrn", [DEPTH, TS, HG, 128, 128])
    c_prompt = din("c_prompt", [NSEQ, D]); c_sample = din("c_sample", [TS, D])
    norm_pre = din("norm_pre", [DEPTH, 2, D]); norm_post = din("norm_post", [DEPTH, 2, D])
    w_mod = din("w_mod", [DEPTH, D, 6 * D]); b_mod = din("b_mod", [DEPTH, 6 * D])
    w_in = din("w_in", [DEPTH, D, 7168]); conv_w = din("conv_w", [DEPTH, 3, CONV_DIM])
    conv_norm = din("conv_norm", [DEPTH, CONV_DIM]); lb_logits = din("lb_logits", [DEPTH, 1024])
    hgrn_norm = din("hgrn_norm", [DEPTH, 1024]); w_out = din("w_out", [DEPTH, D, D])
    ffn_w1 = din("ffn_w1", [1, D, c.dff]); ffn_w3 = din("ffn_w3", [1, D, c.dff]); ffn_w2 = din("ffn_w2", [1, c.dff, D])
    router_w = din("router_w", [1, D, NE]); router_b = din("router_b", [1, NE])
    moe_w1 = din("moe_w1", [1, NE, D, c.dffe]); moe_w3 = din("moe_w3", [1, NE, D, c.dffe]); moe_w2 = din("moe_w2", [1, NE, c.dffe, D])

    y_prompt = dout("y_prompt", [NSEQ, SEQ, D]); y_sample = dout("y_sample", [TS, 1, D])
    new_conv_prompt = dout("new_conv_prompt", [DEPTH, NSEQ, 2, CONV_DIM])
    new_hgrn_prompt = dout("new_hgrn_prompt", [DEPTH, NSEQ, HG, 128, 128])
    new_conv_sample = dout("new_conv_sample", [DEPTH, TS, 2, CONV_DIM])
    new_hgrn_sample = dout("new_hgrn_sample", [DEPTH, TS, HG, 128, 128])

    KCMAX = max(c.dff, c.dffe, D) // 128
    X = dscr("X", [T, D]); MOD = dscr("MOD", [DEPTH, R, 6 * D]); CFT = dscr("CFT", [DC, 128, R], BF16)
    HT = dscr("HT", [DC, 128, T], BF16); PA = dscr("PA", [40, 128, T]); PB = dscr("PB", [T, 2048])
    PO = dscr("PO", [T, 1024]); MIXT = dscr("MIXT", [DC, 128, T], BF16); FO = dscr("FO", [T, D])
    AT = dscr("AT", [KCMAX, 128, T], BF16); GT = dscr("GT", [T, NE])
    TE = min(T, ((int(1.72 * 2 * T / NE) + 511) // 512) * 512)
    HETM = dscr("HETM", [NE * TE, D], BF16); HE = dscr("HE", [NE, DC, 128, TE], BF16); YE = dscr("YE", [NE * TE, D], BF16)
    IDXD = dscr("IDXD", [T, 2], mybir.dt.int32); G2D = dscr("G2D", [T, 2])

    from contextlib import ExitStack
    es = ExitStack()
    sem = es.enter_context(nc.semaphore("g"))
    S = Seq(nc, sem)
    lsem = es.enter_context(nc.semaphore("lsem"))
    es.enter_context(nc.allow_non_contiguous_dma(reason="small strided tiles"))
    V, A, P, PE, SY = nc.vector, nc.scalar, nc.gpsimd, nc.tensor, nc.sync

    _cnt = [0]

    def SBT(name, shape, dt=F32):
        _cnt[0] += 1
        return nc.sbuf_tensor(f"{name}_{_cnt[0]}", list(shape), dt)

    def sb(name, shape, dt=F32):
        return es.enter_context(SBT(name, list(shape), dt))

    ident_b = sb("ident_b", [128, 128], BF16); ident_f = sb("ident_f", [128, 128])
    mask64 = sb("mask64", [128, 64]); bd64 = sb("bd64", [128, 128]); ones_t = sb("ones_t", [128, 128])
    scanm = sb("scanm", [128, SEQ]); epsb = sb("epsb", [128, 1])
    ps = es.enter_context(nc.psum_tensor("ps", [128, 4, 512], F32))
    pt = es.enter_context(nc.psum_tensor("pt", [128, 2048], BF16))
    pf = es.enter_context(nc.psum_tensor("pf", [128, 2, 512], F32))

    S.op(P, lambda: P.memset(ones_t[:], 1.0))
    S.op(P, lambda: P.memset(epsb[:], EPS))
    S.op(P, lambda: P.affine_select(out=ident_f[:], in_=ones_t[:], pattern=[[-1, 128]], compare_op=ALU.is_equal,
                                     fill=0.0, base=0, channel_multiplier=1))
    S.op(V, lambda: V.tensor_copy(out=ident_b[:], in_=ident_f[:]))
    S.op(P, lambda: P.affine_select(out=mask64[0:64, :], in_=ones_t[0:64, 0:64], pattern=[[1, 64]], compare_op=ALU.is_ge,
                                     fill=0.0, base=0, channel_multiplier=-1))
    S.op(P, lambda: P.memset(bd64[:], 0.0))
    S.op(P, lambda: P.memset(bd64[0:64, 0:64], 1.0 / 64))
    S.op(P, lambda: P.memset(bd64[64:128, 64:128], 1.0 / 64))
    S.op(P, lambda: P.memset(scanm[:], 1.0))
    S.op(P, lambda: P.memset(scanm[:].rearrange("p (n l) -> p n l", l=LCH)[:, :, 0:1], 0.0))

    tri_b = sb("tri_b", [128, 128], BF16); ones_b = sb("ones_b", [128, 128], BF16); tri_f = sb("tri_f", [128, 128])
    eoff_i = sb("eoff_i", [128, NE], mybir.dt.int32); eoff = sb("eoff", [128, NE])
    S.op(P, lambda: P.affine_select(out=tri_f[:], in_=ones_t[:], pattern=[[1, 128]], compare_op=ALU.is_gt, fill=0.0, base=0, channel_multiplier=-1))
    S.op(V, lambda: V.tensor_copy(out=tri_b[:], in_=tri_f[:]))
    S.op(V, lambda: V.tensor_copy(out=ones_b[:], in_=ones_t[:]))
    S.op(P, lambda: P.iota(eoff_i[:], pattern=[[TE, NE]], base=0, channel_multiplier=0))
    S.op(V, lambda: V.tensor_copy(out=eoff[:], in_=eoff_i[:]))

    bcreg = P.alloc_register("bcreg")
    P.reg_mov(bcreg, NE * TE - 1)

    S.op(SY, lambda: SY.dma_start(out=X[0:TP, :], in_=x_prompt.rearrange("b t d -> (b t) d")), True)
    S.op(SY, lambda: SY.dma_start(out=X[TP:T, :], in_=x_sample.rearrange("b t d -> (b t) d")), True)

    DQ = [SY]

    def row_bcast(ap_row, n=128):
        return ap_row.partition_broadcast(n)

    def transpose_store(src, tn, DST, t0, nchunks, c0=0):
        with SBT("tstage", [128, nchunks, 128], BF16) as stg:
            S.grp(PE, [(lambda k=k: PE.transpose(out=pt[:, k * 128:k * 128 + tn], in_=src[:tn, k * 128:(k + 1) * 128],
                                                  identity=ident_b[:tn, :tn])) for k in range(nchunks)])
            S.op(V, lambda: V.tensor_copy(out=stg[:, :, :tn], in_=pt[:, 0:nchunks * 128].rearrange("p (k t) -> p k t", t=128)[:, :, :tn]))
            S.op(SY, lambda: SY.dma_start(out=DST[c0:c0 + nchunks, :, t0:t0 + tn].rearrange("k p t -> p k t"), in_=stg[:, :, :tn]), True)

    def gemm_a(Ws, INT, KC, N, Tn, evac, tgs=512):
        nW = len(Ws)
        cols = 512 // nW
        with SBT("ga_w", [128, nW, KC, cols], BF16) as wb, SBT("ga_in", [128, KC, tgs], BF16) as inb:
            for nb in range(0, N, cols):
                ncol = min(cols, N - nb)
                for wi, W in enumerate(Ws):
                    S.op(P, lambda wi=wi, W=W: P.dma_start(out=wb[:, wi, :, :ncol],
                                                           in_=(W(nb, ncol) if callable(W) else W[:, nb:nb + ncol]).rearrange("(k p) n -> p k n", p=128)), True)
                for (t0, tn) in tiles(Tn, tgs):
                    S.op(DQ[0], lambda: DQ[0].dma_start(out=inb[:, :, :tn], in_=INT[0:KC, :, t0:t0 + tn].rearrange("k p t -> p k t")), True)
                    for j in range(ncol // 128):
                        fns = []
                        for wi in range(nW):
                            for k in range(KC):
                                fns.append(lambda wi=wi, k=k: PE.matmul(ps[:, wi * 2 + (j % 2) if nW == 2 else j, :tn],
                                                                        lhsT=wb[:, wi, k, j * 128:(j + 1) * 128], rhs=inb[:, k, :tn],
                                                                        start=(k == 0), stop=(k == KC - 1)))
                        S.grp(PE, fns)
                        evac((nb // 128) + j, t0, tn, [ps[:, wi * 2 + (j % 2) if nW == 2 else j, :tn] for wi in range(nW)])

    def gemm_b(W, INT, KC, N, tls, evac):
        with SBT("gb_w", [128, KC, 512], BF16) as wb, SBT("gb_in", [128, KC, 128], BF16) as inb:
            for nb in range(0, N, 512):
                ncol = min(512, N - nb)
                for k0 in range(0, KC, 16):
                    kn = min(16, KC - k0)
                    S.op(P, lambda: P.dma_start(out=wb[:, k0:k0 + kn, :ncol],
                                                in_=W[k0 * 128:(k0 + kn) * 128, nb:nb + ncol].rearrange("(k p) n -> p k n", p=128)), True)
                for (t0, tn) in tls:
                    S.op(DQ[0], lambda: DQ[0].dma_start(out=inb[:, :, :tn], in_=INT[0:KC, :, t0:t0 + tn].rearrange("k p t -> p k t")), True)
                    S.grp(PE, [(lambda k=k: PE.matmul(ps[:tn, 0, :ncol], lhsT=inb[:, k, :tn], rhs=wb[:, k, :ncol],
                                                      start=(k == 0), stop=(k == KC - 1))) for k in range(KC)])
                    evac(t0, tn, nb, ncol, ps[:tn, 0, :ncol])

    def store_b(DST):
        def ev(t0, tn, nb, ncol, p):
            with SBT("sb_ev", [128, 512], F32) as o:
                S.op(V, lambda: V.tensor_copy(out=o[:tn, :ncol], in_=p))
                S.op(SY, lambda: SY.dma_start(out=DST[t0:t0 + tn, nb:nb + ncol], in_=o[:tn, :ncol]), True)
        return ev

    def mod_rows(l, comp, t0, tn):
        if t0 < TP:
            b = t0 // SEQ
            return row_bcast(MOD[l, b:b + 1, comp * D:(comp + 1) * D], tn)
        r0 = NSEQ + (t0 - TP)
        return MOD[l, r0:r0 + tn, comp * D:(comp + 1) * D]

    def rstd_of(src, tn, out_rstd, junk):
        S.op(A, lambda: A.activation(out=junk[:tn, :], in_=src, func=AF.Square, accum_out=out_rstd[:tn, :]))
        S.op(V, lambda: V.tensor_scalar(out=out_rstd[:tn, :], in0=out_rstd[:tn, :], scalar1=1.0 / D, scalar2=EPS, op0=ALU.mult, op1=ALU.add))
        S.op(A, lambda: A.sqrt(out_rstd[:tn, :], out_rstd[:tn, :]))
        S.op(V, lambda: V.reciprocal(out=out_rstd[:tn, :], in_=out_rstd[:tn, :]))

    def stage_mod():
        with SBT("m_c", [128, D], F32) as ct, SBT("m_cb", [128, D], BF16) as cb:
            for (r0, rn, src) in [(0, NSEQ, c_prompt), (NSEQ, TS, c_sample)]:
                S.op(SY, lambda: SY.dma_start(out=ct[:rn, :], in_=src[:, :]), True)
                S.op(A, lambda: A.activation(out=cb[:rn, :], in_=ct[:rn, :], func=AF.Silu))
                transpose_store(cb, rn, CFT, r0, DC)
        for l in range(DEPTH):
            def ev(t0, tn, nb, ncol, p, l=l):
                with SBT("m_b", [128, 512], F32) as bt, SBT("m_o", [128, 512], F32) as o:
                    S.op(SY, lambda: SY.dma_start(out=bt[:tn, :ncol], in_=row_bcast(b_mod[l:l + 1, nb:nb + ncol], tn)), True)
                    S.op(V, lambda: V.tensor_tensor(out=o[:tn, :ncol], in0=p, in1=bt[:tn, :ncol], op=ALU.add))
                    S.op(SY, lambda: SY.dma_start(out=MOD[l, t0:t0 + tn, nb:nb + ncol], in_=o[:tn, :ncol]), True)
            gemm_b(w_mod[l], CFT, DC, 6 * D, [(0, NSEQ), (NSEQ, TS)], ev)

    def stage_normmod(l, which, router=False):
        sc_c, sh_c = (1, 0) if which == 0 else (4, 3)
        with ExitStack() as st:
            def t(name, shape, dt=F32):
                return st.enter_context(SBT(name, list(shape), dt))
            xt = t("n_x", [128, D]); at = t("n_a", [128, D]); sh = t("n_sh", [128, D]); gb = t("n_g", [128, D])
            hf = t("n_hf", [128, D]); rs = t("n_rs", [128, 1]); hb = t("n_hb", [128, D], BF16)
            S.op(SY, lambda: SY.dma_start(out=gb[:], in_=row_bcast(norm_pre[l, which:which + 1, :], 128)), True)
            if router:
                rwb = t("n_rw", [128, NE, D]); rbb = t("n_rb", [128, NE]); lg = t("n_lg", [128, NE]); l2 = t("n_l2", [128, NE])
                m1 = t("n_m1", [128, 1]); m2 = t("n_m2", [128, 1]); k1 = t("n_k1", [128, NE]); k2 = t("n_k2", [128, NE])
                g12 = t("n_g12", [128, 2]); dd = t("n_dd", [128, 1])
                mf = t("n_mf", [128, NE]); mb = t("n_mb", [128, NE], BF16); pos = t("n_pos", [128, NE]); cnt = t("n_cnt", [128, NE])
                ovf = t("n_ovf", [128, NE]); idf = t("n_idf", [128, 2]); idi = t("n_idi", [128, 2], mybir.dt.int32)
                RWT = dscr("RWT", [NE, D])
                S.op(SY, lambda: SY.dma_start(out=RWT[:, :], in_=router_w[0, :, :].rearrange("d e -> e d")), True)
                for e in range(NE):
                    S.op(SY, lambda e=e: SY.dma_start(out=rwb[:, e, :], in_=row_bcast(RWT[e:e + 1, :], 128)), True)
                S.op(SY, lambda: SY.dma_start(out=rbb[:], in_=row_bcast(router_b[0:1, :], 128)), True)
                S.op(V, lambda: V.memset(cnt[:], 0.0))
            for (t0, tn) in tiles(TP) + [(TP, TS)]:
                S.op(SY, lambda: SY.dma_start(out=xt[:tn, :], in_=X[t0:t0 + tn, :]), True)
                S.op(SY, lambda: SY.dma_start(out=at[:tn, :], in_=mod_rows(l, sc_c, t0, tn)), True)
                S.op(SY, lambda: SY.dma_start(out=sh[:tn, :], in_=mod_rows(l, sh_c, t0, tn)), True)
                rstd_of(xt[:tn, :], tn, rs, hf)
                S.op(V, lambda: V.scalar_tensor_tensor(out=at[:tn, :], in0=at[:tn, :], scalar=1.0, in1=gb[:tn, :], op0=ALU.add, op1=ALU.mult))
                S.op(V, lambda: V.scalar_tensor_tensor(out=hf[:tn, :], in0=xt[:tn, :], scalar=rs[:tn, 0:1], in1=at[:tn, :], op0=ALU.mult, op1=ALU.mult))
                S.op(V, lambda: V.tensor_tensor(out=hf[:tn, :], in0=hf[:tn, :], in1=sh[:tn, :], op=ALU.add))
                S.op(A, lambda: A.copy(hb[:tn, :], hf[:tn, :]))
                if not router:
                    transpose_store(hb, tn, HT, t0, DC)
                    continue
                for e in range(NE):
                    S.op(V, lambda e=e: V.tensor_tensor(out=at[:tn, :], in0=hf[:tn, :], in1=rwb[:tn, e, :], op=ALU.mult))
                    S.op(V, lambda e=e: V.reduce_sum(out=lg[:tn, e:e + 1], in_=at[:tn, :], axis=AX.X))
                S.op(V, lambda: V.tensor_tensor(out=lg[:tn, :], in0=lg[:tn, :], in1=rbb[:tn, :], op=ALU.add))
                S.op(V, lambda: V.reduce_max(out=m1[:tn, :], in_=lg[:tn, :], axis=AX.X))
                S.op(V, lambda: V.tensor_scalar(out=k1[:tn, :], in0=lg[:tn, :], scalar1=m1[:tn, 0:1], scalar2=None, op0=ALU.is_equal))
                S.op(V, lambda: V.scalar_tensor_tensor(out=l2[:tn, :], in0=k1[:tn, :], scalar=-1e30, in1=lg[:tn, :], op0=ALU.mult, op1=ALU.add))
                S.op(V, lambda: V.reduce_max(out=m2[:tn, :], in_=l2[:tn, :], axis=AX.X))
                S.op(V, lambda: V.tensor_scalar(out=k2[:tn, :], in0=l2[:tn, :], scalar1=m2[:tn, 0:1], scalar2=None, op0=ALU.is_equal))
                S.op(V, lambda: V.tensor_tensor(out=dd[:tn, :], in0=m1[:tn, :], in1=m2[:tn, :], op=ALU.subtract))
                S.op(A, lambda: A.activation(out=g12[:tn, 0:1], in_=dd[:tn, :], func=AF.Sigmoid))
                S.op(A, lambda: A.activation(out=g12[:tn, 1:2], in_=dd[:tn, :], func=AF.Sigmoid, scale=-1.0))
                S.op(SY, lambda: SY.dma_start(out=G2D[t0:t0 + tn, :], in_=g12[:tn, :]), True)
                S.op(V, lambda: V.tensor_tensor(out=mf[:tn, :], in0=k1[:tn, :], in1=k2[:tn, :], op=ALU.add))
                S.op(V, lambda: V.tensor_copy(out=mb[:tn, :], in_=mf[:tn, :]))
                S.op(PE, lambda: PE.matmul(pf[:tn, 0, 0:NE], lhsT=tri_b[:tn, :tn], rhs=mb[:tn, :], start=True, stop=True))
                S.op(V, lambda: V.tensor_tensor(out=pos[:tn, :], in0=pf[:tn, 0, 0:NE], in1=cnt[:tn, :], op=ALU.add))
                S.op(PE, lambda: PE.matmul(pf[:, 1, 0:NE], lhsT=ones_b[:tn, :], rhs=mb[:tn, :], start=True, stop=True))
                S.op(V, lambda: V.tensor_tensor(out=cnt[:], in0=cnt[:], in1=pf[:, 1, 0:NE], op=ALU.add))
                S.op(V, lambda: V.tensor_scalar(out=ovf[:tn, :], in0=pos[:tn, :], scalar1=float(TE), scalar2=float(BIGIDX), op0=ALU.is_ge, op1=ALU.mult))
                S.op(V, lambda: V.tensor_tensor(out=pos[:tn, :], in0=pos[:tn, :], in1=eoff[:tn, :], op=ALU.add))
                S.op(V, lambda: V.tensor_tensor(out=pos[:tn, :], in0=pos[:tn, :], in1=ovf[:tn, :], op=ALU.add))
                S.op(V, lambda: V.tensor_tensor(out=k1[:tn, :], in0=k1[:tn, :], in1=pos[:tn, :], op=ALU.mult))
                S.op(V, lambda: V.reduce_sum(out=idf[:tn, 0:1], in_=k1[:tn, :], axis=AX.X))
                S.op(V, lambda: V.tensor_tensor(out=k2[:tn, :], in0=k2[:tn, :], in1=pos[:tn, :], op=ALU.mult))
                S.op(V, lambda: V.reduce_sum(out=idf[:tn, 1:2], in_=k2[:tn, :], axis=AX.X))
                S.op(V, lambda: V.tensor_copy(out=idi[:tn, :], in_=idf[:tn, :]))
                S.op(SY, lambda: SY.dma_start(out=IDXD[t0:t0 + tn, :], in_=idi[:tn, :]), True)
                for j in range(2):
                    S.op(P, lambda j=j: P.indirect_dma_start(out=HETM[:, :], out_offset=bass.IndirectOffsetOnAxis(ap=idi[:tn, j:j + 1], axis=0),
                                                             in_=hb[:tn, :], in_offset=None, bounds_check=bcreg, oob_is_err=False), True)

    def stage_he_transpose():
        with SBT("het", [128, D], BF16) as ht_:
            for e in range(NE):
                for (t0, tn) in tiles(TE):
                    S.op(SY, lambda: SY.dma_start(out=ht_[:tn, :], in_=HETM[e * TE + t0:e * TE + t0 + tn, :]), True)
                    transpose_store(ht_, tn, HE[e], t0, DC)

    def stage_combine():
        with ExitStack() as st:
            def t(name, shape, dt=F32):
                return st.enter_context(SBT(name, list(shape), dt))
            r1 = t("cb_r1", [128, D], BF16); r2 = t("cb_r2", [128, D], BF16); y = t("cb_y", [128, D]); g = t("cb_g", [128, 2]); idi = t("cb_i", [128, 2], mybir.dt.int32)
            for (t0, tn) in tiles(TP) + [(TP, TS)]:
                S.op(SY, lambda: SY.dma_start(out=g[:tn, :], in_=G2D[t0:t0 + tn, :]), True)
                S.op(SY, lambda: SY.dma_start(out=idi[:tn, :], in_=IDXD[t0:t0 + tn, :]), True)
                S.op(V, lambda: V.memset(r1[:], 0.0))
                S.op(V, lambda: V.memset(r2[:], 0.0))
                for j, r in ((0, r1), (1, r2)):
                    S.op(P, lambda: P.indirect_dma_start(out=r[:tn, :], out_offset=None, in_=YE[:, :],
                                                         in_offset=bass.IndirectOffsetOnAxis(ap=idi[:tn, j:j + 1], axis=0),
                                                         bounds_check=bcreg, oob_is_err=False), True)
                S.op(V, lambda: V.tensor_scalar(out=y[:tn, :], in0=r1[:tn, :], scalar1=g[:tn, 0:1], scalar2=None, op0=ALU.mult))
                S.op(V, lambda: V.scalar_tensor_tensor(out=y[:tn, :], in0=r2[:tn, :], scalar=g[:tn, 1:2], in1=y[:tn, :], op0=ALU.mult, op1=ALU.add))
                S.op(SY, lambda: SY.dma_start(out=FO[t0:t0 + tn, :], in_=y[:tn, :]), True)

    def stage_epilogue(l, which, final=False):
        ga_c = 2 if which == 0 else 5
        with ExitStack() as st:
            def t(name, shape, dt=F32):
                return st.enter_context(SBT(name, list(shape), dt))
            xt = t("e_x", [128, D]); ft = t("e_f", [128, D]); ga = t("e_ga", [128, D]); gb = t("e_g", [128, D]); jk = t("e_j", [128, D]); rs = t("e_rs", [128, 1])
            S.op(SY, lambda: SY.dma_start(out=gb[:], in_=row_bcast(norm_post[l, which:which + 1, :], 128)), True)
            for (t0, tn) in tiles(TP) + [(TP, TS)]:
                S.op(SY, lambda: SY.dma_start(out=xt[:tn, :], in_=X[t0:t0 + tn, :]), True)
                S.op(SY, lambda: SY.dma_start(out=ft[:tn, :], in_=FO[t0:t0 + tn, :]), True)
                S.op(SY, lambda: SY.dma_start(out=ga[:tn, :], in_=mod_rows(l, ga_c, t0, tn)), True)
                rstd_of(ft[:tn, :], tn, rs, jk)
                S.op(V, lambda: V.tensor_tensor(out=ga[:tn, :], in0=ga[:tn, :], in1=gb[:tn, :], op=ALU.mult))
                S.op(V, lambda: V.scalar_tensor_tensor(out=ft[:tn, :], in0=ft[:tn, :], scalar=rs[:tn, 0:1], in1=ga[:tn, :], op0=ALU.mult, op1=ALU.mult))
                S.op(V, lambda: V.tensor_tensor(out=xt[:tn, :], in0=xt[:tn, :], in1=ft[:tn, :], op=ALU.add))
                S.op(SY, lambda: SY.dma_start(out=X[t0:t0 + tn, :], in_=xt[:tn, :]), True)
                if final:
                    dst = y_prompt.rearrange("b t d -> (b t) d")[t0:t0 + tn, :] if t0 < TP else y_sample.rearrange("b t d -> (b t) d")[t0 - TP:t0 - TP + tn, :]
                    S.op(SY, lambda: SY.dma_start(out=dst, in_=xt[:tn, :]), True)

    def stage_win(l):
        def evA(j, t0, tn, pss):
            with SBT("wa_o", [128, 512], F32) as o:
                S.op(V, lambda: V.tensor_copy(out=o[:, :tn], in_=pss[0]))
                S.op(SY, lambda: SY.dma_start(out=PA[j, :, t0:t0 + tn], in_=o[:, :tn]), True)
        gemm_a([w_in[l, :, 0:5120]], HT, DC, 5120, T, evA)
        gemm_b(w_in[l, :, 5120:7168], HT, DC, 2048, tiles(TP) + [(TP, TS)], store_b(PB))

    def load_percol(dst, src_row, nch):
        with nc.allow_non_contiguous_dma(reason="small per-channel vector"):
            S.op(SY, lambda: SY.dma_start(out=dst, in_=src_row.rearrange("o (c p) -> p (o c)", p=128)), True)

    def stage_conv(l):
        NCH = CONV_DIM // 128
        segs = [(b * SEQ, SEQ, b) for b in range(NSEQ)] + [(TP, TS, None)]
        LM = max(SEQ, TS)
        with ExitStack() as st:
            def t(name, shape, dt=F32):
                return st.enter_context(SBT(name, list(shape), dt))
            cw = t("c_w", [128, 3, NCH]); cn = t("c_n", [128, NCH])
            hc = t("c_hc", [128, LM]); bg = t("c_bg", [128, LM]); ext = t("c_ext", [128, LM + 2]); cv = t("c_cv", [128, LM])
            s0 = t("c_s0", [128, 128]); s1 = t("c_s1", [128, 128]); tm = t("c_tm", [128, 128]); sq = t("c_sq", [128, 512]); ob = t("c_ob", [128, 512], BF16)
            for j in range(3):
                load_percol(cw[:, j, :], conv_w[l, j:j + 1, :], NCH)
            load_percol(cn[:, :], conv_norm[l:l + 1, :], NCH)
            S.op(SY, lambda: SY.dma_start(out=new_conv_sample[l, :, 0, :], in_=state_conv[l, :, 1, :]), True)
            for (t0, L, b) in segs:
                for ch in range(NCH):
                    S.op(SY, lambda: SY.dma_start(out=hc[:, :L], in_=PA[ch, :, t0:t0 + L]), True)
                    S.op(SY, lambda: SY.dma_start(out=bg[:, :L], in_=PA[NCH + ch, :, t0:t0 + L]), True)
                    S.op(SY, lambda: SY.dma_start(out=ext[:, 2:L + 2], in_=PA[2 * NCH + ch, :, t0:t0 + L]), True)
                    S.op(V, lambda: V.tensor_tensor(out=ext[:, 2:L + 2], in0=ext[:, 2:L + 2], in1=hc[:, :L], op=ALU.mult))
                    if b is not None:
                        S.op(V, lambda: V.memset(ext[:, 0:2], 0.0))
                        a0, a1 = ext[:, 0:L], ext[:, 1:L + 1]
                    else:
                        for j, dstt in ((0, s0), (1, s1)):
                            S.op(SY, lambda: SY.dma_start(out=tm[:L, :], in_=state_conv[l, :, j, ch * 128:(ch + 1) * 128]), True)
                            S.op(PE, lambda: PE.transpose(out=pf[:, 0, :L], in_=tm[:L, :], identity=ident_f[:L, :L]))
                            S.op(V, lambda: V.tensor_copy(out=dstt[:, :L], in_=pf[:, 0, :L]))
                        a0, a1 = s0[:, :L], s1[:, :L]
                    S.op(V, lambda: V.tensor_scalar(out=cv[:, :L], in0=a0, scalar1=cw[:, 0, ch:ch + 1], scalar2=None, op0=ALU.mult))
                    S.op(V, lambda: V.scalar_tensor_tensor(out=cv[:, :L], in0=a1, scalar=cw[:, 1, ch:ch + 1], in1=cv[:, :L], op0=ALU.mult, op1=ALU.add))
                    S.op(V, lambda: V.scalar_tensor_tensor(out=cv[:, :L], in0=ext[:, 2:L + 2], scalar=cw[:, 2, ch:ch + 1], in1=cv[:, :L], op0=ALU.mult, op1=ALU.add))
                    S.op(V, lambda: V.tensor_tensor(out=cv[:, :L], in0=cv[:, :L], in1=bg[:, :L], op=ALU.mult))
                    for (q0, qn) in tiles(L, 512):
                        S.op(V, lambda: V.tensor_tensor(out=sq[:, :qn], in0=cv[:, q0:q0 + qn], in1=cv[:, q0:q0 + qn], op=ALU.mult))
                        S.op(PE, lambda: PE.matmul(pf[:, 0, :qn], lhsT=bd64[:], rhs=sq[:, :qn], start=True, stop=True))
                        S.op(A, lambda: A.activation(out=sq[:, :qn], in_=pf[:, 0, :qn], func=AF.Sqrt, bias=epsb[:, 0:1]))
                        S.op(V, lambda: V.reciprocal(out=sq[:, :qn], in_=sq[:, :qn]))
                        S.op(V, lambda: V.scalar_tensor_tensor(out=ob[:, :qn], in0=cv[:, q0:q0 + qn], scalar=cn[:, ch:ch + 1], in1=sq[:, :qn], op0=ALU.mult, op1=ALU.mult))
                        S.op(SY, lambda: SY.dma_start(out=MIXT[ch, :, t0 + q0:t0 + q0 + qn], in_=ob[:, :qn]), True)
                    if b is not None:
                        with nc.allow_non_contiguous_dma(reason="conv state tail (small)"):
                            S.op(SY, lambda: SY.dma_start(out=new_conv_prompt[l, b, :, ch * 128:(ch + 1) * 128].rearrange("j p -> p j"), in_=ext[:, L:L + 2]), True)
                    else:
                        S.op(PE, lambda: PE.transpose(out=pf[:L, 1, 0:128], in_=ext[:, 2:L + 2], identity=ident_f[:]))
                        S.op(V, lambda: V.tensor_copy(out=tm[:L, :], in_=pf[:L, 1, 0:128]))
                        S.op(SY, lambda: SY.dma_start(out=new_conv_sample[l, :, 1, ch * 128:(ch + 1) * 128], in_=tm[:L, :]), True)

    def load_lb(l, lbt, oml):
        if l == 0:
            S.op(V, lambda: V.memset(lbt[:], 0.0))
        else:
            with SBT("lb_a", [128, HG], F32) as a0, SBT("lb_b", [128, HG], F32) as a1:
                load_percol(a0[:, :], lb_logits[0:1, :], HG)
                load_percol(a1[:, :], lb_logits[1:2, :], HG)
                S.op(V, lambda: V.tensor_tensor(out=a1[:], in0=a1[:], in1=a0[:], op=ALU.subtract))
                S.op(A, lambda: A.activation(out=lbt[:], in_=a1[:], func=AF.Sigmoid))
        S.op(V, lambda: V.tensor_scalar(out=oml[:], in0=lbt[:], scalar1=-1.0, scalar2=1.0, op0=ALU.mult, op1=ALU.add))

    def stage_hgrn_prompt(l):
        L = LCH
        NCK = SEQ // L
        with ExitStack() as st:
            def t(name, shape, dt=F32):
                return st.enter_context(SBT(name, list(shape), dt))
            lbt = t("h_lb", [128, HG]); oml = t("h_oml", [128, HG])
            q = t("h_q", [128, SEQ]); z = t("h_z", [128, SEQ]); Aa = t("h_A", [128, SEQ]); kk = t("h_k", [128, SEQ]); ea = t("h_ea", [128, SEQ])
            qa = t("h_qa", [128, SEQ], BF16); kd = t("h_kd", [128, SEQ], BF16); ke = t("h_ke", [128, SEQ], BF16)
            al = t("h_al", [128, NCK]); el = t("h_el", [128, NCK]); ket = t("h_ket", [L, NCK, 128], BF16)
            vf = t("h_vf", [L, NCK, 128]); vb = t("h_vb", [L, NCK, 128], BF16)
            Sf = t("h_S", [128, 128]); Sb = t("h_Sb", [128, 128], BF16); ptt = t("h_pt", [L, L], BF16); ot = t("h_o", [L, 128])
            load_lb(l, lbt, oml)
            for b in range(NSEQ):
                t0 = b * SEQ
                for h in range(HG):
                    S.op(SY, lambda: SY.dma_start(out=q[:], in_=PA[24 + h, :, t0:t0 + SEQ]), True)
                    S.op(SY, lambda: SY.dma_start(out=z[:], in_=PA[32 + h, :, t0:t0 + SEQ]), True)
                    S.op(SY, lambda: SY.dma_start(out=vf[:], in_=PB[t0:t0 + SEQ, h * 128:(h + 1) * 128].rearrange("(n p) v -> p n v", p=L)), True)
                    S.op(V, lambda: V.tensor_copy(out=vb[:], in_=vf[:]))
                    S.op(A, lambda: A.activation(out=z[:], in_=z[:], func=AF.Sigmoid))
                    S.op(V, lambda: V.tensor_scalar(out=z[:], in0=z[:], scalar1=oml[:, h:h + 1], scalar2=lbt[:, h:h + 1], op0=ALU.mult, op1=ALU.add))
                    S.op(V, lambda: V.tensor_scalar(out=kk[:], in0=z[:], scalar1=-1.0, scalar2=1.0, op0=ALU.mult, op1=ALU.add))
                    S.op(V, lambda: V.tensor_scalar_max(out=z[:], in0=z[:], scalar1=F_MIN))
                    S.op(A, lambda: A.activation(out=z[:], in_=z[:], func=AF.Ln))
                    S.op(V, lambda: V.tensor_tensor_scan(out=Aa[:], data0=scanm[:], data1=z[:], initial=0.0, op0=ALU.mult, op1=ALU.add))
                    S.op(V, lambda: V.tensor_copy(out=al[:], in_=Aa[:].rearrange("p (n l) -> p n l", l=L)[:, :, L - 1]))
                    S.op(A, lambda: A.activation(out=el[:], in_=al[:], func=AF.Exp))
                    S.op(A, lambda: A.activation(out=ea[:], in_=Aa[:], func=AF.Exp))
                    S.op(V, lambda: V.tensor_tensor(out=qa[:], in0=q[:], in1=ea[:], op=ALU.mult))
                    S.op(V, lambda: V.tensor_tensor(out=ea[:].rearrange("p (n l) -> p n l", l=L), in0=Aa[:].rearrange("p (n l) -> p n l", l=L),
                                                    in1=al[:].unsqueeze(2).to_broadcast([128, NCK, L]), op=ALU.subtract))
                    S.op(A, lambda: A.activation(out=ea[:], in_=ea[:], func=AF.Exp, scale=-1.0))
                    S.op(V, lambda: V.tensor_tensor(out=ke[:], in0=kk[:], in1=ea[:], op=ALU.mult))
                    S.op(V, lambda: V.tensor_scalar(out=ea[:], in0=Aa[:], scalar1=-1.0, scalar2=80.0, op0=ALU.mult, op1=ALU.min))
                    S.op(A, lambda: A.activation(out=ea[:], in_=ea[:], func=AF.Exp))
                    S.op(V, lambda: V.tensor_tensor(out=kd[:], in0=kk[:], in1=ea[:], op=ALU.mult))
                    for c0 in range(0, NCK, 16):
                        nck = min(16, NCK - c0)
                        S.grp(PE, [(lambda j=j: PE.transpose(out=pt[0:L, j * 128:(j + 1) * 128], in_=ke[:, (c0 + j) * L:(c0 + j + 1) * L], identity=ident_b[:]))
                                   for j in range(nck)])
                        S.op(V, lambda: V.tensor_copy(out=ket[:, c0:c0 + nck, :], in_=pt[0:L, 0:nck * 128].rearrange("p (n d) -> p n d", d=128)))
                    S.op(V, lambda: V.memset(Sf[:], 0.0))
                    S.op(V, lambda: V.memset(Sb[:], 0.0))
                    for ck in range(NCK):
                        cs = slice(ck * L, (ck + 1) * L)
                        S.op(PE, lambda: PE.matmul(pf[0:L, 0, 0:L], lhsT=kd[:, cs], rhs=qa[:, cs], start=True, stop=True))
                        S.op(V, lambda: V.tensor_tensor(out=ptt[:], in0=pf[0:L, 0, 0:L], in1=mask64[0:L, 0:L], op=ALU.mult))
                        S.grp(PE, [lambda: PE.matmul(pf[0:L, 1, 0:128], lhsT=ptt[:], rhs=vb[:, ck, :], start=True, stop=False),
                                   lambda: PE.matmul(pf[0:L, 1, 0:128], lhsT=qa[:, cs], rhs=Sb[:], start=False, stop=True)])
                        S.op(V, lambda: V.tensor_copy(out=ot[:], in_=pf[0:L, 1, 0:128]))
                        S.op(SY, lambda: SY.dma_start(out=PO[t0 + ck * L:t0 + (ck + 1) * L, h * 128:(h + 1) * 128], in_=ot[:]), True)
                        S.op(PE, lambda: PE.matmul(pf[:, 0, 128:256], lhsT=ket[:, ck, :], rhs=vb[:, ck, :], start=True, stop=True))
                        S.op(V, lambda: V.scalar_tensor_tensor(out=Sf[:], in0=Sf[:], scalar=el[:, ck:ck + 1], in1=pf[:, 0, 128:256], op0=ALU.mult, op1=ALU.add))
                        S.op(V, lambda: V.tensor_copy(out=Sb[:], in_=Sf[:]))
                    S.op(SY, lambda: SY.dma_start(out=new_hgrn_prompt[l, b, h, :, :], in_=Sf[:]), True)

    def stage_hgrn_sample(l):
        with ExitStack() as st:
            def t(name, shape, dt=F32):
                return st.enter_context(SBT(name, list(shape), dt))
            lbt = t("s_lb", [128, HG]); oml = t("s_oml", [128, HG])
            q = t("s_q", [128, HG, TS]); f = t("s_f", [128, HG, TS]); k = t("s_k", [128, HG, TS])
            so = t("s_so", [128, HG, 128]); sn = t("s_sn", [128, HG, 128]); vB = t("s_vB", [128, HG, 128]); orow = t("s_or", [1, 1024])
            load_lb(l, lbt, oml)
            for h in range(HG):
                S.op(SY, lambda h=h: SY.dma_start(out=q[:, h, :], in_=PA[24 + h, :, TP:TP + TS]), True)
                S.op(SY, lambda h=h: SY.dma_start(out=f[:, h, :], in_=PA[32 + h, :, TP:TP + TS]), True)
                S.op(A, lambda h=h: A.activation(out=f[:, h, :], in_=f[:, h, :], func=AF.Sigmoid))
                S.op(V, lambda h=h: V.tensor_scalar(out=f[:, h, :], in0=f[:, h, :], scalar1=oml[:, h:h + 1], scalar2=lbt[:, h:h + 1], op0=ALU.mult, op1=ALU.add))
                S.op(V, lambda h=h: V.tensor_scalar(out=k[:, h, :], in0=f[:, h, :], scalar1=-1.0, scalar2=1.0, op0=ALU.mult, op1=ALU.add))
                S.op(V, lambda h=h: V.tensor_scalar_max(out=f[:, h, :], in0=f[:, h, :], scalar1=F_MIN))
            for b in range(TS):
                S.op(SY, lambda: SY.dma_start(out=so[:], in_=state_hgrn[l, b].rearrange("h d v -> d h v")), True)
                S.op(SY, lambda: SY.dma_start(out=vB[:].rearrange("p h v -> p (h v)"), in_=row_bcast(PB[TP + b:TP + b + 1, 0:1024], 128)), True)
                for h in range(HG):
                    S.op(V, lambda h=h: V.tensor_scalar(out=vB[:, h, :], in0=vB[:, h, :], scalar1=k[:, h, b:b + 1], scalar2=None, op0=ALU.mult))
                    S.op(V, lambda h=h: V.scalar_tensor_tensor(out=sn[:, h, :], in0=so[:, h, :], scalar=f[:, h, b:b + 1], in1=vB[:, h, :], op0=ALU.mult, op1=ALU.add))
                S.grp(PE, [(lambda h=h: PE.matmul(pf[0:1, h // 4, (h % 4) * 128:(h % 4 + 1) * 128], lhsT=q[:, h, b:b + 1], rhs=sn[:, h, :], start=True, stop=True)) for h in range(HG)])
                S.op(V, lambda: V.tensor_copy(out=orow[:].rearrange("p (a c) -> p a c", a=2), in_=pf[0:1, :, :]))
                S.op(SY, lambda: SY.dma_start(out=PO[TP + b:TP + b + 1, :], in_=orow[:]), True)
                S.op(SY, lambda: SY.dma_start(out=new_hgrn_sample[l, b].rearrange("h d v -> d h v"), in_=sn[:]), True)

    def stage_hgrn_post(l):
        with ExitStack() as st:
            def t(name, shape, dt=F32):
                return st.enter_context(SBT(name, list(shape), dt))
            o = t("p_o", [128, 1024]); og = t("p_og", [128, 1024]); sq = t("p_sq", [128, 1024]); hn = t("p_hn", [128, 1024])
            ms = t("p_ms", [128, HG]); ob = t("p_ob", [128, 1024], BF16)
            S.op(SY, lambda: SY.dma_start(out=hn[:], in_=row_bcast(hgrn_norm[l:l + 1, :], 128)), True)
            for (t0, tn) in tiles(TP) + [(TP, TS)]:
                S.op(SY, lambda: SY.dma_start(out=o[:tn, :], in_=PO[t0:t0 + tn, :]), True)
                S.op(SY, lambda: SY.dma_start(out=og[:tn, :], in_=PB[t0:t0 + tn, 1024:2048]), True)
                S.op(V, lambda: V.tensor_tensor(out=sq[:tn, :], in0=o[:tn, :], in1=o[:tn, :], op=ALU.mult))
                S.op(V, lambda: V.reduce_sum(out=ms[:tn, :], in_=sq[:tn, :].rearrange("p (h v) -> p h v", v=128), axis=AX.X))
                S.op(V, lambda: V.tensor_scalar(out=ms[:tn, :], in0=ms[:tn, :], scalar1=1.0 / 128, scalar2=EPS, op0=ALU.mult, op1=ALU.add))
                S.op(A, lambda: A.sqrt(ms[:tn, :], ms[:tn, :]))
                S.op(V, lambda: V.reciprocal(out=ms[:tn, :], in_=ms[:tn, :]))
                S.op(V, lambda: V.tensor_tensor(out=o[:tn, :].rearrange("p (h v) -> p h v", v=128), in0=o[:tn, :].rearrange("p (h v) -> p h v", v=128),
                                                in1=ms[:tn, :].unsqueeze(2).to_broadcast([tn, HG, 128]), op=ALU.mult))
                S.op(V, lambda: V.tensor_tensor(out=o[:tn, :], in0=o[:tn, :], in1=hn[:tn, :], op=ALU.mult))
                S.op(A, lambda: A.activation(out=og[:tn, :], in_=og[:tn, :], func=AF.Silu))
                S.op(V, lambda: V.tensor_tensor(out=ob[:tn, :], in0=o[:tn, :], in1=og[:tn, :], op=ALU.mult))
                transpose_store(ob, tn, MIXT, t0, 8, c0=8)

    def stage_ffn(W1, W3, W2, dff, INT=None, Tn=None, tls=None, evB=None):
        KC = dff // 128
        INT = HT if INT is None else INT
        Tn = T if Tn is None else Tn
        tls = (tiles(TP) + [(TP, TS)]) if tls is None else tls

        def evA(j, t0, tn, pss):
            with SBT("f_g", [128, 512], F32) as g, SBT("f_o", [128, 512], BF16) as o:
                S.op(A, lambda: A.activation(out=g[:, :tn], in_=pss[0], func=AF.Silu))
                S.op(V, lambda: V.tensor_tensor(out=o[:, :tn], in0=g[:, :tn], in1=pss[1], op=ALU.mult))
                S.op(SY, lambda: SY.dma_start(out=AT[j, :, t0:t0 + tn], in_=o[:, :tn]), True)
        gemm_a([W1, W3], INT, DC, dff, Tn, evA)
        gemm_b(W2, AT, KC, D, tls, store_b(FO) if evB is None else evB)

    def store_bf(DST):
        def ev(t0, tn, nb, ncol, p):
            with SBT("sbf_ev", [128, 512], BF16) as o:
                S.op(V, lambda: V.tensor_copy(out=o[:tn, :ncol], in_=p))
                S.op(SY, lambda: SY.dma_start(out=DST[t0:t0 + tn, nb:nb + ncol], in_=o[:tn, :ncol]), True)
        return ev

    stage_mod()
    for l in range(DEPTH):
        stage_normmod(l, 0)
        stage_win(l)
        stage_conv(l)
        stage_hgrn_prompt(l)
        stage_hgrn_sample(l)
        stage_hgrn_post(l)
        gemm_b(w_out[l], MIXT, DC, D, tiles(TP) + [(TP, TS)], store_b(FO))
        stage_epilogue(l, 0)
        if l % 2 == 0:
            stage_normmod(l, 1)
            stage_ffn(ffn_w1[l // 2], ffn_w3[l // 2], ffn_w2[l // 2], c.dff)
        else:
            stage_normmod(l, 1, router=True)
            stage_he_transpose()
            for e in range(NE):
                stage_ffn(moe_w1[l // 2, e], moe_w3[l // 2, e], moe_w2[l // 2, e], c.dffe, INT=HE[e], Tn=TE, tls=tiles(TE), evB=store_bf(YE[e * TE:(e + 1) * TE, :]))
            stage_combine()
        stage_epilogue(l, 1, final=(l == DEPTH - 1))
    S.fin(SY)
    es.close()
    return nc


_NAMES = ["x_prompt", "x_sample", "state_conv", "state_hgrn", "c_prompt", "c_sample", "norm_pre", "norm_post", "w_mod", "b_mod",
          "w_in", "conv_w", "conv_norm", "lb_logits", "hgrn_norm", "w_out", "ffn_w1", "ffn_w3", "ffn_w2", "router_w", "router_b",
          "moe_w1", "moe_w3", "moe_w2"]
_OUTS = ["y_prompt", "y_sample", "new_conv_prompt", "new_hgrn_prompt", "new_conv_sample", "new_hgrn_sample"]


def kernel(**inputs):
    nseq, seq = inputs["x_prompt"].shape[0], inputs["x_prompt"].shape[1]
    cfg = Cfg(nseq=nseq, seq=seq, ts=inputs["x_sample"].shape[0], dff=inputs["ffn_w1"].shape[2], dffe=inputs["moe_w1"].shape[3])
    nc = build(cfg)
    in_map = {k: np.ascontiguousarray(np.asarray(inputs[k], dtype=np.float32)) for k in _NAMES}
    res = run_bass_kernel_spmd(nc, [in_map], core_ids=[0])
    r = res.results[0]
    return tuple(np.asarray(r[k], dtype=np.float32) for k in _OUTS)
```

```python
import numpy as np
import concourse.bass as bass
import concourse.mybir as mybir
from concourse.bass_utils import run_bass_kernel_spmd

F32, BF16 = mybir.dt.float32, mybir.dt.bfloat16
AF = mybir.ActivationFunctionType
ALU = mybir.AluOpType
AX = mybir.AxisListType

D = 2048
DC = D // 128
DEPTH = 2
CONV_DIM = 1024
HG = 8
NE = 8
EPS = 1e-6
F_MIN = 1e-6
LCH = 32
BIGIDX = 1 << 24


class Cfg:
    def __init__(s, nseq=4, seq=2048, ts=128, dff=5632, dffe=7168):
        s.nseq, s.seq, s.ts, s.dff, s.dffe = nseq, seq, ts, dff, dffe
        s.tp = nseq * seq
        s.T = s.tp + ts
        s.R = nseq + ts


def tiles(T, step=128):
    return [(i, min(step, T - i)) for i in range(0, T, step)]


class Seq:
    def __init__(s, nc, sem):
        s.nc, s.sem, s.val, s.sym, s.dry = nc, sem, 0, None, False

    def _w(s):
        return s.val if s.sym is None else s.sym + s.val

    def op(s, eng, fn, dma=False):
        inc = 16 if dma else 1
        if not s.dry:
            eng.wait_ge(s.sem, s._w())
            fn().then_inc(s.sem, inc)
        s.val += inc

    def grp(s, eng, fns):
        if not s.dry:
            eng.wait_ge(s.sem, s._w())
            ins = None
            for f in fns:
                ins = f()
            ins.then_inc(s.sem, 1)
        s.val += 1

    def fin(s, eng):
        eng.wait_ge(s.sem, s.val)


def build(cfg):
    nc = bass.Bass("TRN2", target_bir_lowering=False)
    c = cfg
    T, TP, TS, R, NSEQ, SEQ = c.T, c.tp, c.ts, c.R, c.nseq, c.seq

    def din(name, shape):
        return nc.dram_tensor(name, list(shape), F32, kind="ExternalInput").ap()

    def dout(name, shape):
        return nc.dram_tensor(name, list(shape), F32, kind="ExternalOutput").ap()

    def dscr(name, shape, dt=F32):
        return nc.dram_tensor(name, list(shape), dt, kind="Internal").ap()

    x_prompt = din("x_prompt", [NSEQ, SEQ, D]); x_sample = din("x_sample", [TS, 1, D])
    state_conv = din("state_conv", [DEPTH, TS, 2, CONV_DIM]); state_hgrn = din("state_hgrn", [DEPTH, TS, HG, 128, 128])
    c_prompt = din("c_prompt", [NSEQ, D]); c_sample = din("c_sample", [TS, D])
    norm_pre = din("norm_pre", [DEPTH, 2, D]); norm_post = din("norm_post", [DEPTH, 2, D])
    w_mod = din("w_mod", [DEPTH, D, 6 * D]); b_mod = din("b_mod", [DEPTH, 6 * D])
    w_in = din("w_in", [DEPTH, D, 7168]); conv_w = din("conv_w", [DEPTH, 3, CONV_DIM])
    conv_norm = din("conv_norm", [DEPTH, CONV_DIM]); lb_logits = din("lb_logits", [DEPTH, 1024])
    hgrn_norm = din("hgrn_norm", [DEPTH, 1024]); w_out = din("w_out", [DEPTH, D, D])
    ffn_w1 = din("ffn_w1", [1, D, c.dff]); ffn_w3 = din("ffn_w3", [1, D, c.dff]); ffn_w2 = din("ffn_w2", [1, c.dff, D])
    router_w = din("router_w", [1, D, NE]); router_b = din("router_b", [1, NE])
    moe_w1 = din("moe_w1", [1, NE, D, c.dffe]); moe_w3 = din("moe_w3", [1, NE, D, c.dffe]); moe_w2 = din("moe_w2", [1, NE, c.dffe, D])

    y_prompt = dout("y_prompt", [NSEQ, SEQ, D]); y_sample = dout("y_sample", [TS, 1, D])
    new_conv_prompt = dout("new_conv_prompt", [DEPTH, NSEQ, 2, CONV_DIM])
    new_hgrn_prompt = dout("new_hgrn_prompt", [DEPTH, NSEQ, HG, 128, 128])
    new_conv_sample = dout("new_conv_sample", [DEPTH, TS, 2, CONV_DIM])
    new_hgrn_sample = dout("new_hgrn_sample", [DEPTH, TS, HG, 128, 128])

    KCMAX = max(c.dff, c.dffe, D) // 128
    X = dscr("X", [T, D]); MOD = dscr("MOD", [DEPTH, R, 6 * D]); CFT = dscr("CFT", [DC, 128, R], BF16)
    HT = dscr("HT", [DC, 128, T], BF16); PA = dscr("PA", [40, 128, T]); PB = dscr("PB", [T, 2048])
    PO = dscr("PO", [T, 1024]); MIXT = dscr("MIXT", [DC, 128, T], BF16); FO = dscr("FO", [T, D])
    AT = dscr("AT", [KCMAX, 128, T], BF16); GT = dscr("GT", [T, NE])
    TE = min(T, ((int(1.72 * 2 * T / NE) + 511) // 512) * 512)
    HETM = dscr("HETM", [NE * TE, D], BF16); HE = dscr("HE", [NE, DC, 128, TE], BF16); YE = dscr("YE", [NE * TE, D], BF16)
    IDXD = dscr("IDXD", [T, 2], mybir.dt.int32); G2D = dscr("G2D", [T, 2])

    from contextlib import ExitStack
    es = ExitStack()
    sem = es.enter_context(nc.semaphore("g"))
    S = Seq(nc, sem)
    lsem = es.enter_context(nc.semaphore("lsem"))
    es.enter_context(nc.allow_non_contiguous_dma(reason="small strided tiles"))
    V, A, P, PE, SY = nc.vector, nc.scalar, nc.gpsimd, nc.tensor, nc.sync

    _cnt = [0]

    def SBT(name, shape, dt=F32):
        _cnt[0] += 1
        return nc.sbuf_tensor(f"{name}_{_cnt[0]}", list(shape), dt)

    def sb(name, shape, dt=F32):
        return es.enter_context(SBT(name, list(shape), dt))

    ident_b = sb("ident_b", [128, 128], BF16); ident_f = sb("ident_f", [128, 128])
    mask64 = sb("mask64", [128, 64]); bd64 = sb("bd64", [128, 128]); ones_t = sb("ones_t", [128, 128])
    scanm = sb("scanm", [128, SEQ]); epsb = sb("epsb", [128, 1])
    ps = es.enter_context(nc.psum_tensor("ps", [128, 4, 512], F32))
    pt = es.enter_context(nc.psum_tensor("pt", [128, 2048], BF16))
    pf = es.enter_context(nc.psum_tensor("pf", [128, 2, 512], F32))

    S.op(P, lambda: P.memset(ones_t[:], 1.0))
    S.op(P, lambda: P.memset(epsb[:], EPS))
    S.op(P, lambda: P.affine_select(out=ident_f[:], in_=ones_t[:], pattern=[[-1, 128]], compare_op=ALU.is_equal,
                                     fill=0.0, base=0, channel_multiplier=1))
    S.op(V, lambda: V.tensor_copy(out=ident_b[:], in_=ident_f[:]))
    S.op(P, lambda: P.affine_select(out=mask64[0:64, :], in_=ones_t[0:64, 0:64], pattern=[[1, 64]], compare_op=ALU.is_ge,
                                     fill=0.0, base=0, channel_multiplier=-1))
    S.op(P, lambda: P.memset(bd64[:], 0.0))
    S.op(P, lambda: P.memset(bd64[0:64, 0:64], 1.0 / 64))
    S.op(P, lambda: P.memset(bd64[64:128, 64:128], 1.0 / 64))
    S.op(P, lambda: P.memset(scanm[:], 1.0))
    S.op(P, lambda: P.memset(scanm[:].rearrange("p (n l) -> p n l", l=LCH)[:, :, 0:1], 0.0))

    tri_b = sb("tri_b", [128, 128], BF16); ones_b = sb("ones_b", [128, 128], BF16); tri_f = sb("tri_f", [128, 128])
    eoff_i = sb("eoff_i", [128, NE], mybir.dt.int32); eoff = sb("eoff", [128, NE])
    S.op(P, lambda: P.affine_select(out=tri_f[:], in_=ones_t[:], pattern=[[1, 128]], compare_op=ALU.is_gt, fill=0.0, base=0, channel_multiplier=-1))
    S.op(V, lambda: V.tensor_copy(out=tri_b[:], in_=tri_f[:]))
    S.op(V, lambda: V.tensor_copy(out=ones_b[:], in_=ones_t[:]))
    S.op(P, lambda: P.iota(eoff_i[:], pattern=[[TE, NE]], base=0, channel_multiplier=0))
    S.op(V, lambda: V.tensor_copy(out=eoff[:], in_=eoff_i[:]))

    bcreg = P.alloc_register("bcreg")
    P.reg_mov(bcreg, NE * TE - 1)

    S.op(SY, lambda: SY.dma_start(out=X[0:TP, :], in_=x_prompt.rearrange("b t d -> (b t) d")), True)
    S.op(SY, lambda: SY.dma_start(out=X[TP:T, :], in_=x_sample.rearrange("b t d -> (b t) d")), True)

    DQ = [SY]

    def row_bcast(ap_row, n=128):
        return ap_row.partition_broadcast(n)

    def transpose_store(src, tn, DST, t0, nchunks, c0=0):
        with SBT("tstage", [128, nchunks, 128], BF16) as stg:
            S.grp(PE, [(lambda k=k: PE.transpose(out=pt[:, k * 128:k * 128 + tn], in_=src[:tn, k * 128:(k + 1) * 128],
                                                  identity=ident_b[:tn, :tn])) for k in range(nchunks)])
            S.op(V, lambda: V.tensor_copy(out=stg[:, :, :tn], in_=pt[:, 0:nchunks * 128].rearrange("p (k t) -> p k t", t=128)[:, :, :tn]))
            S.op(SY, lambda: SY.dma_start(out=DST[c0:c0 + nchunks, :, t0:t0 + tn].rearrange("k p t -> p k t"), in_=stg[:, :, :tn]), True)

    def gemm_a_ser(Ws, INT, KC, N, Tn, evac, tgs=512):
        nW = len(Ws)
        cols = 512 // nW
        with SBT("ga_w", [128, nW, KC, cols], BF16) as wb, SBT("ga_in", [128, KC, tgs], BF16) as inb:
            for nb in range(0, N, cols):
                ncol = min(cols, N - nb)
                for wi, W in enumerate(Ws):
                    S.op(P, lambda wi=wi, W=W: P.dma_start(out=wb[:, wi, :, :ncol],
                                                           in_=(W(nb, ncol) if callable(W) else W[:, nb:nb + ncol]).rearrange("(k p) n -> p k n", p=128)), True)
                for (t0, tn) in tiles(Tn, tgs):
                    S.op(DQ[0], lambda: DQ[0].dma_start(out=inb[:, :, :tn], in_=INT[0:KC, :, t0:t0 + tn].rearrange("k p t -> p k t")), True)
                    for j in range(ncol // 128):
                        fns = []
                        for wi in range(nW):
                            for k in range(KC):
                                fns.append(lambda wi=wi, k=k: PE.matmul(ps[:, wi * 2 + (j % 2) if nW == 2 else j, :tn],
                                                                        lhsT=wb[:, wi, k, j * 128:(j + 1) * 128], rhs=inb[:, k, :tn],
                                                                        start=(k == 0), stop=(k == KC - 1)))
                        S.grp(PE, fns)
                        evac((nb // 128) + j, t0, tn, [ps[:, wi * 2 + (j % 2) if nW == 2 else j, :tn] for wi in range(nW)])

    def gemm_b_ser(W, INT, KC, N, tls, evac):
        with SBT("gb_w", [128, KC, 512], BF16) as wb, SBT("gb_in", [128, KC, 128], BF16) as inb:
            for nb in range(0, N, 512):
                ncol = min(512, N - nb)
                for k0 in range(0, KC, 16):
                    kn = min(16, KC - k0)
                    S.op(P, lambda: P.dma_start(out=wb[:, k0:k0 + kn, :ncol],
                                                in_=W[k0 * 128:(k0 + kn) * 128, nb:nb + ncol].rearrange("(k p) n -> p k n", p=128)), True)
                for (t0, tn) in tls:
                    S.op(DQ[0], lambda: DQ[0].dma_start(out=inb[:, :, :tn], in_=INT[0:KC, :, t0:t0 + tn].rearrange("k p t -> p k t")), True)
                    S.grp(PE, [(lambda k=k: PE.matmul(ps[:tn, 0, :ncol], lhsT=inb[:, k, :tn], rhs=wb[:, k, :ncol],
                                                      start=(k == 0), stop=(k == KC - 1))) for k in range(KC)])
                    evac(t0, tn, nb, ncol, ps[:tn, 0, :ncol])

    def PW(eng, sem_, val_):
        eng.wait_ge(sem_, val_)
        eng.nop(nofuse=True)

    psem = {k: es.enter_context(nc.semaphore("p_" + k)) for k in ("w", "in", "mm", "e1", "ev", "st")}
    pc = {k: 0 for k in psem}

    def pipe_enter():
        for eng in (P, SY, PE, V, A):
            PW(eng, S.sem, S.val)

    def pipe_exit():
        PW(V, psem["st"], pc["st"])
        PW(V, psem["ev"], pc["ev"])
        S.op(V, lambda: V.memset(epsb[:], EPS))

    def gemm_a(Ws, INT, KC, N, Tn, kind, DST, tgs=512):
        nW = len(Ws)
        cols = 512 // nW
        NB = 2
        odt = F32 if kind == "copy" else BF16
        with ExitStack() as st_:
            wb = [st_.enter_context(SBT("ga_w", [128, nW, KC, cols], BF16)) for _ in range(2)]
            inb = [st_.enter_context(SBT("ga_in", [128, KC, tgs], BF16)) for _ in range(2)]
            ob = [st_.enter_context(SBT("ga_o", [128, 512], odt)) for _ in range(2)]
            gb_ = [st_.enter_context(SBT("ga_g", [128, 512], F32)) for _ in range(2)] if kind == "swiglu" else None
            pipe_enter()
            blocks = [(nb, min(cols, N - nb)) for nb in range(0, N, cols)]
            tgl = tiles(Tn, tgs)
            mm_of_block_end = {}
            w0 = pc["w"]; in0 = pc["in"]; mm0 = pc["mm"]; ev0 = pc["ev"]; st0 = pc["st"]; e10 = pc["e1"]
            gi = 0
            li = 0
            mm_after_load = {}

            def issue_w(bi):
                nb, ncol = blocks[bi]
                if bi >= 2:
                    PW(P, psem["mm"], mm_of_block_end[bi - 2])
                for wi, W in enumerate(Ws):
                    P.dma_start(out=wb[bi % 2][:, wi, :, :ncol], in_=W[:, nb:nb + ncol].rearrange("(k p) n -> p k n", p=128)).then_inc(psem["w"], 16)
                    pc["w"] += 16

            def issue_in(l_i, t0, tn):
                if l_i >= 2:
                    PW(SY, psem["mm"], mm_after_load[l_i - 2])
                SY.dma_start(out=inb[l_i % 2][:, :, :tn], in_=INT[0:KC, :, t0:t0 + tn].rearrange("k p t -> p k t")).then_inc(psem["in"], 16)
                pc["in"] += 16

            seq_ = [(bi, ti) for bi in range(len(blocks)) for ti in range(len(tgl))]
            issue_w(0)
            issue_in(0, *tgl[0])
            for si, (bi, ti) in enumerate(seq_):
                nb, ncol = blocks[bi]
                t0, tn = tgl[ti]
                if ti == 0 and bi + 1 < len(blocks):
                    pass
                if si + 1 < len(seq_):
                    nbi, nti = seq_[si + 1]
                    if nbi != bi:
                        pass
                w_need = w0 + 16 * nW * (bi + 1)
                for j in range(ncol // 128):
                    st_i = gi % NB
                    PW(PE, psem["w"], w_need)
                    PW(PE, psem["in"], in0 + 16 * (li + 1))
                    if gi >= NB:
                        PW(PE, psem["ev"], ev0 + (gi - NB + 1))
                    ins = None
                    for wi in range(nW):
                        for k in range(KC):
                            ins = PE.matmul(ps[:, st_i * 2 + wi, :tn], lhsT=wb[bi % 2][:, wi, k, j * 128:(j + 1) * 128], rhs=inb[li % 2][:, k, :tn],
                                            start=(k == 0), stop=(k == KC - 1))
                    ins.then_inc(psem["mm"], 1); pc["mm"] += 1
                    o = ob[gi % 2]
                    if kind == "swiglu":
                        PW(A, psem["mm"], mm0 + gi + 1)
                        if gi >= 2:
                            PW(A, psem["ev"], ev0 + gi - 1)
                        A.activation(out=gb_[gi % 2][:, :tn], in_=ps[:, st_i * 2, :tn], func=AF.Silu).then_inc(psem["e1"], 1); pc["e1"] += 1
                        PW(V, psem["e1"], e10 + gi + 1)
                    else:
                        PW(V, psem["mm"], mm0 + gi + 1)
                    if gi >= 2:
                        PW(V, psem["st"], st0 + 16 * (gi - 1))
                    if kind == "swiglu":
                        V.tensor_tensor(out=o[:, :tn], in0=gb_[gi % 2][:, :tn], in1=ps[:, st_i * 2 + 1, :tn], op=ALU.mult).then_inc(psem["ev"], 1)
                    else:
                        V.tensor_copy(out=o[:, :tn], in_=ps[:, st_i * 2, :tn]).then_inc(psem["ev"], 1)
                    pc["ev"] += 1
                    PW(A, psem["ev"], ev0 + gi + 1)
                    A.dma_start(out=DST[(nb // 128) + j, :, t0:t0 + tn], in_=o[:, :tn]).then_inc(psem["st"], 16); pc["st"] += 16
                    gi += 1
                mm_after_load[li] = mm0 + gi
                if ti == len(tgl) - 1:
                    mm_of_block_end[bi] = mm0 + gi
                if si + 1 < len(seq_):
                    nbi, nti = seq_[si + 1]
                    if nbi != bi:
                        issue_w(nbi)
                    issue_in(li + 1, *tgl[nti])
                li += 1
            pipe_exit()

    def gemm_b(W, INT, KC, N, tls, DST, odt=F32):
        with ExitStack() as st_:
            wb = [st_.enter_context(SBT("gb_w", [128, KC, 512], BF16)) for _ in range(2 if KC <= 32 else 1)]
            inb = [st_.enter_context(SBT("gb_in", [128, KC, 128], BF16)) for _ in range(2)]
            ob = [st_.enter_context(SBT("gb_o", [128, 512], odt)) for _ in range(2)]
            nwb = len(wb)
            pipe_enter()
            blocks = [(nb, min(512, N - nb)) for nb in range(0, N, 512)]
            w0 = pc["w"]; in0 = pc["in"]; mm0 = pc["mm"]; ev0 = pc["ev"]; st0 = pc["st"]
            nkd = (KC + 15) // 16
            mm_of_block_end = {}
            seq_ = [(bi, ti) for bi in range(len(blocks)) for ti in range(len(tls))]

            def issue_w(bi):
                nb, ncol = blocks[bi]
                if bi >= nwb:
                    PW(P, psem["mm"], mm_of_block_end[bi - nwb])
                for k0 in range(0, KC, 16):
                    kn = min(16, KC - k0)
                    P.dma_start(out=wb[bi % nwb][:, k0:k0 + kn, :ncol],
                                in_=W[k0 * 128:(k0 + kn) * 128, nb:nb + ncol].rearrange("(k p) n -> p k n", p=128)).then_inc(psem["w"], 16)
                    pc["w"] += 16

            def issue_in(gi_, t0, tn):
                if gi_ >= 2:
                    PW(SY, psem["mm"], mm0 + gi_ - 1)
                SY.dma_start(out=inb[gi_ % 2][:, :, :tn], in_=INT[0:KC, :, t0:t0 + tn].rearrange("k p t -> p k t")).then_inc(psem["in"], 16)
                pc["in"] += 16

            issue_w(0)
            issue_in(0, *tls[0])
            for gi, (bi, ti) in enumerate(seq_):
                nb, ncol = blocks[bi]
                t0, tn = tls[ti]
                bank = gi % 4
                PW(PE, psem["w"], w0 + 16 * nkd * (bi + 1))
                PW(PE, psem["in"], in0 + 16 * (gi + 1))
                if gi >= 4:
                    PW(PE, psem["ev"], ev0 + gi - 3)
                ins = None
                for k in range(KC):
                    ins = PE.matmul(ps[:tn, bank, :ncol], lhsT=inb[gi % 2][:, k, :tn], rhs=wb[bi % nwb][:, k, :ncol], start=(k == 0), stop=(k == KC - 1))
                ins.then_inc(psem["mm"], 1); pc["mm"] += 1
                if ti == len(tls) - 1:
                    mm_of_block_end[bi] = mm0 + gi + 1
                PW(V, psem["mm"], mm0 + gi + 1)
                if gi >= 2:
                    PW(V, psem["st"], st0 + 16 * (gi - 1))
                V.tensor_copy(out=ob[gi % 2][:tn, :ncol], in_=ps[:tn, bank, :ncol]).then_inc(psem["ev"], 1); pc["ev"] += 1
                PW(A, psem["ev"], ev0 + gi + 1)
                A.dma_start(out=DST[t0:t0 + tn, nb:nb + ncol], in_=ob[gi % 2][:tn, :ncol]).then_inc(psem["st"], 16); pc["st"] += 16
                if gi + 1 < len(seq_):
                    nbi, nti = seq_[gi + 1]
                    if nbi != bi:
                        issue_w(nbi)
                    issue_in(gi + 1, *tls[nti])
            pipe_exit()

    def store_b(DST):
        def ev(t0, tn, nb, ncol, p):
            with SBT("sb_ev", [128, 512], F32) as o:
                S.op(V, lambda: V.tensor_copy(out=o[:tn, :ncol], in_=p))
                S.op(SY, lambda: SY.dma_start(out=DST[t0:t0 + tn, nb:nb + ncol], in_=o[:tn, :ncol]), True)
        return ev

    def mod_rows(l, comp, t0, tn):
        if t0 < TP:
            b = t0 // SEQ
            return row_bcast(MOD[l, b:b + 1, comp * D:(comp + 1) * D], tn)
        r0 = NSEQ + (t0 - TP)
        return MOD[l, r0:r0 + tn, comp * D:(comp + 1) * D]

    def rstd_of(src, tn, out_rstd, junk):
        S.op(A, lambda: A.activation(out=junk[:tn, :], in_=src, func=AF.Square, accum_out=out_rstd[:tn, :]))
        S.op(V, lambda: V.tensor_scalar(out=out_rstd[:tn, :], in0=out_rstd[:tn, :], scalar1=1.0 / D, scalar2=EPS, op0=ALU.mult, op1=ALU.add))
        S.op(A, lambda: A.sqrt(out_rstd[:tn, :], out_rstd[:tn, :]))
        S.op(V, lambda: V.reciprocal(out=out_rstd[:tn, :], in_=out_rstd[:tn, :]))

    def stage_mod():
        with SBT("m_c", [128, D], F32) as ct, SBT("m_cb", [128, D], BF16) as cb:
            for (r0, rn, src) in [(0, NSEQ, c_prompt), (NSEQ, TS, c_sample)]:
                S.op(SY, lambda: SY.dma_start(out=ct[:rn, :], in_=src[:, :]), True)
                S.op(A, lambda: A.activation(out=cb[:rn, :], in_=ct[:rn, :], func=AF.Silu))
                transpose_store(cb, rn, CFT, r0, DC)
        for l in range(DEPTH):
            def ev(t0, tn, nb, ncol, p, l=l):
                with SBT("m_b", [128, 512], F32) as bt, SBT("m_o", [128, 512], F32) as o:
                    S.op(SY, lambda: SY.dma_start(out=bt[:tn, :ncol], in_=row_bcast(b_mod[l:l + 1, nb:nb + ncol], tn)), True)
                    S.op(V, lambda: V.tensor_tensor(out=o[:tn, :ncol], in0=p, in1=bt[:tn, :ncol], op=ALU.add))
                    S.op(SY, lambda: SY.dma_start(out=MOD[l, t0:t0 + tn, nb:nb + ncol], in_=o[:tn, :ncol]), True)
            gemm_b_ser(w_mod[l], CFT, DC, 6 * D, [(0, NSEQ), (NSEQ, TS)], ev)

    def stage_normmod(l, which, router=False):
        sc_c, sh_c = (1, 0) if which == 0 else (4, 3)
        with ExitStack() as st:
            def t(name, shape, dt=F32):
                return st.enter_context(SBT(name, list(shape), dt))
            xt = t("n_x", [128, D]); at = t("n_a", [128, D]); sh = t("n_sh", [128, D]); gb = t("n_g", [128, D])
            hf = t("n_hf", [128, D]); rs = t("n_rs", [128, 1]); hb = t("n_hb", [128, D], BF16)
            S.op(SY, lambda: SY.dma_start(out=gb[:], in_=row_bcast(norm_pre[l, which:which + 1, :], 128)), True)
            if router:
                rwb = t("n_rw", [128, NE, D]); rbb = t("n_rb", [128, NE]); lg = t("n_lg", [128, NE]); l2 = t("n_l2", [128, NE])
                m1 = t("n_m1", [128, 1]); m2 = t("n_m2", [128, 1]); k1 = t("n_k1", [128, NE]); k2 = t("n_k2", [128, NE])
                g12 = t("n_g12", [128, 2]); dd = t("n_dd", [128, 1])
                mf = t("n_mf", [128, NE]); mb = t("n_mb", [128, NE], BF16); pos = t("n_pos", [128, NE]); cnt = t("n_cnt", [128, NE])
                ovf = t("n_ovf", [128, NE]); idf = t("n_idf", [128, 2]); idi = t("n_idi", [128, 2], mybir.dt.int32)
                RWT = dscr("RWT", [NE, D])
                S.op(SY, lambda: SY.dma_start(out=RWT[:, :], in_=router_w[0, :, :].rearrange("d e -> e d")), True)
                for e in range(NE):
                    S.op(SY, lambda e=e: SY.dma_start(out=rwb[:, e, :], in_=row_bcast(RWT[e:e + 1, :], 128)), True)
                S.op(SY, lambda: SY.dma_start(out=rbb[:], in_=row_bcast(router_b[0:1, :], 128)), True)
                S.op(V, lambda: V.memset(cnt[:], 0.0))
            for (t0, tn) in tiles(TP) + [(TP, TS)]:
                S.op(SY, lambda: SY.dma_start(out=xt[:tn, :], in_=X[t0:t0 + tn, :]), True)
                S.op(SY, lambda: SY.dma_start(out=at[:tn, :], in_=mod_rows(l, sc_c, t0, tn)), True)
                S.op(SY, lambda: SY.dma_start(out=sh[:tn, :], in_=mod_rows(l, sh_c, t0, tn)), True)
                rstd_of(xt[:tn, :], tn, rs, hf)
                S.op(V, lambda: V.scalar_tensor_tensor(out=at[:tn, :], in0=at[:tn, :], scalar=1.0, in1=gb[:tn, :], op0=ALU.add, op1=ALU.mult))
                S.op(V, lambda: V.scalar_tensor_tensor(out=hf[:tn, :], in0=xt[:tn, :], scalar=rs[:tn, 0:1], in1=at[:tn, :], op0=ALU.mult, op1=ALU.mult))
                S.op(V, lambda: V.tensor_tensor(out=hf[:tn, :], in0=hf[:tn, :], in1=sh[:tn, :], op=ALU.add))
                S.op(A, lambda: A.copy(hb[:tn, :], hf[:tn, :]))
                if not router:
                    transpose_store(hb, tn, HT, t0, DC)
                    continue
                for e in range(NE):
                    S.op(V, lambda e=e: V.tensor_tensor(out=at[:tn, :], in0=hf[:tn, :], in1=rwb[:tn, e, :], op=ALU.mult))
                    S.op(V, lambda e=e: V.reduce_sum(out=lg[:tn, e:e + 1], in_=at[:tn, :], axis=AX.X))
                S.op(V, lambda: V.tensor_tensor(out=lg[:tn, :], in0=lg[:tn, :], in1=rbb[:tn, :], op=ALU.add))
                S.op(V, lambda: V.reduce_max(out=m1[:tn, :], in_=lg[:tn, :], axis=AX.X))
                S.op(V, lambda: V.tensor_scalar(out=k1[:tn, :], in0=lg[:tn, :], scalar1=m1[:tn, 0:1], scalar2=None, op0=ALU.is_equal))
                S.op(V, lambda: V.scalar_tensor_tensor(out=l2[:tn, :], in0=k1[:tn, :], scalar=-1e30, in1=lg[:tn, :], op0=ALU.mult, op1=ALU.add))
                S.op(V, lambda: V.reduce_max(out=m2[:tn, :], in_=l2[:tn, :], axis=AX.X))
                S.op(V, lambda: V.tensor_scalar(out=k2[:tn, :], in0=l2[:tn, :], scalar1=m2[:tn, 0:1], scalar2=None, op0=ALU.is_equal))
                S.op(V, lambda: V.tensor_tensor(out=dd[:tn, :], in0=m1[:tn, :], in1=m2[:tn, :], op=ALU.subtract))
                S.op(A, lambda: A.activation(out=g12[:tn, 0:1], in_=dd[:tn, :], func=AF.Sigmoid))
                S.op(A, lambda: A.activation(out=g12[:tn, 1:2], in_=dd[:tn, :], func=AF.Sigmoid, scale=-1.0))
                S.op(SY, lambda: SY.dma_start(out=G2D[t0:t0 + tn, :], in_=g12[:tn, :]), True)
                S.op(V, lambda: V.tensor_tensor(out=mf[:tn, :], in0=k1[:tn, :], in1=k2[:tn, :], op=ALU.add))
                S.op(V, lambda: V.tensor_copy(out=mb[:tn, :], in_=mf[:tn, :]))
                S.op(PE, lambda: PE.matmul(pf[:tn, 0, 0:NE], lhsT=tri_b[:tn, :tn], rhs=mb[:tn, :], start=True, stop=True))
                S.op(V, lambda: V.tensor_tensor(out=pos[:tn, :], in0=pf[:tn, 0, 0:NE], in1=cnt[:tn, :], op=ALU.add))
                S.op(PE, lambda: PE.matmul(pf[:, 1, 0:NE], lhsT=ones_b[:tn, :], rhs=mb[:tn, :], start=True, stop=True))
                S.op(V, lambda: V.tensor_tensor(out=cnt[:], in0=cnt[:], in1=pf[:, 1, 0:NE], op=ALU.add))
                S.op(V, lambda: V.tensor_scalar(out=ovf[:tn, :], in0=pos[:tn, :], scalar1=float(TE), scalar2=float(BIGIDX), op0=ALU.is_ge, op1=ALU.mult))
                S.op(V, lambda: V.tensor_tensor(out=pos[:tn, :], in0=pos[:tn, :], in1=eoff[:tn, :], op=ALU.add))
                S.op(V, lambda: V.tensor_tensor(out=pos[:tn, :], in0=pos[:tn, :], in1=ovf[:tn, :], op=ALU.add))
                S.op(V, lambda: V.tensor_tensor(out=k1[:tn, :], in0=k1[:tn, :], in1=pos[:tn, :], op=ALU.mult))
                S.op(V, lambda: V.reduce_sum(out=idf[:tn, 0:1], in_=k1[:tn, :], axis=AX.X))
                S.op(V, lambda: V.tensor_tensor(out=k2[:tn, :], in0=k2[:tn, :], in1=pos[:tn, :], op=ALU.mult))
                S.op(V, lambda: V.reduce_sum(out=idf[:tn, 1:2], in_=k2[:tn, :], axis=AX.X))
                S.op(V, lambda: V.tensor_copy(out=idi[:tn, :], in_=idf[:tn, :]))
                S.op(SY, lambda: SY.dma_start(out=IDXD[t0:t0 + tn, :], in_=idi[:tn, :]), True)
                for j in range(2):
                    S.op(P, lambda j=j: P.indirect_dma_start(out=HETM[:, :], out_offset=bass.IndirectOffsetOnAxis(ap=idi[:tn, j:j + 1], axis=0),
                                                             in_=hb[:tn, :], in_offset=None, bounds_check=bcreg, oob_is_err=False), True)

    def stage_he_transpose():
        with SBT("het", [128, D], BF16) as ht_:
            for e in range(NE):
                for (t0, tn) in tiles(TE):
                    S.op(SY, lambda: SY.dma_start(out=ht_[:tn, :], in_=HETM[e * TE + t0:e * TE + t0 + tn, :]), True)
                    transpose_store(ht_, tn, HE[e], t0, DC)

    def stage_combine():
        with ExitStack() as st:
            def t(name, shape, dt=F32):
                return st.enter_context(SBT(name, list(shape), dt))
            r1 = t("cb_r1", [128, D], BF16); r2 = t("cb_r2", [128, D], BF16); y = t("cb_y", [128, D]); g = t("cb_g", [128, 2]); idi = t("cb_i", [128, 2], mybir.dt.int32)
            for (t0, tn) in tiles(TP) + [(TP, TS)]:
                S.op(SY, lambda: SY.dma_start(out=g[:tn, :], in_=G2D[t0:t0 + tn, :]), True)
                S.op(SY, lambda: SY.dma_start(out=idi[:tn, :], in_=IDXD[t0:t0 + tn, :]), True)
                S.op(V, lambda: V.memset(r1[:], 0.0))
                S.op(V, lambda: V.memset(r2[:], 0.0))
                for j, r in ((0, r1), (1, r2)):
                    S.op(P, lambda: P.indirect_dma_start(out=r[:tn, :], out_offset=None, in_=YE[:, :],
                                                         in_offset=bass.IndirectOffsetOnAxis(ap=idi[:tn, j:j + 1], axis=0),
                                                         bounds_check=bcreg, oob_is_err=False), True)
                S.op(V, lambda: V.tensor_scalar(out=y[:tn, :], in0=r1[:tn, :], scalar1=g[:tn, 0:1], scalar2=None, op0=ALU.mult))
                S.op(V, lambda: V.scalar_tensor_tensor(out=y[:tn, :], in0=r2[:tn, :], scalar=g[:tn, 1:2], in1=y[:tn, :], op0=ALU.mult, op1=ALU.add))
                S.op(SY, lambda: SY.dma_start(out=FO[t0:t0 + tn, :], in_=y[:tn, :]), True)

    def stage_epilogue(l, which, final=False):
        ga_c = 2 if which == 0 else 5
        with ExitStack() as st:
            def t(name, shape, dt=F32):
                return st.enter_context(SBT(name, list(shape), dt))
            xt = t("e_x", [128, D]); ft = t("e_f", [128, D]); ga = t("e_ga", [128, D]); gb = t("e_g", [128, D]); jk = t("e_j", [128, D]); rs = t("e_rs", [128, 1])
            S.op(SY, lambda: SY.dma_start(out=gb[:], in_=row_bcast(norm_post[l, which:which + 1, :], 128)), True)
            for (t0, tn) in tiles(TP) + [(TP, TS)]:
                S.op(SY, lambda: SY.dma_start(out=xt[:tn, :], in_=X[t0:t0 + tn, :]), True)
                S.op(SY, lambda: SY.dma_start(out=ft[:tn, :], in_=FO[t0:t0 + tn, :]), True)
                S.op(SY, lambda: SY.dma_start(out=ga[:tn, :], in_=mod_rows(l, ga_c, t0, tn)), True)
                rstd_of(ft[:tn, :], tn, rs, jk)
                S.op(V, lambda: V.tensor_tensor(out=ga[:tn, :], in0=ga[:tn, :], in1=gb[:tn, :], op=ALU.mult))
                S.op(V, lambda: V.scalar_tensor_tensor(out=ft[:tn, :], in0=ft[:tn, :], scalar=rs[:tn, 0:1], in1=ga[:tn, :], op0=ALU.mult, op1=ALU.mult))
                S.op(V, lambda: V.tensor_tensor(out=xt[:tn, :], in0=xt[:tn, :], in1=ft[:tn, :], op=ALU.add))
                S.op(SY, lambda: SY.dma_start(out=X[t0:t0 + tn, :], in_=xt[:tn, :]), True)
                if final:
                    dst = y_prompt.rearrange("b t d -> (b t) d")[t0:t0 + tn, :] if t0 < TP else y_sample.rearrange("b t d -> (b t) d")[t0 - TP:t0 - TP + tn, :]
                    S.op(SY, lambda: SY.dma_start(out=dst, in_=xt[:tn, :]), True)

    def stage_win(l):
        gemm_a([w_in[l, :, 0:5120]], HT, DC, 5120, T, "copy", PA)
        gemm_b(w_in[l, :, 5120:7168], HT, DC, 2048, tiles(TP) + [(TP, TS)], PB)

    def load_percol(dst, src_row, nch):
        with nc.allow_non_contiguous_dma(reason="small per-channel vector"):
            S.op(SY, lambda: SY.dma_start(out=dst, in_=src_row.rearrange("o (c p) -> p (o c)", p=128)), True)

    def stage_conv(l):
        NCH = CONV_DIM // 128
        segs = [(b * SEQ, SEQ, b) for b in range(NSEQ)] + [(TP, TS, None)]
        LM = max(SEQ, TS)
        with ExitStack() as st:
            def t(name, shape, dt=F32):
                return st.enter_context(SBT(name, list(shape), dt))
            cw = t("c_w", [128, 3, NCH]); cn = t("c_n", [128, NCH])
            hc = t("c_hc", [128, LM]); bg = t("c_bg", [128, LM]); ext = t("c_ext", [128, LM + 2]); cv = t("c_cv", [128, LM])
            s0 = t("c_s0", [128, 128]); s1 = t("c_s1", [128, 128]); tm = t("c_tm", [128, 128]); sq = t("c_sq", [128, 512]); ob = t("c_ob", [128, 512], BF16)
            for j in range(3):
                load_percol(cw[:, j, :], conv_w[l, j:j + 1, :], NCH)
            load_percol(cn[:, :], conv_norm[l:l + 1, :], NCH)
            S.op(SY, lambda: SY.dma_start(out=new_conv_sample[l, :, 0, :], in_=state_conv[l, :, 1, :]), True)
            for (t0, L, b) in segs:
                for ch in range(NCH):
                    S.op(SY, lambda: SY.dma_start(out=hc[:, :L], in_=PA[ch, :, t0:t0 + L]), True)
                    S.op(SY, lambda: SY.dma_start(out=bg[:, :L], in_=PA[NCH + ch, :, t0:t0 + L]), True)
                    S.op(SY, lambda: SY.dma_start(out=ext[:, 2:L + 2], in_=PA[2 * NCH + ch, :, t0:t0 + L]), True)
                    S.op(V, lambda: V.tensor_tensor(out=ext[:, 2:L + 2], in0=ext[:, 2:L + 2], in1=hc[:, :L], op=ALU.mult))
                    if b is not None:
                        S.op(V, lambda: V.memset(ext[:, 0:2], 0.0))
                        a0, a1 = ext[:, 0:L], ext[:, 1:L + 1]
                    else:
                        for j, dstt in ((0, s0), (1, s1)):
                            S.op(SY, lambda: SY.dma_start(out=tm[:L, :], in_=state_conv[l, :, j, ch * 128:(ch + 1) * 128]), True)
                            S.op(PE, lambda: PE.transpose(out=pf[:, 0, :L], in_=tm[:L, :], identity=ident_f[:L, :L]))
                            S.op(V, lambda: V.tensor_copy(out=dstt[:, :L], in_=pf[:, 0, :L]))
                        a0, a1 = s0[:, :L], s1[:, :L]
                    S.op(V, lambda: V.tensor_scalar(out=cv[:, :L], in0=a0, scalar1=cw[:, 0, ch:ch + 1], scalar2=None, op0=ALU.mult))
                    S.op(V, lambda: V.scalar_tensor_tensor(out=cv[:, :L], in0=a1, scalar=cw[:, 1, ch:ch + 1], in1=cv[:, :L], op0=ALU.mult, op1=ALU.add))
                    S.op(V, lambda: V.scalar_tensor_tensor(out=cv[:, :L], in0=ext[:, 2:L + 2], scalar=cw[:, 2, ch:ch + 1], in1=cv[:, :L], op0=ALU.mult, op1=ALU.add))
                    S.op(V, lambda: V.tensor_tensor(out=cv[:, :L], in0=cv[:, :L], in1=bg[:, :L], op=ALU.mult))
                    for (q0, qn) in tiles(L, 512):
                        S.op(V, lambda: V.tensor_tensor(out=sq[:, :qn], in0=cv[:, q0:q0 + qn], in1=cv[:, q0:q0 + qn], op=ALU.mult))
                        S.op(PE, lambda: PE.matmul(pf[:, 0, :qn], lhsT=bd64[:], rhs=sq[:, :qn], start=True, stop=True))
                        S.op(A, lambda: A.activation(out=sq[:, :qn], in_=pf[:, 0, :qn], func=AF.Sqrt, bias=epsb[:, 0:1]))
                        S.op(V, lambda: V.reciprocal(out=sq[:, :qn], in_=sq[:, :qn]))
                        S.op(V, lambda: V.scalar_tensor_tensor(out=ob[:, :qn], in0=cv[:, q0:q0 + qn], scalar=cn[:, ch:ch + 1], in1=sq[:, :qn], op0=ALU.mult, op1=ALU.mult))
                        S.op(SY, lambda: SY.dma_start(out=MIXT[ch, :, t0 + q0:t0 + q0 + qn], in_=ob[:, :qn]), True)
                    if b is not None:
                        with nc.allow_non_contiguous_dma(reason="conv state tail (small)"):
                            S.op(SY, lambda: SY.dma_start(out=new_conv_prompt[l, b, :, ch * 128:(ch + 1) * 128].rearrange("j p -> p j"), in_=ext[:, L:L + 2]), True)
                    else:
                        S.op(PE, lambda: PE.transpose(out=pf[:L, 1, 0:128], in_=ext[:, 2:L + 2], identity=ident_f[:]))
                        S.op(V, lambda: V.tensor_copy(out=tm[:L, :], in_=pf[:L, 1, 0:128]))
                        S.op(SY, lambda: SY.dma_start(out=new_conv_sample[l, :, 1, ch * 128:(ch + 1) * 128], in_=tm[:L, :]), True)

    def load_lb(l, lbt, oml):
        if l == 0:
            S.op(V, lambda: V.memset(lbt[:], 0.0))
        else:
            with SBT("lb_a", [128, HG], F32) as a0, SBT("lb_b", [128, HG], F32) as a1:
                load_percol(a0[:, :], lb_logits[0:1, :], HG)
                load_percol(a1[:, :], lb_logits[1:2, :], HG)
                S.op(V, lambda: V.tensor_tensor(out=a1[:], in0=a1[:], in1=a0[:], op=ALU.subtract))
                S.op(A, lambda: A.activation(out=lbt[:], in_=a1[:], func=AF.Sigmoid))
        S.op(V, lambda: V.tensor_scalar(out=oml[:], in0=lbt[:], scalar1=-1.0, scalar2=1.0, op0=ALU.mult, op1=ALU.add))

    def stage_hgrn_prompt(l):
        L = LCH
        NCK = SEQ // L
        with ExitStack() as st:
            def t(name, shape, dt=F32):
                return st.enter_context(SBT(name, list(shape), dt))
            lbt = t("h_lb", [128, HG]); oml = t("h_oml", [128, HG])
            q = t("h_q", [128, SEQ]); z = t("h_z", [128, SEQ]); Aa = t("h_A", [128, SEQ]); kk = t("h_k", [128, SEQ]); ea = t("h_ea", [128, SEQ])
            qa = t("h_qa", [128, SEQ], BF16); kd = t("h_kd", [128, SEQ], BF16); ke = t("h_ke", [128, SEQ], BF16)
            al = t("h_al", [128, NCK]); el = t("h_el", [128, NCK]); ket = t("h_ket", [L, NCK, 128], BF16)
            vf = t("h_vf", [L, NCK, 128]); vb = t("h_vb", [L, NCK, 128], BF16)
            Sf = t("h_S", [128, 128]); Sb = t("h_Sb", [128, 128], BF16); ptt = t("h_pt", [L, L], BF16); ot = t("h_o", [L, 128])
            load_lb(l, lbt, oml)
            for b in range(NSEQ):
                t0 = b * SEQ
                for h in range(HG):
                    S.op(SY, lambda: SY.dma_start(out=q[:], in_=PA[24 + h, :, t0:t0 + SEQ]), True)
                    S.op(SY, lambda: SY.dma_start(out=z[:], in_=PA[32 + h, :, t0:t0 + SEQ]), True)
                    S.op(SY, lambda: SY.dma_start(out=vf[:], in_=PB[t0:t0 + SEQ, h * 128:(h + 1) * 128].rearrange("(n p) v -> p n v", p=L)), True)
                    S.op(V, lambda: V.tensor_copy(out=vb[:], in_=vf[:]))
                    S.op(A, lambda: A.activation(out=z[:], in_=z[:], func=AF.Sigmoid))
                    S.op(V, lambda: V.tensor_scalar(out=z[:], in0=z[:], scalar1=oml[:, h:h + 1], scalar2=lbt[:, h:h + 1], op0=ALU.mult, op1=ALU.add))
                    S.op(V, lambda: V.tensor_scalar(out=kk[:], in0=z[:], scalar1=-1.0, scalar2=1.0, op0=ALU.mult, op1=ALU.add))
                    S.op(V, lambda: V.tensor_scalar_max(out=z[:], in0=z[:], scalar1=F_MIN))
                    S.op(A, lambda: A.activation(out=z[:], in_=z[:], func=AF.Ln))
                    S.op(V, lambda: V.tensor_tensor_scan(out=Aa[:], data0=scanm[:], data1=z[:], initial=0.0, op0=ALU.mult, op1=ALU.add))
                    S.op(V, lambda: V.tensor_copy(out=al[:], in_=Aa[:].rearrange("p (n l) -> p n l", l=L)[:, :, L - 1]))
                    S.op(A, lambda: A.activation(out=el[:], in_=al[:], func=AF.Exp))
                    S.op(A, lambda: A.activation(out=ea[:], in_=Aa[:], func=AF.Exp))
                    S.op(V, lambda: V.tensor_tensor(out=qa[:], in0=q[:], in1=ea[:], op=ALU.mult))
                    S.op(V, lambda: V.tensor_tensor(out=ea[:].rearrange("p (n l) -> p n l", l=L), in0=Aa[:].rearrange("p (n l) -> p n l", l=L),
                                                    in1=al[:].unsqueeze(2).to_broadcast([128, NCK, L]), op=ALU.subtract))
                    S.op(A, lambda: A.activation(out=ea[:], in_=ea[:], func=AF.Exp, scale=-1.0))
                    S.op(V, lambda: V.tensor_tensor(out=ke[:], in0=kk[:], in1=ea[:], op=ALU.mult))
                    S.op(V, lambda: V.tensor_scalar(out=ea[:], in0=Aa[:], scalar1=-1.0, scalar2=80.0, op0=ALU.mult, op1=ALU.min))
                    S.op(A, lambda: A.activation(out=ea[:], in_=ea[:], func=AF.Exp))
                    S.op(V, lambda: V.tensor_tensor(out=kd[:], in0=kk[:], in1=ea[:], op=ALU.mult))
                    for c0 in range(0, NCK, 16):
                        nck = min(16, NCK - c0)
                        S.grp(PE, [(lambda j=j: PE.transpose(out=pt[0:L, j * 128:(j + 1) * 128], in_=ke[:, (c0 + j) * L:(c0 + j + 1) * L], identity=ident_b[:]))
                                   for j in range(nck)])
                        S.op(V, lambda: V.tensor_copy(out=ket[:, c0:c0 + nck, :], in_=pt[0:L, 0:nck * 128].rearrange("p (n d) -> p n d", d=128)))
                    S.op(V, lambda: V.memset(Sf[:], 0.0))
                    S.op(V, lambda: V.memset(Sb[:], 0.0))
                    for ck in range(NCK):
                        cs = slice(ck * L, (ck + 1) * L)
                        S.op(PE, lambda: PE.matmul(pf[0:L, 0, 0:L], lhsT=kd[:, cs], rhs=qa[:, cs], start=True, stop=True))
                        S.op(V, lambda: V.tensor_tensor(out=ptt[:], in0=pf[0:L, 0, 0:L], in1=mask64[0:L, 0:L], op=ALU.mult))
                        S.grp(PE, [lambda: PE.matmul(pf[0:L, 1, 0:128], lhsT=ptt[:], rhs=vb[:, ck, :], start=True, stop=False),
                                   lambda: PE.matmul(pf[0:L, 1, 0:128], lhsT=qa[:, cs], rhs=Sb[:], start=False, stop=True)])
                        S.op(V, lambda: V.tensor_copy(out=ot[:], in_=pf[0:L, 1, 0:128]))
                        S.op(SY, lambda: SY.dma_start(out=PO[t0 + ck * L:t0 + (ck + 1) * L, h * 128:(h + 1) * 128], in_=ot[:]), True)
                        S.op(PE, lambda: PE.matmul(pf[:, 0, 128:256], lhsT=ket[:, ck, :], rhs=vb[:, ck, :], start=True, stop=True))
                        S.op(V, lambda: V.scalar_tensor_tensor(out=Sf[:], in0=Sf[:], scalar=el[:, ck:ck + 1], in1=pf[:, 0, 128:256], op0=ALU.mult, op1=ALU.add))
                        S.op(V, lambda: V.tensor_copy(out=Sb[:], in_=Sf[:]))
                    S.op(SY, lambda: SY.dma_start(out=new_hgrn_prompt[l, b, h, :, :], in_=Sf[:]), True)

    def stage_hgrn_sample(l):
        with ExitStack() as st:
            def t(name, shape, dt=F32):
                return st.enter_context(SBT(name, list(shape), dt))
            lbt = t("s_lb", [128, HG]); oml = t("s_oml", [128, HG])
            q = t("s_q", [128, HG, TS]); f = t("s_f", [128, HG, TS]); k = t("s_k", [128, HG, TS])
            so = t("s_so", [128, HG, 128]); sn = t("s_sn", [128, HG, 128]); vB = t("s_vB", [128, HG, 128]); orow = t("s_or", [1, 1024])
            load_lb(l, lbt, oml)
            for h in range(HG):
                S.op(SY, lambda h=h: SY.dma_start(out=q[:, h, :], in_=PA[24 + h, :, TP:TP + TS]), True)
                S.op(SY, lambda h=h: SY.dma_start(out=f[:, h, :], in_=PA[32 + h, :, TP:TP + TS]), True)
                S.op(A, lambda h=h: A.activation(out=f[:, h, :], in_=f[:, h, :], func=AF.Sigmoid))
                S.op(V, lambda h=h: V.tensor_scalar(out=f[:, h, :], in0=f[:, h, :], scalar1=oml[:, h:h + 1], scalar2=lbt[:, h:h + 1], op0=ALU.mult, op1=ALU.add))
                S.op(V, lambda h=h: V.tensor_scalar(out=k[:, h, :], in0=f[:, h, :], scalar1=-1.0, scalar2=1.0, op0=ALU.mult, op1=ALU.add))
                S.op(V, lambda h=h: V.tensor_scalar_max(out=f[:, h, :], in0=f[:, h, :], scalar1=F_MIN))
            for b in range(TS):
                S.op(SY, lambda: SY.dma_start(out=so[:], in_=state_hgrn[l, b].rearrange("h d v -> d h v")), True)
                S.op(SY, lambda: SY.dma_start(out=vB[:].rearrange("p h v -> p (h v)"), in_=row_bcast(PB[TP + b:TP + b + 1, 0:1024], 128)), True)
                for h in range(HG):
                    S.op(V, lambda h=h: V.tensor_scalar(out=vB[:, h, :], in0=vB[:, h, :], scalar1=k[:, h, b:b + 1], scalar2=None, op0=ALU.mult))
                    S.op(V, lambda h=h: V.scalar_tensor_tensor(out=sn[:, h, :], in0=so[:, h, :], scalar=f[:, h, b:b + 1], in1=vB[:, h, :], op0=ALU.mult, op1=ALU.add))
                S.grp(PE, [(lambda h=h: PE.matmul(pf[0:1, h // 4, (h % 4) * 128:(h % 4 + 1) * 128], lhsT=q[:, h, b:b + 1], rhs=sn[:, h, :], start=True, stop=True)) for h in range(HG)])
                S.op(V, lambda: V.tensor_copy(out=orow[:].rearrange("p (a c) -> p a c", a=2), in_=pf[0:1, :, :]))
                S.op(SY, lambda: SY.dma_start(out=PO[TP + b:TP + b + 1, :], in_=orow[:]), True)
                S.op(SY, lambda: SY.dma_start(out=new_hgrn_sample[l, b].rearrange("h d v -> d h v"), in_=sn[:]), True)

    def stage_hgrn_post(l):
        with ExitStack() as st:
            def t(name, shape, dt=F32):
                return st.enter_context(SBT(name, list(shape), dt))
            o = t("p_o", [128, 1024]); og = t("p_og", [128, 1024]); sq = t("p_sq", [128, 1024]); hn = t("p_hn", [128, 1024])
            ms = t("p_ms", [128, HG]); ob = t("p_ob", [128, 1024], BF16)
            S.op(SY, lambda: SY.dma_start(out=hn[:], in_=row_bcast(hgrn_norm[l:l + 1, :], 128)), True)
            for (t0, tn) in tiles(TP) + [(TP, TS)]:
                S.op(SY, lambda: SY.dma_start(out=o[:tn, :], in_=PO[t0:t0 + tn, :]), True)
                S.op(SY, lambda: SY.dma_start(out=og[:tn, :], in_=PB[t0:t0 + tn, 1024:2048]), True)
                S.op(V, lambda: V.tensor_tensor(out=sq[:tn, :], in0=o[:tn, :], in1=o[:tn, :], op=ALU.mult))
                S.op(V, lambda: V.reduce_sum(out=ms[:tn, :], in_=sq[:tn, :].rearrange("p (h v) -> p h v", v=128), axis=AX.X))
                S.op(V, lambda: V.tensor_scalar(out=ms[:tn, :], in0=ms[:tn, :], scalar1=1.0 / 128, scalar2=EPS, op0=ALU.mult, op1=ALU.add))
                S.op(A, lambda: A.sqrt(ms[:tn, :], ms[:tn, :]))
                S.op(V, lambda: V.reciprocal(out=ms[:tn, :], in_=ms[:tn, :]))
                S.op(V, lambda: V.tensor_tensor(out=o[:tn, :].rearrange("p (h v) -> p h v", v=128), in0=o[:tn, :].rearrange("p (h v) -> p h v", v=128),
                                                in1=ms[:tn, :].unsqueeze(2).to_broadcast([tn, HG, 128]), op=ALU.mult))
                S.op(V, lambda: V.tensor_tensor(out=o[:tn, :], in0=o[:tn, :], in1=hn[:tn, :], op=ALU.mult))
                S.op(A, lambda: A.activation(out=og[:tn, :], in_=og[:tn, :], func=AF.Silu))
                S.op(V, lambda: V.tensor_tensor(out=ob[:tn, :], in0=o[:tn, :], in1=og[:tn, :], op=ALU.mult))
                transpose_store(ob, tn, MIXT, t0, 8, c0=8)

    def stage_ffn(W1, W3, W2, dff, INT=None, Tn=None, tls=None, DSTB=None, odt=F32):
        KC = dff // 128
        INT = HT if INT is None else INT
        Tn = T if Tn is None else Tn
        tls = (tiles(TP) + [(TP, TS)]) if tls is None else tls
        gemm_a([W1, W3], INT, DC, dff, Tn, "swiglu", AT)
        gemm_b(W2, AT, KC, D, tls, FO if DSTB is None else DSTB, odt)

    stage_mod()
    for l in range(DEPTH):
        stage_normmod(l, 0)
        stage_win(l)
        stage_conv(l)
        stage_hgrn_prompt(l)
        stage_hgrn_sample(l)
        stage_hgrn_post(l)
        gemm_b(w_out[l], MIXT, DC, D, tiles(TP) + [(TP, TS)], FO)
        stage_epilogue(l, 0)
        if l % 2 == 0:
            stage_normmod(l, 1)
            stage_ffn(ffn_w1[l // 2], ffn_w3[l // 2], ffn_w2[l // 2], c.dff)
        else:
            stage_normmod(l, 1, router=True)
            stage_he_transpose()
            for e in range(NE):
                stage_ffn(moe_w1[l // 2, e], moe_w3[l // 2, e], moe_w2[l // 2, e], c.dffe, INT=HE[e], Tn=TE, tls=tiles(TE), DSTB=YE[e * TE:(e + 1) * TE, :], odt=BF16)
            stage_combine()
        stage_epilogue(l, 1, final=(l == DEPTH - 1))
    S.fin(SY)
    es.close()
    return nc


_NAMES = ["x_prompt", "x_sample", "state_conv", "state_hgrn", "c_prompt", "c_sample", "norm_pre", "norm_post", "w_mod", "b_mod",
          "w_in", "conv_w", "conv_norm", "lb_logits", "hgrn_norm", "w_out", "ffn_w1", "ffn_w3", "ffn_w2", "router_w", "router_b",
          "moe_w1", "moe_w3", "moe_w2"]
_OUTS = ["y_prompt", "y_sample", "new_conv_prompt", "new_hgrn_prompt", "new_conv_sample", "new_hgrn_sample"]


def kernel(**inputs):
    nseq, seq = inputs["x_prompt"].shape[0], inputs["x_prompt"].shape[1]
    cfg = Cfg(nseq=nseq, seq=seq, ts=inputs["x_sample"].shape[0], dff=inputs["ffn_w1"].shape[2], dffe=inputs["moe_w1"].shape[3])
    nc = build(cfg)
    in_map = {k: np.ascontiguousarray(np.asarray(inputs[k], dtype=np.float32)) for k in _NAMES}
    res = run_bass_kernel_spmd(nc, [in_map], core_ids=[0])
    r = res.results[0]
    return tuple(np.asarray(r[k], dtype=np.float32) for k in _OUTS)
```

```python
import numpy as np
import concourse.bass as bass
import concourse.mybir as mybir
from concourse.bass_utils import run_bass_kernel_spmd

F32, BF16 = mybir.dt.float32, mybir.dt.bfloat16
AF = mybir.ActivationFunctionType
ALU = mybir.AluOpType
AX = mybir.AxisListType

D = 2048
DC = D // 128
DEPTH = 2
CONV_DIM = 1024
HG = 8
NE = 8
EPS = 1e-6
F_MIN = 1e-6
LCH = 32
BIGIDX = 1 << 24


class Cfg:
    def __init__(s, nseq=4, seq=2048, ts=128, dff=5632, dffe=7168):
        s.nseq, s.seq, s.ts, s.dff, s.dffe = nseq, seq, ts, dff, dffe
        s.tp = nseq * seq
        s.T = s.tp + ts
        s.R = nseq + ts


def tiles(T, step=128):
    return [(i, min(step, T - i)) for i in range(0, T, step)]


class Seq:
    def __init__(s, nc, sem):
        s.nc, s.sem, s.val, s.sym, s.dry = nc, sem, 0, None, False

    def _w(s):
        return s.val if s.sym is None else s.sym + s.val

    def op(s, eng, fn, dma=False):
        inc = 16 if dma else 1
        if not s.dry:
            eng.wait_ge(s.sem, s._w())
            fn().then_inc(s.sem, inc)
        s.val += inc

    def grp(s, eng, fns):
        if not s.dry:
            eng.wait_ge(s.sem, s._w())
            ins = None
            for f in fns:
                ins = f()
            ins.then_inc(s.sem, 1)
        s.val += 1

    def fin(s, eng):
        eng.wait_ge(s.sem, s.val)


def build(cfg):
    nc = bass.Bass("TRN2", target_bir_lowering=False)
    c = cfg
    T, TP, TS, R, NSEQ, SEQ = c.T, c.tp, c.ts, c.R, c.nseq, c.seq

    def din(name, shape):
        return nc.dram_tensor(name, list(shape), F32, kind="ExternalInput").ap()

    def dout(name, shape):
        return nc.dram_tensor(name, list(shape), F32, kind="ExternalOutput").ap()

    def dscr(name, shape, dt=F32):
        return nc.dram_tensor(name, list(shape), dt, kind="Internal").ap()

    x_prompt = din("x_prompt", [NSEQ, SEQ, D]); x_sample = din("x_sample", [TS, 1, D])
    state_conv = din("state_conv", [DEPTH, TS, 2, CONV_DIM]); state_hgrn = din("state_hgrn", [DEPTH, TS, HG, 128, 128])
    c_prompt = din("c_prompt", [NSEQ, D]); c_sample = din("c_sample", [TS, D])
    norm_pre = din("norm_pre", [DEPTH, 2, D]); norm_post = din("norm_post", [DEPTH, 2, D])
    w_mod = din("w_mod", [DEPTH, D, 6 * D]); b_mod = din("b_mod", [DEPTH, 6 * D])
    w_in = din("w_in", [DEPTH, D, 7168]); conv_w = din("conv_w", [DEPTH, 3, CONV_DIM])
    conv_norm = din("conv_norm", [DEPTH, CONV_DIM]); lb_logits = din("lb_logits", [DEPTH, 1024])
    hgrn_norm = din("hgrn_norm", [DEPTH, 1024]); w_out = din("w_out", [DEPTH, D, D])
    ffn_w1 = din("ffn_w1", [1, D, c.dff]); ffn_w3 = din("ffn_w3", [1, D, c.dff]); ffn_w2 = din("ffn_w2", [1, c.dff, D])
    router_w = din("router_w", [1, D, NE]); router_b = din("router_b", [1, NE])
    moe_w1 = din("moe_w1", [1, NE, D, c.dffe]); moe_w3 = din("moe_w3", [1, NE, D, c.dffe]); moe_w2 = din("moe_w2", [1, NE, c.dffe, D])

    y_prompt = dout("y_prompt", [NSEQ, SEQ, D]); y_sample = dout("y_sample", [TS, 1, D])
    new_conv_prompt = dout("new_conv_prompt", [DEPTH, NSEQ, 2, CONV_DIM])
    new_hgrn_prompt = dout("new_hgrn_prompt", [DEPTH, NSEQ, HG, 128, 128])
    new_conv_sample = dout("new_conv_sample", [DEPTH, TS, 2, CONV_DIM])
    new_hgrn_sample = dout("new_hgrn_sample", [DEPTH, TS, HG, 128, 128])

    KCMAX = max(c.dff, c.dffe, D) // 128
    X = dscr("X", [T, D]); MOD = dscr("MOD", [DEPTH, R, 6 * D]); CFT = dscr("CFT", [DC, 128, R], BF16)
    HT = dscr("HT", [DC, 128, T], BF16); PA = dscr("PA", [40, 128, T]); PB = dscr("PB", [T, 2048])
    PO = dscr("PO", [T, 1024]); MIXT = dscr("MIXT", [DC, 128, T], BF16); FO = dscr("FO", [T, D])
    AT = dscr("AT", [KCMAX, 128, T], BF16); GT = dscr("GT", [T, NE])
    TE = min(T, ((int(1.72 * 2 * T / NE) + 511) // 512) * 512)
    HETM = dscr("HETM", [NE * TE, D], BF16); HE = dscr("HE", [NE, DC, 128, TE], BF16); YE = dscr("YE", [NE * TE, D], BF16)
    IDXD = dscr("IDXD", [T, 2], mybir.dt.int32); G2D = dscr("G2D", [T, 2])

    from contextlib import ExitStack
    es = ExitStack()
    sem = es.enter_context(nc.semaphore("g"))
    S = Seq(nc, sem)
    lsem = es.enter_context(nc.semaphore("lsem"))
    es.enter_context(nc.allow_non_contiguous_dma(reason="small strided tiles"))
    V, A, P, PE, SY = nc.vector, nc.scalar, nc.gpsimd, nc.tensor, nc.sync

    _cnt = [0]

    def SBT(name, shape, dt=F32):
        _cnt[0] += 1
        return nc.sbuf_tensor(f"{name}_{_cnt[0]}", list(shape), dt)

    def sb(name, shape, dt=F32):
        return es.enter_context(SBT(name, list(shape), dt))

    ident_b = sb("ident_b", [128, 128], BF16); ident_f = sb("ident_f", [128, 128])
    mask64 = sb("mask64", [128, 64]); bd64 = sb("bd64", [128, 128]); ones_t = sb("ones_t", [128, 128])
    scanm = sb("scanm", [128, SEQ]); epsb = sb("epsb", [128, 1])
    ps = es.enter_context(nc.psum_tensor("ps", [128, 4, 512], F32))
    pt = es.enter_context(nc.psum_tensor("pt", [128, 2048], BF16))
    pf = es.enter_context(nc.psum_tensor("pf", [128, 2, 512], F32))

    S.op(P, lambda: P.memset(ones_t[:], 1.0))
    S.op(P, lambda: P.memset(epsb[:], EPS))
    S.op(P, lambda: P.affine_select(out=ident_f[:], in_=ones_t[:], pattern=[[-1, 128]], compare_op=ALU.is_equal,
                                     fill=0.0, base=0, channel_multiplier=1))
    S.op(V, lambda: V.tensor_copy(out=ident_b[:], in_=ident_f[:]))
    S.op(P, lambda: P.affine_select(out=mask64[0:64, :], in_=ones_t[0:64, 0:64], pattern=[[1, 64]], compare_op=ALU.is_ge,
                                     fill=0.0, base=0, channel_multiplier=-1))
    S.op(P, lambda: P.memset(bd64[:], 0.0))
    S.op(P, lambda: P.memset(bd64[0:64, 0:64], 1.0 / 64))
    S.op(P, lambda: P.memset(bd64[64:128, 64:128], 1.0 / 64))
    S.op(P, lambda: P.memset(scanm[:], 1.0))
    S.op(P, lambda: P.memset(scanm[:].rearrange("p (n l) -> p n l", l=LCH)[:, :, 0:1], 0.0))

    tri_b = sb("tri_b", [128, 128], BF16); ones_b = sb("ones_b", [128, 128], BF16); tri_f = sb("tri_f", [128, 128])
    eoff_i = sb("eoff_i", [128, NE], mybir.dt.int32); eoff = sb("eoff", [128, NE])
    S.op(P, lambda: P.affine_select(out=tri_f[:], in_=ones_t[:], pattern=[[1, 128]], compare_op=ALU.is_gt, fill=0.0, base=0, channel_multiplier=-1))
    S.op(V, lambda: V.tensor_copy(out=tri_b[:], in_=tri_f[:]))
    S.op(V, lambda: V.tensor_copy(out=ones_b[:], in_=ones_t[:]))
    S.op(P, lambda: P.iota(eoff_i[:], pattern=[[TE, NE]], base=0, channel_multiplier=0))
    S.op(V, lambda: V.tensor_copy(out=eoff[:], in_=eoff_i[:]))

    bcreg = P.alloc_register("bcreg")
    P.reg_mov(bcreg, NE * TE - 1)

    S.op(SY, lambda: SY.dma_start(out=X[0:TP, :], in_=x_prompt.rearrange("b t d -> (b t) d")), True)
    S.op(SY, lambda: SY.dma_start(out=X[TP:T, :], in_=x_sample.rearrange("b t d -> (b t) d")), True)

    DQ = [SY]

    def row_bcast(ap_row, n=128):
        return ap_row.partition_broadcast(n)

    def transpose_store(src, tn, DST, t0, nchunks, c0=0):
        with SBT("tstage", [128, nchunks, 128], BF16) as stg:
            S.grp(PE, [(lambda k=k: PE.transpose(out=pt[:, k * 128:k * 128 + tn], in_=src[:tn, k * 128:(k + 1) * 128],
                                                  identity=ident_b[:tn, :tn])) for k in range(nchunks)])
            S.op(V, lambda: V.tensor_copy(out=stg[:, :, :tn], in_=pt[:, 0:nchunks * 128].rearrange("p (k t) -> p k t", t=128)[:, :, :tn]))
            S.op(SY, lambda: SY.dma_start(out=DST[c0:c0 + nchunks, :, t0:t0 + tn].rearrange("k p t -> p k t"), in_=stg[:, :, :tn]), True)

    def gemm_a_ser(Ws, INT, KC, N, Tn, evac, tgs=512):
        nW = len(Ws)
        cols = 512 // nW
        with SBT("ga_w", [128, nW, KC, cols], BF16) as wb, SBT("ga_in", [128, KC, tgs], BF16) as inb:
            for nb in range(0, N, cols):
                ncol = min(cols, N - nb)
                for wi, W in enumerate(Ws):
                    S.op(P, lambda wi=wi, W=W: P.dma_start(out=wb[:, wi, :, :ncol],
                                                           in_=(W(nb, ncol) if callable(W) else W[:, nb:nb + ncol]).rearrange("(k p) n -> p k n", p=128)), True)
                for (t0, tn) in tiles(Tn, tgs):
                    S.op(DQ[0], lambda: DQ[0].dma_start(out=inb[:, :, :tn], in_=INT[0:KC, :, t0:t0 + tn].rearrange("k p t -> p k t")), True)
                    for j in range(ncol // 128):
                        fns = []
                        for wi in range(nW):
                            for k in range(KC):
                                fns.append(lambda wi=wi, k=k: PE.matmul(ps[:, wi * 2 + (j % 2) if nW == 2 else j, :tn],
                                                                        lhsT=wb[:, wi, k, j * 128:(j + 1) * 128], rhs=inb[:, k, :tn],
                                                                        start=(k == 0), stop=(k == KC - 1)))
                        S.grp(PE, fns)
                        evac((nb // 128) + j, t0, tn, [ps[:, wi * 2 + (j % 2) if nW == 2 else j, :tn] for wi in range(nW)])

    def gemm_b_ser(W, INT, KC, N, tls, evac):
        with SBT("gb_w", [128, KC, 512], BF16) as wb, SBT("gb_in", [128, KC, 128], BF16) as inb:
            for nb in range(0, N, 512):
                ncol = min(512, N - nb)
                for k0 in range(0, KC, 16):
                    kn = min(16, KC - k0)
                    S.op(P, lambda: P.dma_start(out=wb[:, k0:k0 + kn, :ncol],
                                                in_=W[k0 * 128:(k0 + kn) * 128, nb:nb + ncol].rearrange("(k p) n -> p k n", p=128)), True)
                for (t0, tn) in tls:
                    S.op(DQ[0], lambda: DQ[0].dma_start(out=inb[:, :, :tn], in_=INT[0:KC, :, t0:t0 + tn].rearrange("k p t -> p k t")), True)
                    S.grp(PE, [(lambda k=k: PE.matmul(ps[:tn, 0, :ncol], lhsT=inb[:, k, :tn], rhs=wb[:, k, :ncol],
                                                      start=(k == 0), stop=(k == KC - 1))) for k in range(KC)])
                    evac(t0, tn, nb, ncol, ps[:tn, 0, :ncol])

    def PW(eng, sem_, val_):
        eng.wait_ge(sem_, val_)
        eng.nop(nofuse=True)

    psem = {k: es.enter_context(nc.semaphore("p_" + k)) for k in ("w", "in", "mm", "e1", "ev", "st")}
    pc = {k: 0 for k in psem}

    def pipe_enter():
        for eng in (P, SY, PE, V, A):
            PW(eng, S.sem, S.val)

    def pipe_exit():
        PW(V, psem["st"], pc["st"])
        PW(V, psem["ev"], pc["ev"])
        S.op(V, lambda: V.memset(epsb[:], EPS))

    def gemm_a(Ws, INT, KC, N, Tn, kind, DST, tgs=512):
        nW = len(Ws)
        cols = 512 // nW
        NB = 2
        odt = F32 if kind == "copy" else BF16
        with ExitStack() as st_:
            wb = [st_.enter_context(SBT("ga_w", [128, nW, KC, cols], BF16)) for _ in range(2)]
            inb = [st_.enter_context(SBT("ga_in", [128, KC, tgs], BF16)) for _ in range(2)]
            ob = [st_.enter_context(SBT("ga_o", [128, 512], odt)) for _ in range(2)]
            gb_ = [st_.enter_context(SBT("ga_g", [128, 512], F32)) for _ in range(2)] if kind == "swiglu" else None
            pipe_enter()
            blocks = [(nb, min(cols, N - nb)) for nb in range(0, N, cols)]
            tgl = tiles(Tn, tgs)
            mm_of_block_end = {}
            w0 = pc["w"]; in0 = pc["in"]; mm0 = pc["mm"]; ev0 = pc["ev"]; st0 = pc["st"]; e10 = pc["e1"]
            gi = 0
            li = 0
            mm_after_load = {}

            def issue_w(bi):
                nb, ncol = blocks[bi]
                if bi >= 2:
                    PW(P, psem["mm"], mm_of_block_end[bi - 2])
                for wi, W in enumerate(Ws):
                    P.dma_start(out=wb[bi % 2][:, wi, :, :ncol], in_=W[:, nb:nb + ncol].rearrange("(k p) n -> p k n", p=128)).then_inc(psem["w"], 16)
                    pc["w"] += 16

            def issue_in(l_i, t0, tn):
                if l_i >= 2:
                    PW(SY, psem["mm"], mm_after_load[l_i - 2])
                SY.dma_start(out=inb[l_i % 2][:, :, :tn], in_=INT[0:KC, :, t0:t0 + tn].rearrange("k p t -> p k t")).then_inc(psem["in"], 16)
                pc["in"] += 16

            seq_ = [(bi, ti) for bi in range(len(blocks)) for ti in range(len(tgl))]
            issue_w(0)
            issue_in(0, *tgl[0])
            for si, (bi, ti) in enumerate(seq_):
                nb, ncol = blocks[bi]
                t0, tn = tgl[ti]
                if ti == 0 and bi + 1 < len(blocks):
                    pass
                if si + 1 < len(seq_):
                    nbi, nti = seq_[si + 1]
                    if nbi != bi:
                        pass
                w_need = w0 + 16 * nW * (bi + 1)
                for j in range(ncol // 128):
                    st_i = gi % NB
                    PW(PE, psem["w"], w_need)
                    PW(PE, psem["in"], in0 + 16 * (li + 1))
                    if gi >= NB:
                        PW(PE, psem["ev"], ev0 + (gi - NB + 1))
                    ins = None
                    for wi in range(nW):
                        for k in range(KC):
                            ins = PE.matmul(ps[:, st_i * 2 + wi, :tn], lhsT=wb[bi % 2][:, wi, k, j * 128:(j + 1) * 128], rhs=inb[li % 2][:, k, :tn],
                                            start=(k == 0), stop=(k == KC - 1))
                    ins.then_inc(psem["mm"], 1); pc["mm"] += 1
                    o = ob[gi % 2]
                    if kind == "swiglu":
                        PW(A, psem["mm"], mm0 + gi + 1)
                        if gi >= 2:
                            PW(A, psem["ev"], ev0 + gi - 1)
                        A.activation(out=gb_[gi % 2][:, :tn], in_=ps[:, st_i * 2, :tn], func=AF.Silu).then_inc(psem["e1"], 1); pc["e1"] += 1
                        PW(V, psem["e1"], e10 + gi + 1)
                    else:
                        PW(V, psem["mm"], mm0 + gi + 1)
                    if gi >= 2:
                        PW(V, psem["st"], st0 + 16 * (gi - 1))
                    if kind == "swiglu":
                        V.tensor_tensor(out=o[:, :tn], in0=gb_[gi % 2][:, :tn], in1=ps[:, st_i * 2 + 1, :tn], op=ALU.mult).then_inc(psem["ev"], 1)
                    else:
                        V.tensor_copy(out=o[:, :tn], in_=ps[:, st_i * 2, :tn]).then_inc(psem["ev"], 1)
                    pc["ev"] += 1
                    PW(A, psem["ev"], ev0 + gi + 1)
                    A.dma_start(out=DST[(nb // 128) + j, :, t0:t0 + tn], in_=o[:, :tn]).then_inc(psem["st"], 16); pc["st"] += 16
                    gi += 1
                mm_after_load[li] = mm0 + gi
                if ti == len(tgl) - 1:
                    mm_of_block_end[bi] = mm0 + gi
                if si + 1 < len(seq_):
                    nbi, nti = seq_[si + 1]
                    if nbi != bi:
                        issue_w(nbi)
                    issue_in(li + 1, *tgl[nti])
                li += 1
            pipe_exit()

    def gemm_b(W, INT, KC, N, tls, DST, odt=F32):
        with ExitStack() as st_:
            wb = [st_.enter_context(SBT("gb_w", [128, KC, 512], BF16)) for _ in range(2 if KC <= 32 else 1)]
            inb = [st_.enter_context(SBT("gb_in", [128, KC, 128], BF16)) for _ in range(2)]
            ob = [st_.enter_context(SBT("gb_o", [128, 512], odt)) for _ in range(2)]
            nwb = len(wb)
            pipe_enter()
            blocks = [(nb, min(512, N - nb)) for nb in range(0, N, 512)]
            w0 = pc["w"]; in0 = pc["in"]; mm0 = pc["mm"]; ev0 = pc["ev"]; st0 = pc["st"]
            nkd = (KC + 15) // 16
            mm_of_block_end = {}
            seq_ = [(bi, ti) for bi in range(len(blocks)) for ti in range(len(tls))]

            def issue_w(bi):
                nb, ncol = blocks[bi]
                if bi >= nwb:
                    PW(P, psem["mm"], mm_of_block_end[bi - nwb])
                for k0 in range(0, KC, 16):
                    kn = min(16, KC - k0)
                    P.dma_start(out=wb[bi % nwb][:, k0:k0 + kn, :ncol],
                                in_=W[k0 * 128:(k0 + kn) * 128, nb:nb + ncol].rearrange("(k p) n -> p k n", p=128)).then_inc(psem["w"], 16)
                    pc["w"] += 16

            def issue_in(gi_, t0, tn):
                if gi_ >= 2:
                    PW(SY, psem["mm"], mm0 + gi_ - 1)
                SY.dma_start(out=inb[gi_ % 2][:, :, :tn], in_=INT[0:KC, :, t0:t0 + tn].rearrange("k p t -> p k t")).then_inc(psem["in"], 16)
                pc["in"] += 16

            issue_w(0)
            issue_in(0, *tls[0])
            for gi, (bi, ti) in enumerate(seq_):
                nb, ncol = blocks[bi]
                t0, tn = tls[ti]
                bank = gi % 4
                PW(PE, psem["w"], w0 + 16 * nkd * (bi + 1))
                PW(PE, psem["in"], in0 + 16 * (gi + 1))
                if gi >= 4:
                    PW(PE, psem["ev"], ev0 + gi - 3)
                ins = None
                for k in range(KC):
                    ins = PE.matmul(ps[:tn, bank, :ncol], lhsT=inb[gi % 2][:, k, :tn], rhs=wb[bi % nwb][:, k, :ncol], start=(k == 0), stop=(k == KC - 1))
                ins.then_inc(psem["mm"], 1); pc["mm"] += 1
                if ti == len(tls) - 1:
                    mm_of_block_end[bi] = mm0 + gi + 1
                PW(V, psem["mm"], mm0 + gi + 1)
                if gi >= 2:
                    PW(V, psem["st"], st0 + 16 * (gi - 1))
                V.tensor_copy(out=ob[gi % 2][:tn, :ncol], in_=ps[:tn, bank, :ncol]).then_inc(psem["ev"], 1); pc["ev"] += 1
                PW(A, psem["ev"], ev0 + gi + 1)
                A.dma_start(out=DST[t0:t0 + tn, nb:nb + ncol], in_=ob[gi % 2][:tn, :ncol]).then_inc(psem["st"], 16); pc["st"] += 16
                if gi + 1 < len(seq_):
                    nbi, nti = seq_[gi + 1]
                    if nbi != bi:
                        issue_w(nbi)
                    issue_in(gi + 1, *tls[nti])
            pipe_exit()

    def par(ops):
        w = S._w()
        for eng, fn in ops:
            eng.wait_ge(S.sem, w)
            fn().then_inc(S.sem, 1)
        S.val += len(ops)

    def store_b(DST):
        def ev(t0, tn, nb, ncol, p):
            with SBT("sb_ev", [128, 512], F32) as o:
                S.op(V, lambda: V.tensor_copy(out=o[:tn, :ncol], in_=p))
                S.op(SY, lambda: SY.dma_start(out=DST[t0:t0 + tn, nb:nb + ncol], in_=o[:tn, :ncol]), True)
        return ev

    def mod_rows(l, comp, t0, tn):
        if t0 < TP:
            b = t0 // SEQ
            return row_bcast(MOD[l, b:b + 1, comp * D:(comp + 1) * D], tn)
        r0 = NSEQ + (t0 - TP)
        return MOD[l, r0:r0 + tn, comp * D:(comp + 1) * D]

    def rstd_of(src, tn, out_rstd, junk):
        S.op(A, lambda: A.activation(out=junk[:tn, :], in_=src, func=AF.Square, accum_out=out_rstd[:tn, :]))
        S.op(V, lambda: V.tensor_scalar(out=out_rstd[:tn, :], in0=out_rstd[:tn, :], scalar1=1.0 / D, scalar2=EPS, op0=ALU.mult, op1=ALU.add))
        S.op(A, lambda: A.sqrt(out_rstd[:tn, :], out_rstd[:tn, :]))
        S.op(V, lambda: V.reciprocal(out=out_rstd[:tn, :], in_=out_rstd[:tn, :]))

    def stage_mod():
        with SBT("m_c", [128, D], F32) as ct, SBT("m_cb", [128, D], BF16) as cb:
            for (r0, rn, src) in [(0, NSEQ, c_prompt), (NSEQ, TS, c_sample)]:
                S.op(SY, lambda: SY.dma_start(out=ct[:rn, :], in_=src[:, :]), True)
                S.op(A, lambda: A.activation(out=cb[:rn, :], in_=ct[:rn, :], func=AF.Silu))
                transpose_store(cb, rn, CFT, r0, DC)
        for l in range(DEPTH):
            def ev(t0, tn, nb, ncol, p, l=l):
                with SBT("m_b", [128, 512], F32) as bt, SBT("m_o", [128, 512], F32) as o:
                    S.op(SY, lambda: SY.dma_start(out=bt[:tn, :ncol], in_=row_bcast(b_mod[l:l + 1, nb:nb + ncol], tn)), True)
                    S.op(V, lambda: V.tensor_tensor(out=o[:tn, :ncol], in0=p, in1=bt[:tn, :ncol], op=ALU.add))
                    S.op(SY, lambda: SY.dma_start(out=MOD[l, t0:t0 + tn, nb:nb + ncol], in_=o[:tn, :ncol]), True)
            gemm_b_ser(w_mod[l], CFT, DC, 6 * D, [(0, NSEQ), (NSEQ, TS)], ev)

    def stage_normmod(l, which, router=False):
        sc_c, sh_c = (1, 0) if which == 0 else (4, 3)
        with ExitStack() as st:
            def t(name, shape, dt=F32):
                return st.enter_context(SBT(name, list(shape), dt))
            xt = t("n_x", [128, D]); at = t("n_a", [128, D]); sh = t("n_sh", [128, D]); gb = t("n_g", [128, D])
            hf = t("n_hf", [128, D]); rs = t("n_rs", [128, 1]); hb = t("n_hb", [128, D], BF16)
            S.op(SY, lambda: SY.dma_start(out=gb[:], in_=row_bcast(norm_pre[l, which:which + 1, :], 128)), True)
            if router:
                rwb = t("n_rw", [128, NE, D]); rbb = t("n_rb", [128, NE]); lg = t("n_lg", [128, NE]); l2 = t("n_l2", [128, NE])
                m1 = t("n_m1", [128, 1]); m2 = t("n_m2", [128, 1]); k1 = t("n_k1", [128, NE]); k2 = t("n_k2", [128, NE])
                g12 = t("n_g12", [128, 2]); dd = t("n_dd", [128, 1])
                mf = t("n_mf", [128, NE]); mb = t("n_mb", [128, NE], BF16); pos = t("n_pos", [128, NE]); cnt = t("n_cnt", [128, NE])
                ovf = t("n_ovf", [128, NE]); idf = t("n_idf", [128, 2]); idi = t("n_idi", [128, 2], mybir.dt.int32)
                RWT = dscr("RWT", [NE, D])
                S.op(SY, lambda: SY.dma_start(out=RWT[:, :], in_=router_w[0, :, :].rearrange("d e -> e d")), True)
                for e in range(NE):
                    S.op(SY, lambda e=e: SY.dma_start(out=rwb[:, e, :], in_=row_bcast(RWT[e:e + 1, :], 128)), True)
                S.op(SY, lambda: SY.dma_start(out=rbb[:], in_=row_bcast(router_b[0:1, :], 128)), True)
                S.op(V, lambda: V.memset(cnt[:], 0.0))
            for (t0, tn) in tiles(TP) + [(TP, TS)]:
                S.op(SY, lambda: SY.dma_start(out=xt[:tn, :], in_=X[t0:t0 + tn, :]), True)
                S.op(SY, lambda: SY.dma_start(out=at[:tn, :], in_=mod_rows(l, sc_c, t0, tn)), True)
                S.op(SY, lambda: SY.dma_start(out=sh[:tn, :], in_=mod_rows(l, sh_c, t0, tn)), True)
                rstd_of(xt[:tn, :], tn, rs, hf)
                S.op(V, lambda: V.scalar_tensor_tensor(out=at[:tn, :], in0=at[:tn, :], scalar=1.0, in1=gb[:tn, :], op0=ALU.add, op1=ALU.mult))
                S.op(V, lambda: V.scalar_tensor_tensor(out=hf[:tn, :], in0=xt[:tn, :], scalar=rs[:tn, 0:1], in1=at[:tn, :], op0=ALU.mult, op1=ALU.mult))
                S.op(V, lambda: V.tensor_tensor(out=hf[:tn, :], in0=hf[:tn, :], in1=sh[:tn, :], op=ALU.add))
                S.op(A, lambda: A.copy(hb[:tn, :], hf[:tn, :]))
                if not router:
                    transpose_store(hb, tn, HT, t0, DC)
                    continue
                for e in range(NE):
                    S.op(V, lambda e=e: V.tensor_tensor(out=at[:tn, :], in0=hf[:tn, :], in1=rwb[:tn, e, :], op=ALU.mult))
                    S.op(V, lambda e=e: V.reduce_sum(out=lg[:tn, e:e + 1], in_=at[:tn, :], axis=AX.X))
                S.op(V, lambda: V.tensor_tensor(out=lg[:tn, :], in0=lg[:tn, :], in1=rbb[:tn, :], op=ALU.add))
                S.op(V, lambda: V.reduce_max(out=m1[:tn, :], in_=lg[:tn, :], axis=AX.X))
                S.op(V, lambda: V.tensor_scalar(out=k1[:tn, :], in0=lg[:tn, :], scalar1=m1[:tn, 0:1], scalar2=None, op0=ALU.is_equal))
                S.op(V, lambda: V.scalar_tensor_tensor(out=l2[:tn, :], in0=k1[:tn, :], scalar=-1e30, in1=lg[:tn, :], op0=ALU.mult, op1=ALU.add))
                S.op(V, lambda: V.reduce_max(out=m2[:tn, :], in_=l2[:tn, :], axis=AX.X))
                S.op(V, lambda: V.tensor_scalar(out=k2[:tn, :], in0=l2[:tn, :], scalar1=m2[:tn, 0:1], scalar2=None, op0=ALU.is_equal))
                S.op(V, lambda: V.tensor_tensor(out=dd[:tn, :], in0=m1[:tn, :], in1=m2[:tn, :], op=ALU.subtract))
                S.op(A, lambda: A.activation(out=g12[:tn, 0:1], in_=dd[:tn, :], func=AF.Sigmoid))
                S.op(A, lambda: A.activation(out=g12[:tn, 1:2], in_=dd[:tn, :], func=AF.Sigmoid, scale=-1.0))
                S.op(SY, lambda: SY.dma_start(out=G2D[t0:t0 + tn, :], in_=g12[:tn, :]), True)
                S.op(V, lambda: V.tensor_tensor(out=mf[:tn, :], in0=k1[:tn, :], in1=k2[:tn, :], op=ALU.add))
                S.op(V, lambda: V.tensor_copy(out=mb[:tn, :], in_=mf[:tn, :]))
                S.op(PE, lambda: PE.matmul(pf[:tn, 0, 0:NE], lhsT=tri_b[:tn, :tn], rhs=mb[:tn, :], start=True, stop=True))
                S.op(V, lambda: V.tensor_tensor(out=pos[:tn, :], in0=pf[:tn, 0, 0:NE], in1=cnt[:tn, :], op=ALU.add))
                S.op(PE, lambda: PE.matmul(pf[:, 1, 0:NE], lhsT=ones_b[:tn, :], rhs=mb[:tn, :], start=True, stop=True))
                S.op(V, lambda: V.tensor_tensor(out=cnt[:], in0=cnt[:], in1=pf[:, 1, 0:NE], op=ALU.add))
                S.op(V, lambda: V.tensor_scalar(out=ovf[:tn, :], in0=pos[:tn, :], scalar1=float(TE), scalar2=float(BIGIDX), op0=ALU.is_ge, op1=ALU.mult))
                S.op(V, lambda: V.tensor_tensor(out=pos[:tn, :], in0=pos[:tn, :], in1=eoff[:tn, :], op=ALU.add))
                S.op(V, lambda: V.tensor_tensor(out=pos[:tn, :], in0=pos[:tn, :], in1=ovf[:tn, :], op=ALU.add))
                S.op(V, lambda: V.tensor_tensor(out=k1[:tn, :], in0=k1[:tn, :], in1=pos[:tn, :], op=ALU.mult))
                S.op(V, lambda: V.reduce_sum(out=idf[:tn, 0:1], in_=k1[:tn, :], axis=AX.X))
                S.op(V, lambda: V.tensor_tensor(out=k2[:tn, :], in0=k2[:tn, :], in1=pos[:tn, :], op=ALU.mult))
                S.op(V, lambda: V.reduce_sum(out=idf[:tn, 1:2], in_=k2[:tn, :], axis=AX.X))
                S.op(V, lambda: V.tensor_copy(out=idi[:tn, :], in_=idf[:tn, :]))
                S.op(SY, lambda: SY.dma_start(out=IDXD[t0:t0 + tn, :], in_=idi[:tn, :]), True)
                for j in range(2):
                    S.op(P, lambda j=j: P.indirect_dma_start(out=HETM[:, :], out_offset=bass.IndirectOffsetOnAxis(ap=idi[:tn, j:j + 1], axis=0),
                                                             in_=hb[:tn, :], in_offset=None, bounds_check=bcreg, oob_is_err=False), True)

    def stage_he_transpose():
        with SBT("het", [128, D], BF16) as ht_:
            for e in range(NE):
                for (t0, tn) in tiles(TE):
                    S.op(SY, lambda: SY.dma_start(out=ht_[:tn, :], in_=HETM[e * TE + t0:e * TE + t0 + tn, :]), True)
                    transpose_store(ht_, tn, HE[e], t0, DC)

    def stage_combine():
        with ExitStack() as st:
            def t(name, shape, dt=F32):
                return st.enter_context(SBT(name, list(shape), dt))
            r1 = t("cb_r1", [128, D], BF16); r2 = t("cb_r2", [128, D], BF16); y = t("cb_y", [128, D]); g = t("cb_g", [128, 2]); idi = t("cb_i", [128, 2], mybir.dt.int32)
            for (t0, tn) in tiles(TP) + [(TP, TS)]:
                S.op(SY, lambda: SY.dma_start(out=g[:tn, :], in_=G2D[t0:t0 + tn, :]), True)
                S.op(SY, lambda: SY.dma_start(out=idi[:tn, :], in_=IDXD[t0:t0 + tn, :]), True)
                S.op(V, lambda: V.memset(r1[:], 0.0))
                S.op(V, lambda: V.memset(r2[:], 0.0))
                for j, r in ((0, r1), (1, r2)):
                    S.op(P, lambda: P.indirect_dma_start(out=r[:tn, :], out_offset=None, in_=YE[:, :],
                                                         in_offset=bass.IndirectOffsetOnAxis(ap=idi[:tn, j:j + 1], axis=0),
                                                         bounds_check=bcreg, oob_is_err=False), True)
                S.op(V, lambda: V.tensor_scalar(out=y[:tn, :], in0=r1[:tn, :], scalar1=g[:tn, 0:1], scalar2=None, op0=ALU.mult))
                S.op(V, lambda: V.scalar_tensor_tensor(out=y[:tn, :], in0=r2[:tn, :], scalar=g[:tn, 1:2], in1=y[:tn, :], op0=ALU.mult, op1=ALU.add))
                S.op(SY, lambda: SY.dma_start(out=FO[t0:t0 + tn, :], in_=y[:tn, :]), True)

    def stage_epilogue(l, which, final=False):
        ga_c = 2 if which == 0 else 5
        with ExitStack() as st:
            def t(name, shape, dt=F32):
                return st.enter_context(SBT(name, list(shape), dt))
            xt = t("e_x", [128, D]); ft = t("e_f", [128, D]); ga = t("e_ga", [128, D]); gb = t("e_g", [128, D]); jk = t("e_j", [128, D]); rs = t("e_rs", [128, 1])
            S.op(SY, lambda: SY.dma_start(out=gb[:], in_=row_bcast(norm_post[l, which:which + 1, :], 128)), True)
            for (t0, tn) in tiles(TP) + [(TP, TS)]:
                S.op(SY, lambda: SY.dma_start(out=xt[:tn, :], in_=X[t0:t0 + tn, :]), True)
                S.op(SY, lambda: SY.dma_start(out=ft[:tn, :], in_=FO[t0:t0 + tn, :]), True)
                S.op(SY, lambda: SY.dma_start(out=ga[:tn, :], in_=mod_rows(l, ga_c, t0, tn)), True)
                rstd_of(ft[:tn, :], tn, rs, jk)
                S.op(V, lambda: V.tensor_tensor(out=ga[:tn, :], in0=ga[:tn, :], in1=gb[:tn, :], op=ALU.mult))
                S.op(V, lambda: V.scalar_tensor_tensor(out=ft[:tn, :], in0=ft[:tn, :], scalar=rs[:tn, 0:1], in1=ga[:tn, :], op0=ALU.mult, op1=ALU.mult))
                S.op(V, lambda: V.tensor_tensor(out=xt[:tn, :], in0=xt[:tn, :], in1=ft[:tn, :], op=ALU.add))
                S.op(SY, lambda: SY.dma_start(out=X[t0:t0 + tn, :], in_=xt[:tn, :]), True)
                if final:
                    dst = y_prompt.rearrange("b t d -> (b t) d")[t0:t0 + tn, :] if t0 < TP else y_sample.rearrange("b t d -> (b t) d")[t0 - TP:t0 - TP + tn, :]
                    S.op(SY, lambda: SY.dma_start(out=dst, in_=xt[:tn, :]), True)

    def stage_win(l):
        gemm_a([w_in[l, :, 0:5120]], HT, DC, 5120, T, "copy", PA)
        gemm_b(w_in[l, :, 5120:7168], HT, DC, 2048, tiles(TP) + [(TP, TS)], PB)

    def load_percol(dst, src_row, nch):
        with nc.allow_non_contiguous_dma(reason="small per-channel vector"):
            S.op(SY, lambda: SY.dma_start(out=dst, in_=src_row.rearrange("o (c p) -> p (o c)", p=128)), True)

    def stage_conv(l):
        NCH = CONV_DIM // 128
        segs = [(b * SEQ, SEQ, b) for b in range(NSEQ)] + [(TP, TS, None)]
        LM = max(SEQ, TS)
        with ExitStack() as st:
            def t(name, shape, dt=F32):
                return st.enter_context(SBT(name, list(shape), dt))
            cw = t("c_w", [128, 3, NCH]); cn = t("c_n", [128, NCH])
            hc = t("c_hc", [128, LM]); bg = t("c_bg", [128, LM]); ext = t("c_ext", [128, LM + 2]); cv = t("c_cv", [128, LM])
            s0 = t("c_s0", [128, 128]); s1 = t("c_s1", [128, 128]); tm = t("c_tm", [128, 128]); sq = t("c_sq", [128, 512]); ob = t("c_ob", [128, 512], BF16)
            for j in range(3):
                load_percol(cw[:, j, :], conv_w[l, j:j + 1, :], NCH)
            load_percol(cn[:, :], conv_norm[l:l + 1, :], NCH)
            S.op(SY, lambda: SY.dma_start(out=new_conv_sample[l, :, 0, :], in_=state_conv[l, :, 1, :]), True)
            for (t0, L, b) in segs:
                for ch in range(NCH):
                    S.op(SY, lambda: SY.dma_start(out=hc[:, :L], in_=PA[ch, :, t0:t0 + L]), True)
                    S.op(SY, lambda: SY.dma_start(out=bg[:, :L], in_=PA[NCH + ch, :, t0:t0 + L]), True)
                    S.op(SY, lambda: SY.dma_start(out=ext[:, 2:L + 2], in_=PA[2 * NCH + ch, :, t0:t0 + L]), True)
                    S.op(V, lambda: V.tensor_tensor(out=ext[:, 2:L + 2], in0=ext[:, 2:L + 2], in1=hc[:, :L], op=ALU.mult))
                    if b is not None:
                        S.op(V, lambda: V.memset(ext[:, 0:2], 0.0))
                        a0, a1 = ext[:, 0:L], ext[:, 1:L + 1]
                    else:
                        for j, dstt in ((0, s0), (1, s1)):
                            S.op(SY, lambda: SY.dma_start(out=tm[:L, :], in_=state_conv[l, :, j, ch * 128:(ch + 1) * 128]), True)
                            S.op(PE, lambda: PE.transpose(out=pf[:, 0, :L], in_=tm[:L, :], identity=ident_f[:L, :L]))
                            S.op(V, lambda: V.tensor_copy(out=dstt[:, :L], in_=pf[:, 0, :L]))
                        a0, a1 = s0[:, :L], s1[:, :L]
                    S.op(V, lambda: V.tensor_scalar(out=cv[:, :L], in0=a0, scalar1=cw[:, 0, ch:ch + 1], scalar2=None, op0=ALU.mult))
                    S.op(V, lambda: V.scalar_tensor_tensor(out=cv[:, :L], in0=a1, scalar=cw[:, 1, ch:ch + 1], in1=cv[:, :L], op0=ALU.mult, op1=ALU.add))
                    S.op(V, lambda: V.scalar_tensor_tensor(out=cv[:, :L], in0=ext[:, 2:L + 2], scalar=cw[:, 2, ch:ch + 1], in1=cv[:, :L], op0=ALU.mult, op1=ALU.add))
                    S.op(V, lambda: V.tensor_tensor(out=cv[:, :L], in0=cv[:, :L], in1=bg[:, :L], op=ALU.mult))
                    for (q0, qn) in tiles(L, 512):
                        S.op(V, lambda: V.tensor_tensor(out=sq[:, :qn], in0=cv[:, q0:q0 + qn], in1=cv[:, q0:q0 + qn], op=ALU.mult))
                        S.op(PE, lambda: PE.matmul(pf[:, 0, :qn], lhsT=bd64[:], rhs=sq[:, :qn], start=True, stop=True))
                        S.op(A, lambda: A.activation(out=sq[:, :qn], in_=pf[:, 0, :qn], func=AF.Sqrt, bias=epsb[:, 0:1]))
                        S.op(V, lambda: V.reciprocal(out=sq[:, :qn], in_=sq[:, :qn]))
                        S.op(V, lambda: V.scalar_tensor_tensor(out=ob[:, :qn], in0=cv[:, q0:q0 + qn], scalar=cn[:, ch:ch + 1], in1=sq[:, :qn], op0=ALU.mult, op1=ALU.mult))
                        S.op(SY, lambda: SY.dma_start(out=MIXT[ch, :, t0 + q0:t0 + q0 + qn], in_=ob[:, :qn]), True)
                    if b is not None:
                        with nc.allow_non_contiguous_dma(reason="conv state tail (small)"):
                            S.op(SY, lambda: SY.dma_start(out=new_conv_prompt[l, b, :, ch * 128:(ch + 1) * 128].rearrange("j p -> p j"), in_=ext[:, L:L + 2]), True)
                    else:
                        S.op(PE, lambda: PE.transpose(out=pf[:L, 1, 0:128], in_=ext[:, 2:L + 2], identity=ident_f[:]))
                        S.op(V, lambda: V.tensor_copy(out=tm[:L, :], in_=pf[:L, 1, 0:128]))
                        S.op(SY, lambda: SY.dma_start(out=new_conv_sample[l, :, 1, ch * 128:(ch + 1) * 128], in_=tm[:L, :]), True)

    def load_lb(l, lbt, oml):
        if l == 0:
            S.op(V, lambda: V.memset(lbt[:], 0.0))
        else:
            with SBT("lb_a", [128, HG], F32) as a0, SBT("lb_b", [128, HG], F32) as a1:
                load_percol(a0[:, :], lb_logits[0:1, :], HG)
                load_percol(a1[:, :], lb_logits[1:2, :], HG)
                S.op(V, lambda: V.tensor_tensor(out=a1[:], in0=a1[:], in1=a0[:], op=ALU.subtract))
                S.op(A, lambda: A.activation(out=lbt[:], in_=a1[:], func=AF.Sigmoid))
        S.op(V, lambda: V.tensor_scalar(out=oml[:], in0=lbt[:], scalar1=-1.0, scalar2=1.0, op0=ALU.mult, op1=ALU.add))

    def stage_hgrn_prompt(l):
        L = LCH
        NCK = SEQ // L
        with ExitStack() as st:
            def t(name, shape, dt=F32):
                return st.enter_context(SBT(name, list(shape), dt))
            lbt = t("h_lb", [128, HG]); oml = t("h_oml", [128, HG])
            q = t("h_q", [128, SEQ]); z = t("h_z", [128, SEQ]); Aa = t("h_A", [128, SEQ]); kk = t("h_k", [128, SEQ]); ea = t("h_ea", [128, SEQ])
            qa = t("h_qa", [128, SEQ], BF16); kd = t("h_kd", [128, SEQ], BF16); ke = t("h_ke", [128, SEQ], BF16)
            al = t("h_al", [128, NCK]); el = t("h_el", [128, NCK]); ket = t("h_ket", [L, NCK, 128], BF16)
            vf = t("h_vf", [L, NCK, 128]); vb = t("h_vb", [L, NCK, 128], BF16)
            Sf = t("h_S", [128, 128]); Sb = t("h_Sb", [128, 128], BF16)
            GC = 4
            ptt4 = t("h_pt4", [L, GC, L], BF16); ot4 = t("h_o4", [L, GC, 128]); mask4 = t("h_m4", [L, GC, L])
            for j in range(GC):
                S.op(V, lambda j=j: V.tensor_copy(out=mask4[:, j, :], in_=mask64[0:L, 0:L]))
            load_lb(l, lbt, oml)
            for b in range(NSEQ):
                t0 = b * SEQ
                for h in range(HG):
                    S.op(SY, lambda: SY.dma_start(out=q[:], in_=PA[24 + h, :, t0:t0 + SEQ]), True)
                    S.op(SY, lambda: SY.dma_start(out=z[:], in_=PA[32 + h, :, t0:t0 + SEQ]), True)
                    S.op(SY, lambda: SY.dma_start(out=vf[:], in_=PB[t0:t0 + SEQ, h * 128:(h + 1) * 128].rearrange("(n p) v -> p n v", p=L)), True)
                    S.op(V, lambda: V.tensor_copy(out=vb[:], in_=vf[:]))
                    S.op(A, lambda: A.activation(out=z[:], in_=z[:], func=AF.Sigmoid))
                    S.op(V, lambda: V.tensor_scalar(out=z[:], in0=z[:], scalar1=oml[:, h:h + 1], scalar2=lbt[:, h:h + 1], op0=ALU.mult, op1=ALU.add))
                    S.op(V, lambda: V.tensor_scalar(out=kk[:], in0=z[:], scalar1=-1.0, scalar2=1.0, op0=ALU.mult, op1=ALU.add))
                    S.op(V, lambda: V.tensor_scalar_max(out=z[:], in0=z[:], scalar1=F_MIN))
                    S.op(A, lambda: A.activation(out=z[:], in_=z[:], func=AF.Ln))
                    S.op(V, lambda: V.tensor_tensor_scan(out=Aa[:], data0=scanm[:], data1=z[:], initial=0.0, op0=ALU.mult, op1=ALU.add))
                    S.op(V, lambda: V.tensor_copy(out=al[:], in_=Aa[:].rearrange("p (n l) -> p n l", l=L)[:, :, L - 1]))
                    S.op(A, lambda: A.activation(out=el[:], in_=al[:], func=AF.Exp))
                    S.op(A, lambda: A.activation(out=ea[:], in_=Aa[:], func=AF.Exp))
                    S.op(V, lambda: V.tensor_tensor(out=qa[:], in0=q[:], in1=ea[:], op=ALU.mult))
                    S.op(V, lambda: V.tensor_tensor(out=ea[:].rearrange("p (n l) -> p n l", l=L), in0=Aa[:].rearrange("p (n l) -> p n l", l=L),
                                                    in1=al[:].unsqueeze(2).to_broadcast([128, NCK, L]), op=ALU.subtract))
                    S.op(A, lambda: A.activation(out=ea[:], in_=ea[:], func=AF.Exp, scale=-1.0))
                    S.op(V, lambda: V.tensor_tensor(out=ke[:], in0=kk[:], in1=ea[:], op=ALU.mult))
                    S.op(V, lambda: V.tensor_scalar(out=ea[:], in0=Aa[:], scalar1=-1.0, scalar2=80.0, op0=ALU.mult, op1=ALU.min))
                    S.op(A, lambda: A.activation(out=ea[:], in_=ea[:], func=AF.Exp))
                    S.op(V, lambda: V.tensor_tensor(out=kd[:], in0=kk[:], in1=ea[:], op=ALU.mult))
                    for c0 in range(0, NCK, 16):
                        nck = min(16, NCK - c0)
                        S.grp(PE, [(lambda j=j: PE.transpose(out=pt[0:L, j * 128:(j + 1) * 128], in_=ke[:, (c0 + j) * L:(c0 + j + 1) * L], identity=ident_b[:]))
                                   for j in range(nck)])
                        S.op(V, lambda: V.tensor_copy(out=ket[:, c0:c0 + nck, :], in_=pt[0:L, 0:nck * 128].rearrange("p (n d) -> p n d", d=128)))
                    S.op(V, lambda: V.memset(Sf[:], 0.0))
                    S.op(V, lambda: V.memset(Sb[:], 0.0))
                    for c0 in range(0, NCK, GC):
                        fns = []
                        for j in range(GC):
                            cs = slice((c0 + j) * L, (c0 + j + 1) * L)
                            fns.append(lambda j=j, cs=cs: PE.matmul(ps[0:L, 0, j * 128:j * 128 + L], lhsT=kd[:, cs], rhs=qa[:, cs], start=True, stop=True))
                        for j in range(GC):
                            fns.append(lambda j=j, ck=c0 + j: PE.matmul(ps[:, 1, j * 128:(j + 1) * 128], lhsT=ket[:, ck, :], rhs=vb[:, ck, :], start=True, stop=True))
                        S.grp(PE, fns)
                        S.op(V, lambda: V.tensor_tensor(out=ptt4[:], in0=ps[0:L, 0, 0:GC * 128].rearrange("p (j l) -> p j l", l=128)[:, :, 0:L], in1=mask4[:], op=ALU.mult))
                        for j in range(GC):
                            ck = c0 + j
                            cs = slice(ck * L, (ck + 1) * L)
                            S.grp(PE, [lambda j=j, ck=ck: PE.matmul(pf[0:L, 1, 0:128], lhsT=ptt4[:, j, :], rhs=vb[:, ck, :], start=True, stop=False),
                                       lambda cs=cs: PE.matmul(pf[0:L, 1, 0:128], lhsT=qa[:, cs], rhs=Sb[:], start=False, stop=True)])
                            S.op(V, lambda j=j: V.tensor_copy(out=ot4[:, j, :], in_=pf[0:L, 1, 0:128]))
                            S.op(V, lambda j=j, ck=ck: V.scalar_tensor_tensor(out=Sf[:], in0=Sf[:], scalar=el[:, ck:ck + 1], in1=ps[:, 1, j * 128:(j + 1) * 128],
                                                                              op0=ALU.mult, op1=ALU.add))
                            S.op(V, lambda: V.tensor_copy(out=Sb[:], in_=Sf[:]))
                        S.op(SY, lambda c0=c0: SY.dma_start(out=PO[t0 + c0 * L:t0 + (c0 + GC) * L, h * 128:(h + 1) * 128].rearrange("(n p) v -> p n v", p=L), in_=ot4[:]), True)
                    S.op(SY, lambda: SY.dma_start(out=new_hgrn_prompt[l, b, h, :, :], in_=Sf[:]), True)

    def stage_hgrn_sample(l):
        with ExitStack() as st:
            def t(name, shape, dt=F32):
                return st.enter_context(SBT(name, list(shape), dt))
            lbt = t("s_lb", [128, HG]); oml = t("s_oml", [128, HG])
            q = t("s_q", [128, HG, TS]); f = t("s_f", [128, HG, TS]); k = t("s_k", [128, HG, TS])
            so = t("s_so", [128, HG, 128]); sn = t("s_sn", [128, HG, 128]); vB = t("s_vB", [128, HG, 128]); orow = t("s_or", [1, 1024])
            load_lb(l, lbt, oml)
            for h in range(HG):
                S.op(SY, lambda h=h: SY.dma_start(out=q[:, h, :], in_=PA[24 + h, :, TP:TP + TS]), True)
                S.op(SY, lambda h=h: SY.dma_start(out=f[:, h, :], in_=PA[32 + h, :, TP:TP + TS]), True)
                S.op(A, lambda h=h: A.activation(out=f[:, h, :], in_=f[:, h, :], func=AF.Sigmoid))
                S.op(V, lambda h=h: V.tensor_scalar(out=f[:, h, :], in0=f[:, h, :], scalar1=oml[:, h:h + 1], scalar2=lbt[:, h:h + 1], op0=ALU.mult, op1=ALU.add))
                S.op(V, lambda h=h: V.tensor_scalar(out=k[:, h, :], in0=f[:, h, :], scalar1=-1.0, scalar2=1.0, op0=ALU.mult, op1=ALU.add))
                S.op(V, lambda h=h: V.tensor_scalar_max(out=f[:, h, :], in0=f[:, h, :], scalar1=F_MIN))
            for b in range(TS):
                S.op(SY, lambda: SY.dma_start(out=so[:], in_=state_hgrn[l, b].rearrange("h d v -> d h v")), True)
                S.op(SY, lambda: SY.dma_start(out=vB[:].rearrange("p h v -> p (h v)"), in_=row_bcast(PB[TP + b:TP + b + 1, 0:1024], 128)), True)
                for h in range(HG):
                    S.op(V, lambda h=h: V.tensor_scalar(out=vB[:, h, :], in0=vB[:, h, :], scalar1=k[:, h, b:b + 1], scalar2=None, op0=ALU.mult))
                    S.op(V, lambda h=h: V.scalar_tensor_tensor(out=sn[:, h, :], in0=so[:, h, :], scalar=f[:, h, b:b + 1], in1=vB[:, h, :], op0=ALU.mult, op1=ALU.add))
                S.grp(PE, [(lambda h=h: PE.matmul(pf[0:1, h // 4, (h % 4) * 128:(h % 4 + 1) * 128], lhsT=q[:, h, b:b + 1], rhs=sn[:, h, :], start=True, stop=True)) for h in range(HG)])
                S.op(V, lambda: V.tensor_copy(out=orow[:].rearrange("p (a c) -> p a c", a=2), in_=pf[0:1, :, :]))
                S.op(SY, lambda: SY.dma_start(out=PO[TP + b:TP + b + 1, :], in_=orow[:]), True)
                S.op(SY, lambda: SY.dma_start(out=new_hgrn_sample[l, b].rearrange("h d v -> d h v"), in_=sn[:]), True)

    def stage_hgrn_post(l):
        with ExitStack() as st:
            def t(name, shape, dt=F32):
                return st.enter_context(SBT(name, list(shape), dt))
            o = t("p_o", [128, 1024]); og = t("p_og", [128, 1024]); sq = t("p_sq", [128, 1024]); hn = t("p_hn", [128, 1024])
            ms = t("p_ms", [128, HG]); ob = t("p_ob", [128, 1024], BF16)
            S.op(SY, lambda: SY.dma_start(out=hn[:], in_=row_bcast(hgrn_norm[l:l + 1, :], 128)), True)
            for (t0, tn) in tiles(TP) + [(TP, TS)]:
                S.op(SY, lambda: SY.dma_start(out=o[:tn, :], in_=PO[t0:t0 + tn, :]), True)
                S.op(SY, lambda: SY.dma_start(out=og[:tn, :], in_=PB[t0:t0 + tn, 1024:2048]), True)
                S.op(V, lambda: V.tensor_tensor(out=sq[:tn, :], in0=o[:tn, :], in1=o[:tn, :], op=ALU.mult))
                S.op(V, lambda: V.reduce_sum(out=ms[:tn, :], in_=sq[:tn, :].rearrange("p (h v) -> p h v", v=128), axis=AX.X))
                S.op(V, lambda: V.tensor_scalar(out=ms[:tn, :], in0=ms[:tn, :], scalar1=1.0 / 128, scalar2=EPS, op0=ALU.mult, op1=ALU.add))
                S.op(A, lambda: A.sqrt(ms[:tn, :], ms[:tn, :]))
                S.op(V, lambda: V.reciprocal(out=ms[:tn, :], in_=ms[:tn, :]))
                S.op(V, lambda: V.tensor_tensor(out=o[:tn, :].rearrange("p (h v) -> p h v", v=128), in0=o[:tn, :].rearrange("p (h v) -> p h v", v=128),
                                                in1=ms[:tn, :].unsqueeze(2).to_broadcast([tn, HG, 128]), op=ALU.mult))
                S.op(V, lambda: V.tensor_tensor(out=o[:tn, :], in0=o[:tn, :], in1=hn[:tn, :], op=ALU.mult))
                S.op(A, lambda: A.activation(out=og[:tn, :], in_=og[:tn, :], func=AF.Silu))
                S.op(V, lambda: V.tensor_tensor(out=ob[:tn, :], in0=o[:tn, :], in1=og[:tn, :], op=ALU.mult))
                transpose_store(ob, tn, MIXT, t0, 8, c0=8)

    def stage_ffn(W1, W3, W2, dff, INT=None, Tn=None, tls=None, DSTB=None, odt=F32):
        KC = dff // 128
        INT = HT if INT is None else INT
        Tn = T if Tn is None else Tn
        tls = (tiles(TP) + [(TP, TS)]) if tls is None else tls
        gemm_a([W1, W3], INT, DC, dff, Tn, "swiglu", AT)
        gemm_b(W2, AT, KC, D, tls, FO if DSTB is None else DSTB, odt)

    stage_mod()
    for l in range(DEPTH):
        stage_normmod(l, 0)
        stage_win(l)
        stage_conv(l)
        stage_hgrn_prompt(l)
        stage_hgrn_sample(l)
        stage_hgrn_post(l)
        gemm_b(w_out[l], MIXT, DC, D, tiles(TP) + [(TP, TS)], FO)
        stage_epilogue(l, 0)
        if l % 2 == 0:
            stage_normmod(l, 1)
            stage_ffn(ffn_w1[l // 2], ffn_w3[l // 2], ffn_w2[l // 2], c.dff)
        else:
            stage_normmod(l, 1, router=True)
            stage_he_transpose()
            for e in range(NE):
                stage_ffn(moe_w1[l // 2, e], moe_w3[l // 2, e], moe_w2[l // 2, e], c.dffe, INT=HE[e], Tn=TE, tls=tiles(TE), DSTB=YE[e * TE:(e + 1) * TE, :], odt=BF16)
            stage_combine()
        stage_epilogue(l, 1, final=(l == DEPTH - 1))
    S.fin(SY)
    es.close()
    return nc


_NAMES = ["x_prompt", "x_sample", "state_conv", "state_hgrn", "c_prompt", "c_sample", "norm_pre", "norm_post", "w_mod", "b_mod",
          "w_in", "conv_w", "conv_norm", "lb_logits", "hgrn_norm", "w_out", "ffn_w1", "ffn_w3", "ffn_w2", "router_w", "router_b",
          "moe_w1", "moe_w3", "moe_w2"]
_OUTS = ["y_prompt", "y_sample", "new_conv_prompt", "new_hgrn_prompt", "new_conv_sample", "new_hgrn_sample"]


def kernel(**inputs):
    nseq, seq = inputs["x_prompt"].shape[0], inputs["x_prompt"].shape[1]
    cfg = Cfg(nseq=nseq, seq=seq, ts=inputs["x_sample"].shape[0], dff=inputs["ffn_w1"].shape[2], dffe=inputs["moe_w1"].shape[3])
    nc = build(cfg)
    in_map = {k: np.ascontiguousarray(np.asarray(inputs[k], dtype=np.float32)) for k in _NAMES}
    res = run_bass_kernel_spmd(nc, [in_map], core_ids=[0])
    r = res.results[0]
    return tuple(np.asarray(r[k], dtype=np.float32) for k in _OUTS)
```

```python
import numpy as np
import concourse.bass as bass
import concourse.mybir as mybir
from concourse.bass_utils import run_bass_kernel_spmd

F32, BF16 = mybir.dt.float32, mybir.dt.bfloat16
AF = mybir.ActivationFunctionType
ALU = mybir.AluOpType
AX = mybir.AxisListType

D = 2048
DC = D // 128
DEPTH = 2
CONV_DIM = 1024
HG = 8
NE = 8
EPS = 1e-6
F_MIN = 1e-6
LCH = 32
BIGIDX = 1 << 24


class Cfg:
    def __init__(s, nseq=4, seq=2048, ts=128, dff=5632, dffe=7168):
        s.nseq, s.seq, s.ts, s.dff, s.dffe = nseq, seq, ts, dff, dffe
        s.tp = nseq * seq
        s.T = s.tp + ts
        s.R = nseq + ts


def tiles(T, step=128):
    return [(i, min(step, T - i)) for i in range(0, T, step)]


class Seq:
    def __init__(s, nc, sem):
        s.nc, s.sem, s.val, s.sym, s.dry = nc, sem, 0, None, False

    def _w(s):
        return s.val if s.sym is None else s.sym + s.val

    def op(s, eng, fn, dma=False):
        inc = 16 if dma else 1
        if not s.dry:
            eng.wait_ge(s.sem, s._w())
            fn().then_inc(s.sem, inc)
        s.val += inc

    def grp(s, eng, fns):
        if not s.dry:
            eng.wait_ge(s.sem, s._w())
            ins = None
            for f in fns:
                ins = f()
            ins.then_inc(s.sem, 1)
        s.val += 1

    def fin(s, eng):
        eng.wait_ge(s.sem, s.val)


def build(cfg):
    nc = bass.Bass("TRN2", target_bir_lowering=False)
    c = cfg
    T, TP, TS, R, NSEQ, SEQ = c.T, c.tp, c.ts, c.R, c.nseq, c.seq

    def din(name, shape):
        return nc.dram_tensor(name, list(shape), F32, kind="ExternalInput").ap()

    def dout(name, shape):
        return nc.dram_tensor(name, list(shape), F32, kind="ExternalOutput").ap()

    def dscr(name, shape, dt=F32):
        return nc.dram_tensor(name, list(shape), dt, kind="Internal").ap()

    x_prompt = din("x_prompt", [NSEQ, SEQ, D]); x_sample = din("x_sample", [TS, 1, D])
    state_conv = din("state_conv", [DEPTH, TS, 2, CONV_DIM]); state_hgrn = din("state_hgrn", [DEPTH, TS, HG, 128, 128])
    c_prompt = din("c_prompt", [NSEQ, D]); c_sample = din("c_sample", [TS, D])
    norm_pre = din("norm_pre", [DEPTH, 2, D]); norm_post = din("norm_post", [DEPTH, 2, D])
    w_mod = din("w_mod", [DEPTH, D, 6 * D]); b_mod = din("b_mod", [DEPTH, 6 * D])
    w_in = din("w_in", [DEPTH, D, 7168]); conv_w = din("conv_w", [DEPTH, 3, CONV_DIM])
    conv_norm = din("conv_norm", [DEPTH, CONV_DIM]); lb_logits = din("lb_logits", [DEPTH, 1024])
    hgrn_norm = din("hgrn_norm", [DEPTH, 1024]); w_out = din("w_out", [DEPTH, D, D])
    ffn_w1 = din("ffn_w1", [1, D, c.dff]); ffn_w3 = din("ffn_w3", [1, D, c.dff]); ffn_w2 = din("ffn_w2", [1, c.dff, D])
    router_w = din("router_w", [1, D, NE]); router_b = din("router_b", [1, NE])
    moe_w1 = din("moe_w1", [1, NE, D, c.dffe]); moe_w3 = din("moe_w3", [1, NE, D, c.dffe]); moe_w2 = din("moe_w2", [1, NE, c.dffe, D])

    y_prompt = dout("y_prompt", [NSEQ, SEQ, D]); y_sample = dout("y_sample", [TS, 1, D])
    new_conv_prompt = dout("new_conv_prompt", [DEPTH, NSEQ, 2, CONV_DIM])
    new_hgrn_prompt = dout("new_hgrn_prompt", [DEPTH, NSEQ, HG, 128, 128])
    new_conv_sample = dout("new_conv_sample", [DEPTH, TS, 2, CONV_DIM])
    new_hgrn_sample = dout("new_hgrn_sample", [DEPTH, TS, HG, 128, 128])

    KCMAX = max(c.dff, c.dffe, D) // 128
    X = dscr("X", [T, D]); MOD = dscr("MOD", [DEPTH, R, 6 * D]); CFT = dscr("CFT", [DC, 128, R], BF16)
    HT = dscr("HT", [DC, 128, T], BF16); PA = dscr("PA", [40, 128, T]); PB = dscr("PB", [T, 2048])
    PO = dscr("PO", [T, 1024]); MIXT = dscr("MIXT", [DC, 128, T], BF16); FO = dscr("FO", [T, D])
    AT = dscr("AT", [KCMAX, 128, T], BF16); GT = dscr("GT", [T, NE])
    TE = min(T, ((int(1.72 * 2 * T / NE) + 511) // 512) * 512)
    HETM = dscr("HETM", [NE * TE, D], BF16); HE = dscr("HE", [NE, DC, 128, TE], BF16); YE = dscr("YE", [NE * TE, D], BF16)
    IDXD = dscr("IDXD", [T, 2], mybir.dt.int32); G2D = dscr("G2D", [T, 2])

    from contextlib import ExitStack
    es = ExitStack()
    sem = es.enter_context(nc.semaphore("g"))
    S = Seq(nc, sem)
    lsem = es.enter_context(nc.semaphore("lsem"))
    es.enter_context(nc.allow_non_contiguous_dma(reason="small strided tiles"))
    V, A, P, PE, SY = nc.vector, nc.scalar, nc.gpsimd, nc.tensor, nc.sync

    _cnt = [0]

    def SBT(name, shape, dt=F32):
        _cnt[0] += 1
        return nc.sbuf_tensor(f"{name}_{_cnt[0]}", list(shape), dt)

    def sb(name, shape, dt=F32):
        return es.enter_context(SBT(name, list(shape), dt))

    ident_b = sb("ident_b", [128, 128], BF16); ident_f = sb("ident_f", [128, 128])
    mask64 = sb("mask64", [128, 64]); bd64 = sb("bd64", [128, 128]); ones_t = sb("ones_t", [128, 128])
    scanm = sb("scanm", [128, SEQ]); epsb = sb("epsb", [128, 1])
    ps = es.enter_context(nc.psum_tensor("ps", [128, 4, 512], F32))
    pt = es.enter_context(nc.psum_tensor("pt", [128, 2048], BF16))
    pf = es.enter_context(nc.psum_tensor("pf", [128, 2, 512], F32))

    S.op(P, lambda: P.memset(ones_t[:], 1.0))
    S.op(P, lambda: P.memset(epsb[:], EPS))
    S.op(P, lambda: P.affine_select(out=ident_f[:], in_=ones_t[:], pattern=[[-1, 128]], compare_op=ALU.is_equal,
                                     fill=0.0, base=0, channel_multiplier=1))
    S.op(V, lambda: V.tensor_copy(out=ident_b[:], in_=ident_f[:]))
    S.op(P, lambda: P.affine_select(out=mask64[0:64, :], in_=ones_t[0:64, 0:64], pattern=[[1, 64]], compare_op=ALU.is_ge,
                                     fill=0.0, base=0, channel_multiplier=-1))
    S.op(P, lambda: P.memset(bd64[:], 0.0))
    S.op(P, lambda: P.memset(bd64[0:64, 0:64], 1.0 / 64))
    S.op(P, lambda: P.memset(bd64[64:128, 64:128], 1.0 / 64))
    S.op(P, lambda: P.memset(scanm[:], 1.0))
    S.op(P, lambda: P.memset(scanm[:].rearrange("p (n l) -> p n l", l=LCH)[:, :, 0:1], 0.0))

    tri_b = sb("tri_b", [128, 128], BF16); ones_b = sb("ones_b", [128, 128], BF16); tri_f = sb("tri_f", [128, 128])
    eoff_i = sb("eoff_i", [128, NE], mybir.dt.int32); eoff = sb("eoff", [128, NE])
    S.op(P, lambda: P.affine_select(out=tri_f[:], in_=ones_t[:], pattern=[[1, 128]], compare_op=ALU.is_gt, fill=0.0, base=0, channel_multiplier=-1))
    S.op(V, lambda: V.tensor_copy(out=tri_b[:], in_=tri_f[:]))
    S.op(V, lambda: V.tensor_copy(out=ones_b[:], in_=ones_t[:]))
    S.op(P, lambda: P.iota(eoff_i[:], pattern=[[TE, NE]], base=0, channel_multiplier=0))
    S.op(V, lambda: V.tensor_copy(out=eoff[:], in_=eoff_i[:]))

    bcreg = P.alloc_register("bcreg")
    P.reg_mov(bcreg, NE * TE - 1)

    S.op(SY, lambda: SY.dma_start(out=X[0:TP, :], in_=x_prompt.rearrange("b t d -> (b t) d")), True)
    S.op(SY, lambda: SY.dma_start(out=X[TP:T, :], in_=x_sample.rearrange("b t d -> (b t) d")), True)

    DQ = [SY]

    def row_bcast(ap_row, n=128):
        return ap_row.partition_broadcast(n)

    def transpose_store(src, tn, DST, t0, nchunks, c0=0):
        with SBT("tstage", [128, nchunks, 128], BF16) as stg:
            S.grp(PE, [(lambda k=k: PE.transpose(out=pt[:, k * 128:k * 128 + tn], in_=src[:tn, k * 128:(k + 1) * 128],
                                                  identity=ident_b[:tn, :tn])) for k in range(nchunks)])
            S.op(V, lambda: V.tensor_copy(out=stg[:, :, :tn], in_=pt[:, 0:nchunks * 128].rearrange("p (k t) -> p k t", t=128)[:, :, :tn]))
            S.op(SY, lambda: SY.dma_start(out=DST[c0:c0 + nchunks, :, t0:t0 + tn].rearrange("k p t -> p k t"), in_=stg[:, :, :tn]), True)

    def gemm_a_ser(Ws, INT, KC, N, Tn, evac, tgs=512):
        nW = len(Ws)
        cols = 512 // nW
        with SBT("ga_w", [128, nW, KC, cols], BF16) as wb, SBT("ga_in", [128, KC, tgs], BF16) as inb:
            for nb in range(0, N, cols):
                ncol = min(cols, N - nb)
                for wi, W in enumerate(Ws):
                    S.op(P, lambda wi=wi, W=W: P.dma_start(out=wb[:, wi, :, :ncol],
                                                           in_=(W(nb, ncol) if callable(W) else W[:, nb:nb + ncol]).rearrange("(k p) n -> p k n", p=128)), True)
                for (t0, tn) in tiles(Tn, tgs):
                    S.op(DQ[0], lambda: DQ[0].dma_start(out=inb[:, :, :tn], in_=INT[0:KC, :, t0:t0 + tn].rearrange("k p t -> p k t")), True)
                    for j in range(ncol // 128):
                        fns = []
                        for wi in range(nW):
                            for k in range(KC):
                                fns.append(lambda wi=wi, k=k: PE.matmul(ps[:, wi * 2 + (j % 2) if nW == 2 else j, :tn],
                                                                        lhsT=wb[:, wi, k, j * 128:(j + 1) * 128], rhs=inb[:, k, :tn],
                                                                        start=(k == 0), stop=(k == KC - 1)))
                        S.grp(PE, fns)
                        evac((nb // 128) + j, t0, tn, [ps[:, wi * 2 + (j % 2) if nW == 2 else j, :tn] for wi in range(nW)])

    def gemm_b_ser(W, INT, KC, N, tls, evac):
        with SBT("gb_w", [128, KC, 512], BF16) as wb, SBT("gb_in", [128, KC, 128], BF16) as inb:
            for nb in range(0, N, 512):
                ncol = min(512, N - nb)
                for k0 in range(0, KC, 16):
                    kn = min(16, KC - k0)
                    S.op(P, lambda: P.dma_start(out=wb[:, k0:k0 + kn, :ncol],
                                                in_=W[k0 * 128:(k0 + kn) * 128, nb:nb + ncol].rearrange("(k p) n -> p k n", p=128)), True)
                for (t0, tn) in tls:
                    S.op(DQ[0], lambda: DQ[0].dma_start(out=inb[:, :, :tn], in_=INT[0:KC, :, t0:t0 + tn].rearrange("k p t -> p k t")), True)
                    S.grp(PE, [(lambda k=k: PE.matmul(ps[:tn, 0, :ncol], lhsT=inb[:, k, :tn], rhs=wb[:, k, :ncol],
                                                      start=(k == 0), stop=(k == KC - 1))) for k in range(KC)])
                    evac(t0, tn, nb, ncol, ps[:tn, 0, :ncol])

    def PW(eng, sem_, val_):
        eng.wait_ge(sem_, val_)
        eng.nop(nofuse=True)

    psem = {k: es.enter_context(nc.semaphore("p_" + k)) for k in ("w", "in", "mm", "e1", "ev", "st")}
    pc = {k: 0 for k in psem}

    def pipe_enter():
        for eng in (P, SY, PE, V, A):
            PW(eng, S.sem, S.val)

    def pipe_exit():
        PW(V, psem["st"], pc["st"])
        PW(V, psem["ev"], pc["ev"])
        S.op(V, lambda: V.memset(epsb[:], EPS))

    def gemm_a(Ws, INT, KC, N, Tn, kind, DST, tgs=512):
        nW = len(Ws)
        cols = 512 // nW
        NB = 2
        odt = F32 if kind == "copy" else BF16
        with ExitStack() as st_:
            wb = [st_.enter_context(SBT("ga_w", [128, nW, KC, cols], BF16)) for _ in range(2)]
            inb = [st_.enter_context(SBT("ga_in", [128, KC, tgs], BF16)) for _ in range(2)]
            ob = [st_.enter_context(SBT("ga_o", [128, 512], odt)) for _ in range(2)]
            gb_ = [st_.enter_context(SBT("ga_g", [128, 512], F32)) for _ in range(2)] if kind == "swiglu" else None
            pipe_enter()
            blocks = [(nb, min(cols, N - nb)) for nb in range(0, N, cols)]
            tgl = tiles(Tn, tgs)
            mm_of_block_end = {}
            w0 = pc["w"]; in0 = pc["in"]; mm0 = pc["mm"]; ev0 = pc["ev"]; st0 = pc["st"]; e10 = pc["e1"]
            gi = 0
            li = 0
            mm_after_load = {}

            def issue_w(bi):
                nb, ncol = blocks[bi]
                if bi >= 2:
                    PW(P, psem["mm"], mm_of_block_end[bi - 2])
                for wi, W in enumerate(Ws):
                    P.dma_start(out=wb[bi % 2][:, wi, :, :ncol], in_=W[:, nb:nb + ncol].rearrange("(k p) n -> p k n", p=128)).then_inc(psem["w"], 16)
                    pc["w"] += 16

            def issue_in(l_i, t0, tn):
                if l_i >= 2:
                    PW(SY, psem["mm"], mm_after_load[l_i - 2])
                SY.dma_start(out=inb[l_i % 2][:, :, :tn], in_=INT[0:KC, :, t0:t0 + tn].rearrange("k p t -> p k t")).then_inc(psem["in"], 16)
                pc["in"] += 16

            seq_ = [(bi, ti) for bi in range(len(blocks)) for ti in range(len(tgl))]
            issue_w(0)
            issue_in(0, *tgl[0])
            for si, (bi, ti) in enumerate(seq_):
                nb, ncol = blocks[bi]
                t0, tn = tgl[ti]
                if ti == 0 and bi + 1 < len(blocks):
                    pass
                if si + 1 < len(seq_):
                    nbi, nti = seq_[si + 1]
                    if nbi != bi:
                        pass
                w_need = w0 + 16 * nW * (bi + 1)
                for j in range(ncol // 128):
                    st_i = gi % NB
                    PW(PE, psem["w"], w_need)
                    PW(PE, psem["in"], in0 + 16 * (li + 1))
                    if gi >= NB:
                        PW(PE, psem["ev"], ev0 + (gi - NB + 1))
                    ins = None
                    for wi in range(nW):
                        for k in range(KC):
                            ins = PE.matmul(ps[:, st_i * 2 + wi, :tn], lhsT=wb[bi % 2][:, wi, k, j * 128:(j + 1) * 128], rhs=inb[li % 2][:, k, :tn],
                                            start=(k == 0), stop=(k == KC - 1))
                    ins.then_inc(psem["mm"], 1); pc["mm"] += 1
                    o = ob[gi % 2]
                    if kind == "swiglu":
                        PW(A, psem["mm"], mm0 + gi + 1)
                        if gi >= 2:
                            PW(A, psem["ev"], ev0 + gi - 1)
                        A.activation(out=gb_[gi % 2][:, :tn], in_=ps[:, st_i * 2, :tn], func=AF.Silu).then_inc(psem["e1"], 1); pc["e1"] += 1
                        PW(V, psem["e1"], e10 + gi + 1)
                    else:
                        PW(V, psem["mm"], mm0 + gi + 1)
                    if gi >= 2:
                        PW(V, psem["st"], st0 + 16 * (gi - 1))
                    if kind == "swiglu":
                        V.tensor_tensor(out=o[:, :tn], in0=gb_[gi % 2][:, :tn], in1=ps[:, st_i * 2 + 1, :tn], op=ALU.mult).then_inc(psem["ev"], 1)
                    else:
                        V.tensor_copy(out=o[:, :tn], in_=ps[:, st_i * 2, :tn]).then_inc(psem["ev"], 1)
                    pc["ev"] += 1
                    PW(A, psem["ev"], ev0 + gi + 1)
                    A.dma_start(out=DST[(nb // 128) + j, :, t0:t0 + tn], in_=o[:, :tn]).then_inc(psem["st"], 16); pc["st"] += 16
                    gi += 1
                mm_after_load[li] = mm0 + gi
                if ti == len(tgl) - 1:
                    mm_of_block_end[bi] = mm0 + gi
                if si + 1 < len(seq_):
                    nbi, nti = seq_[si + 1]
                    if nbi != bi:
                        issue_w(nbi)
                    issue_in(li + 1, *tgl[nti])
                li += 1
            pipe_exit()

    def gemm_b(W, INT, KC, N, tls, DST, odt=F32):
        with ExitStack() as st_:
            wb = [st_.enter_context(SBT("gb_w", [128, KC, 512], BF16)) for _ in range(2 if KC <= 32 else 1)]
            inb = [st_.enter_context(SBT("gb_in", [128, KC, 128], BF16)) for _ in range(2)]
            ob = [st_.enter_context(SBT("gb_o", [128, 512], odt)) for _ in range(2)]
            nwb = len(wb)
            pipe_enter()
            blocks = [(nb, min(512, N - nb)) for nb in range(0, N, 512)]
            w0 = pc["w"]; in0 = pc["in"]; mm0 = pc["mm"]; ev0 = pc["ev"]; st0 = pc["st"]
            nkd = (KC + 15) // 16
            mm_of_block_end = {}
            seq_ = [(bi, ti) for bi in range(len(blocks)) for ti in range(len(tls))]

            def issue_w(bi):
                nb, ncol = blocks[bi]
                if bi >= nwb:
                    PW(P, psem["mm"], mm_of_block_end[bi - nwb])
                for k0 in range(0, KC, 16):
                    kn = min(16, KC - k0)
                    P.dma_start(out=wb[bi % nwb][:, k0:k0 + kn, :ncol],
                                in_=W[k0 * 128:(k0 + kn) * 128, nb:nb + ncol].rearrange("(k p) n -> p k n", p=128)).then_inc(psem["w"], 16)
                    pc["w"] += 16

            def issue_in(gi_, t0, tn):
                if gi_ >= 2:
                    PW(SY, psem["mm"], mm0 + gi_ - 1)
                SY.dma_start(out=inb[gi_ % 2][:, :, :tn], in_=INT[0:KC, :, t0:t0 + tn].rearrange("k p t -> p k t")).then_inc(psem["in"], 16)
                pc["in"] += 16

            issue_w(0)
            issue_in(0, *tls[0])
            for gi, (bi, ti) in enumerate(seq_):
                nb, ncol = blocks[bi]
                t0, tn = tls[ti]
                bank = gi % 4
                PW(PE, psem["w"], w0 + 16 * nkd * (bi + 1))
                PW(PE, psem["in"], in0 + 16 * (gi + 1))
                if gi >= 4:
                    PW(PE, psem["ev"], ev0 + gi - 3)
                ins = None
                for k in range(KC):
                    ins = PE.matmul(ps[:tn, bank, :ncol], lhsT=inb[gi % 2][:, k, :tn], rhs=wb[bi % nwb][:, k, :ncol], start=(k == 0), stop=(k == KC - 1))
                ins.then_inc(psem["mm"], 1); pc["mm"] += 1
                if ti == len(tls) - 1:
                    mm_of_block_end[bi] = mm0 + gi + 1
                PW(V, psem["mm"], mm0 + gi + 1)
                if gi >= 2:
                    PW(V, psem["st"], st0 + 16 * (gi - 1))
                V.tensor_copy(out=ob[gi % 2][:tn, :ncol], in_=ps[:tn, bank, :ncol]).then_inc(psem["ev"], 1); pc["ev"] += 1
                PW(A, psem["ev"], ev0 + gi + 1)
                A.dma_start(out=DST[t0:t0 + tn, nb:nb + ncol], in_=ob[gi % 2][:tn, :ncol]).then_inc(psem["st"], 16); pc["st"] += 16
                if gi + 1 < len(seq_):
                    nbi, nti = seq_[gi + 1]
                    if nbi != bi:
                        issue_w(nbi)
                    issue_in(gi + 1, *tls[nti])
            pipe_exit()

    def par(ops):
        w = S._w()
        for eng, fn in ops:
            eng.wait_ge(S.sem, w)
            fn().then_inc(S.sem, 1)
        S.val += len(ops)

    def store_b(DST):
        def ev(t0, tn, nb, ncol, p):
            with SBT("sb_ev", [128, 512], F32) as o:
                S.op(V, lambda: V.tensor_copy(out=o[:tn, :ncol], in_=p))
                S.op(SY, lambda: SY.dma_start(out=DST[t0:t0 + tn, nb:nb + ncol], in_=o[:tn, :ncol]), True)
        return ev

    def mod_rows(l, comp, t0, tn):
        if t0 < TP:
            b = t0 // SEQ
            return row_bcast(MOD[l, b:b + 1, comp * D:(comp + 1) * D], tn)
        r0 = NSEQ + (t0 - TP)
        return MOD[l, r0:r0 + tn, comp * D:(comp + 1) * D]

    def rstd_of(src, tn, out_rstd, junk):
        S.op(A, lambda: A.activation(out=junk[:tn, :], in_=src, func=AF.Square, accum_out=out_rstd[:tn, :]))
        S.op(V, lambda: V.tensor_scalar(out=out_rstd[:tn, :], in0=out_rstd[:tn, :], scalar1=1.0 / D, scalar2=EPS, op0=ALU.mult, op1=ALU.add))
        S.op(A, lambda: A.sqrt(out_rstd[:tn, :], out_rstd[:tn, :]))
        S.op(V, lambda: V.reciprocal(out=out_rstd[:tn, :], in_=out_rstd[:tn, :]))

    def stage_mod():
        with SBT("m_c", [128, D], F32) as ct, SBT("m_cb", [128, D], BF16) as cb:
            for (r0, rn, src) in [(0, NSEQ, c_prompt), (NSEQ, TS, c_sample)]:
                S.op(SY, lambda: SY.dma_start(out=ct[:rn, :], in_=src[:, :]), True)
                S.op(A, lambda: A.activation(out=cb[:rn, :], in_=ct[:rn, :], func=AF.Silu))
                transpose_store(cb, rn, CFT, r0, DC)
        for l in range(DEPTH):
            def ev(t0, tn, nb, ncol, p, l=l):
                with SBT("m_b", [128, 512], F32) as bt, SBT("m_o", [128, 512], F32) as o:
                    S.op(SY, lambda: SY.dma_start(out=bt[:tn, :ncol], in_=row_bcast(b_mod[l:l + 1, nb:nb + ncol], tn)), True)
                    S.op(V, lambda: V.tensor_tensor(out=o[:tn, :ncol], in0=p, in1=bt[:tn, :ncol], op=ALU.add))
                    S.op(SY, lambda: SY.dma_start(out=MOD[l, t0:t0 + tn, nb:nb + ncol], in_=o[:tn, :ncol]), True)
            gemm_b_ser(w_mod[l], CFT, DC, 6 * D, [(0, NSEQ), (NSEQ, TS)], ev)

    def stage_normmod(l, which, router=False):
        sc_c, sh_c = (1, 0) if which == 0 else (4, 3)
        with ExitStack() as st:
            def t(name, shape, dt=F32):
                return st.enter_context(SBT(name, list(shape), dt))
            xt = t("n_x", [128, D]); at = t("n_a", [128, D]); sh = t("n_sh", [128, D]); gb = t("n_g", [128, D])
            hf = t("n_hf", [128, D]); rs = t("n_rs", [128, 1]); hb = t("n_hb", [128, D], BF16)
            S.op(SY, lambda: SY.dma_start(out=gb[:], in_=row_bcast(norm_pre[l, which:which + 1, :], 128)), True)
            if router:
                rwb = t("n_rw", [128, NE, D]); rbb = t("n_rb", [128, NE]); lg = t("n_lg", [128, NE]); l2 = t("n_l2", [128, NE])
                m1 = t("n_m1", [128, 1]); m2 = t("n_m2", [128, 1]); k1 = t("n_k1", [128, NE]); k2 = t("n_k2", [128, NE])
                g12 = t("n_g12", [128, 2]); dd = t("n_dd", [128, 1])
                mf = t("n_mf", [128, NE]); mb = t("n_mb", [128, NE], BF16); pos = t("n_pos", [128, NE]); cnt = t("n_cnt", [128, NE])
                ovf = t("n_ovf", [128, NE]); idf = t("n_idf", [128, 2]); idi = t("n_idi", [128, 2], mybir.dt.int32)
                RWT = dscr("RWT", [NE, D])
                S.op(SY, lambda: SY.dma_start(out=RWT[:, :], in_=router_w[0, :, :].rearrange("d e -> e d")), True)
                for e in range(NE):
                    S.op(SY, lambda e=e: SY.dma_start(out=rwb[:, e, :], in_=row_bcast(RWT[e:e + 1, :], 128)), True)
                S.op(SY, lambda: SY.dma_start(out=rbb[:], in_=row_bcast(router_b[0:1, :], 128)), True)
                S.op(V, lambda: V.memset(cnt[:], 0.0))
            for (t0, tn) in tiles(TP) + [(TP, TS)]:
                S.op(SY, lambda: SY.dma_start(out=xt[:tn, :], in_=X[t0:t0 + tn, :]), True)
                S.op(SY, lambda: SY.dma_start(out=at[:tn, :], in_=mod_rows(l, sc_c, t0, tn)), True)
                S.op(SY, lambda: SY.dma_start(out=sh[:tn, :], in_=mod_rows(l, sh_c, t0, tn)), True)
                rstd_of(xt[:tn, :], tn, rs, hf)
                S.op(V, lambda: V.scalar_tensor_tensor(out=at[:tn, :], in0=at[:tn, :], scalar=1.0, in1=gb[:tn, :], op0=ALU.add, op1=ALU.mult))
                S.op(V, lambda: V.scalar_tensor_tensor(out=hf[:tn, :], in0=xt[:tn, :], scalar=rs[:tn, 0:1], in1=at[:tn, :], op0=ALU.mult, op1=ALU.mult))
                S.op(V, lambda: V.tensor_tensor(out=hf[:tn, :], in0=hf[:tn, :], in1=sh[:tn, :], op=ALU.add))
                S.op(A, lambda: A.copy(hb[:tn, :], hf[:tn, :]))
                if not router:
                    transpose_store(hb, tn, HT, t0, DC)
                    continue
                for e in range(NE):
                    S.op(V, lambda e=e: V.tensor_tensor(out=at[:tn, :], in0=hf[:tn, :], in1=rwb[:tn, e, :], op=ALU.mult))
                    S.op(V, lambda e=e: V.reduce_sum(out=lg[:tn, e:e + 1], in_=at[:tn, :], axis=AX.X))
                S.op(V, lambda: V.tensor_tensor(out=lg[:tn, :], in0=lg[:tn, :], in1=rbb[:tn, :], op=ALU.add))
                S.op(V, lambda: V.reduce_max(out=m1[:tn, :], in_=lg[:tn, :], axis=AX.X))
                S.op(V, lambda: V.tensor_scalar(out=k1[:tn, :], in0=lg[:tn, :], scalar1=m1[:tn, 0:1], scalar2=None, op0=ALU.is_equal))
                S.op(V, lambda: V.scalar_tensor_tensor(out=l2[:tn, :], in0=k1[:tn, :], scalar=-1e30, in1=lg[:tn, :], op0=ALU.mult, op1=ALU.add))
                S.op(V, lambda: V.reduce_max(out=m2[:tn, :], in_=l2[:tn, :], axis=AX.X))
                S.op(V, lambda: V.tensor_scalar(out=k2[:tn, :], in0=l2[:tn, :], scalar1=m2[:tn, 0:1], scalar2=None, op0=ALU.is_equal))
                S.op(V, lambda: V.tensor_tensor(out=dd[:tn, :], in0=m1[:tn, :], in1=m2[:tn, :], op=ALU.subtract))
                S.op(A, lambda: A.activation(out=g12[:tn, 0:1], in_=dd[:tn, :], func=AF.Sigmoid))
                S.op(A, lambda: A.activation(out=g12[:tn, 1:2], in_=dd[:tn, :], func=AF.Sigmoid, scale=-1.0))
                S.op(SY, lambda: SY.dma_start(out=G2D[t0:t0 + tn, :], in_=g12[:tn, :]), True)
                S.op(V, lambda: V.tensor_tensor(out=mf[:tn, :], in0=k1[:tn, :], in1=k2[:tn, :], op=ALU.add))
                S.op(V, lambda: V.tensor_copy(out=mb[:tn, :], in_=mf[:tn, :]))
                S.op(PE, lambda: PE.matmul(pf[:tn, 0, 0:NE], lhsT=tri_b[:tn, :tn], rhs=mb[:tn, :], start=True, stop=True))
                S.op(V, lambda: V.tensor_tensor(out=pos[:tn, :], in0=pf[:tn, 0, 0:NE], in1=cnt[:tn, :], op=ALU.add))
                S.op(PE, lambda: PE.matmul(pf[:, 1, 0:NE], lhsT=ones_b[:tn, :], rhs=mb[:tn, :], start=True, stop=True))
                S.op(V, lambda: V.tensor_tensor(out=cnt[:], in0=cnt[:], in1=pf[:, 1, 0:NE], op=ALU.add))
                S.op(V, lambda: V.tensor_scalar(out=ovf[:tn, :], in0=pos[:tn, :], scalar1=float(TE), scalar2=float(BIGIDX), op0=ALU.is_ge, op1=ALU.mult))
                S.op(V, lambda: V.tensor_tensor(out=pos[:tn, :], in0=pos[:tn, :], in1=eoff[:tn, :], op=ALU.add))
                S.op(V, lambda: V.tensor_tensor(out=pos[:tn, :], in0=pos[:tn, :], in1=ovf[:tn, :], op=ALU.add))
                S.op(V, lambda: V.tensor_tensor(out=k1[:tn, :], in0=k1[:tn, :], in1=pos[:tn, :], op=ALU.mult))
                S.op(V, lambda: V.reduce_sum(out=idf[:tn, 0:1], in_=k1[:tn, :], axis=AX.X))
                S.op(V, lambda: V.tensor_tensor(out=k2[:tn, :], in0=k2[:tn, :], in1=pos[:tn, :], op=ALU.mult))
                S.op(V, lambda: V.reduce_sum(out=idf[:tn, 1:2], in_=k2[:tn, :], axis=AX.X))
                S.op(V, lambda: V.tensor_copy(out=idi[:tn, :], in_=idf[:tn, :]))
                S.op(SY, lambda: SY.dma_start(out=IDXD[t0:t0 + tn, :], in_=idi[:tn, :]), True)
                for j in range(2):
                    S.op(P, lambda j=j: P.indirect_dma_start(out=HETM[:, :], out_offset=bass.IndirectOffsetOnAxis(ap=idi[:tn, j:j + 1], axis=0),
                                                             in_=hb[:tn, :], in_offset=None, bounds_check=bcreg, oob_is_err=False), True)

    def stage_he_transpose():
        with SBT("het", [128, D], BF16) as ht_:
            for e in range(NE):
                for (t0, tn) in tiles(TE):
                    S.op(SY, lambda: SY.dma_start(out=ht_[:tn, :], in_=HETM[e * TE + t0:e * TE + t0 + tn, :]), True)
                    transpose_store(ht_, tn, HE[e], t0, DC)

    def stage_combine():
        with ExitStack() as st:
            def t(name, shape, dt=F32):
                return st.enter_context(SBT(name, list(shape), dt))
            r1 = t("cb_r1", [128, D], BF16); r2 = t("cb_r2", [128, D], BF16); y = t("cb_y", [128, D]); g = t("cb_g", [128, 2]); idi = t("cb_i", [128, 2], mybir.dt.int32)
            for (t0, tn) in tiles(TP) + [(TP, TS)]:
                S.op(SY, lambda: SY.dma_start(out=g[:tn, :], in_=G2D[t0:t0 + tn, :]), True)
                S.op(SY, lambda: SY.dma_start(out=idi[:tn, :], in_=IDXD[t0:t0 + tn, :]), True)
                S.op(V, lambda: V.memset(r1[:], 0.0))
                S.op(V, lambda: V.memset(r2[:], 0.0))
                for j, r in ((0, r1), (1, r2)):
                    S.op(P, lambda: P.indirect_dma_start(out=r[:tn, :], out_offset=None, in_=YE[:, :],
                                                         in_offset=bass.IndirectOffsetOnAxis(ap=idi[:tn, j:j + 1], axis=0),
                                                         bounds_check=bcreg, oob_is_err=False), True)
                S.op(V, lambda: V.tensor_scalar(out=y[:tn, :], in0=r1[:tn, :], scalar1=g[:tn, 0:1], scalar2=None, op0=ALU.mult))
                S.op(V, lambda: V.scalar_tensor_tensor(out=y[:tn, :], in0=r2[:tn, :], scalar=g[:tn, 1:2], in1=y[:tn, :], op0=ALU.mult, op1=ALU.add))
                S.op(SY, lambda: SY.dma_start(out=FO[t0:t0 + tn, :], in_=y[:tn, :]), True)

    def stage_epilogue(l, which, final=False):
        ga_c = 2 if which == 0 else 5
        with ExitStack() as st:
            def t(name, shape, dt=F32):
                return st.enter_context(SBT(name, list(shape), dt))
            xt = t("e_x", [128, D]); ft = t("e_f", [128, D]); ga = t("e_ga", [128, D]); gb = t("e_g", [128, D]); jk = t("e_j", [128, D]); rs = t("e_rs", [128, 1])
            S.op(SY, lambda: SY.dma_start(out=gb[:], in_=row_bcast(norm_post[l, which:which + 1, :], 128)), True)
            for (t0, tn) in tiles(TP) + [(TP, TS)]:
                S.op(SY, lambda: SY.dma_start(out=xt[:tn, :], in_=X[t0:t0 + tn, :]), True)
                S.op(SY, lambda: SY.dma_start(out=ft[:tn, :], in_=FO[t0:t0 + tn, :]), True)
                S.op(SY, lambda: SY.dma_start(out=ga[:tn, :], in_=mod_rows(l, ga_c, t0, tn)), True)
                rstd_of(ft[:tn, :], tn, rs, jk)
                S.op(V, lambda: V.tensor_tensor(out=ga[:tn, :], in0=ga[:tn, :], in1=gb[:tn, :], op=ALU.mult))
                S.op(V, lambda: V.scalar_tensor_tensor(out=ft[:tn, :], in0=ft[:tn, :], scalar=rs[:tn, 0:1], in1=ga[:tn, :], op0=ALU.mult, op1=ALU.mult))
                S.op(V, lambda: V.tensor_tensor(out=xt[:tn, :], in0=xt[:tn, :], in1=ft[:tn, :], op=ALU.add))
                S.op(SY, lambda: SY.dma_start(out=X[t0:t0 + tn, :], in_=xt[:tn, :]), True)
                if final:
                    dst = y_prompt.rearrange("b t d -> (b t) d")[t0:t0 + tn, :] if t0 < TP else y_sample.rearrange("b t d -> (b t) d")[t0 - TP:t0 - TP + tn, :]
                    S.op(SY, lambda: SY.dma_start(out=dst, in_=xt[:tn, :]), True)

    def stage_win(l):
        gemm_a([w_in[l, :, 0:5120]], HT, DC, 5120, T, "copy", PA)
        gemm_b(w_in[l, :, 5120:7168], HT, DC, 2048, tiles(TP) + [(TP, TS)], PB)

    def load_percol(dst, src_row, nch):
        with nc.allow_non_contiguous_dma(reason="small per-channel vector"):
            S.op(SY, lambda: SY.dma_start(out=dst, in_=src_row.rearrange("o (c p) -> p (o c)", p=128)), True)

    def stage_conv(l):
        NCH = CONV_DIM // 128
        segs = [(b * SEQ, SEQ, b) for b in range(NSEQ)] + [(TP, TS, None)]
        LM = max(SEQ, TS)
        with ExitStack() as st:
            def t(name, shape, dt=F32):
                return st.enter_context(SBT(name, list(shape), dt))
            cw = t("c_w", [128, 3, NCH]); cn = t("c_n", [128, NCH])
            hc = t("c_hc", [128, LM]); bg = t("c_bg", [128, LM]); ext = t("c_ext", [128, LM + 2]); cv = t("c_cv", [128, LM])
            s0 = t("c_s0", [128, 128]); s1 = t("c_s1", [128, 128]); tm = t("c_tm", [128, 128]); sq = t("c_sq", [128, 512]); ob = t("c_ob", [128, 512], BF16)
            for j in range(3):
                load_percol(cw[:, j, :], conv_w[l, j:j + 1, :], NCH)
            load_percol(cn[:, :], conv_norm[l:l + 1, :], NCH)
            S.op(SY, lambda: SY.dma_start(out=new_conv_sample[l, :, 0, :], in_=state_conv[l, :, 1, :]), True)
            for (t0, L, b) in segs:
                for ch in range(NCH):
                    S.op(SY, lambda: SY.dma_start(out=hc[:, :L], in_=PA[ch, :, t0:t0 + L]), True)
                    S.op(SY, lambda: SY.dma_start(out=bg[:, :L], in_=PA[NCH + ch, :, t0:t0 + L]), True)
                    S.op(SY, lambda: SY.dma_start(out=ext[:, 2:L + 2], in_=PA[2 * NCH + ch, :, t0:t0 + L]), True)
                    S.op(V, lambda: V.tensor_tensor(out=ext[:, 2:L + 2], in0=ext[:, 2:L + 2], in1=hc[:, :L], op=ALU.mult))
                    if b is not None:
                        S.op(V, lambda: V.memset(ext[:, 0:2], 0.0))
                        a0, a1 = ext[:, 0:L], ext[:, 1:L + 1]
                    else:
                        for j, dstt in ((0, s0), (1, s1)):
                            S.op(SY, lambda: SY.dma_start(out=tm[:L, :], in_=state_conv[l, :, j, ch * 128:(ch + 1) * 128]), True)
                            S.op(PE, lambda: PE.transpose(out=pf[:, 0, :L], in_=tm[:L, :], identity=ident_f[:L, :L]))
                            S.op(V, lambda: V.tensor_copy(out=dstt[:, :L], in_=pf[:, 0, :L]))
                        a0, a1 = s0[:, :L], s1[:, :L]
                    S.op(V, lambda: V.tensor_scalar(out=cv[:, :L], in0=a0, scalar1=cw[:, 0, ch:ch + 1], scalar2=None, op0=ALU.mult))
                    S.op(V, lambda: V.scalar_tensor_tensor(out=cv[:, :L], in0=a1, scalar=cw[:, 1, ch:ch + 1], in1=cv[:, :L], op0=ALU.mult, op1=ALU.add))
                    S.op(V, lambda: V.scalar_tensor_tensor(out=cv[:, :L], in0=ext[:, 2:L + 2], scalar=cw[:, 2, ch:ch + 1], in1=cv[:, :L], op0=ALU.mult, op1=ALU.add))
                    S.op(V, lambda: V.tensor_tensor(out=cv[:, :L], in0=cv[:, :L], in1=bg[:, :L], op=ALU.mult))
                    for (q0, qn) in tiles(L, 512):
                        S.op(V, lambda: V.tensor_tensor(out=sq[:, :qn], in0=cv[:, q0:q0 + qn], in1=cv[:, q0:q0 + qn], op=ALU.mult))
                        S.op(PE, lambda: PE.matmul(pf[:, 0, :qn], lhsT=bd64[:], rhs=sq[:, :qn], start=True, stop=True))
                        S.op(A, lambda: A.activation(out=sq[:, :qn], in_=pf[:, 0, :qn], func=AF.Sqrt, bias=epsb[:, 0:1]))
                        S.op(V, lambda: V.reciprocal(out=sq[:, :qn], in_=sq[:, :qn]))
                        S.op(V, lambda: V.scalar_tensor_tensor(out=ob[:, :qn], in0=cv[:, q0:q0 + qn], scalar=cn[:, ch:ch + 1], in1=sq[:, :qn], op0=ALU.mult, op1=ALU.mult))
                        S.op(SY, lambda: SY.dma_start(out=MIXT[ch, :, t0 + q0:t0 + q0 + qn], in_=ob[:, :qn]), True)
                    if b is not None:
                        with nc.allow_non_contiguous_dma(reason="conv state tail (small)"):
                            S.op(SY, lambda: SY.dma_start(out=new_conv_prompt[l, b, :, ch * 128:(ch + 1) * 128].rearrange("j p -> p j"), in_=ext[:, L:L + 2]), True)
                    else:
                        S.op(PE, lambda: PE.transpose(out=pf[:L, 1, 0:128], in_=ext[:, 2:L + 2], identity=ident_f[:]))
                        S.op(V, lambda: V.tensor_copy(out=tm[:L, :], in_=pf[:L, 1, 0:128]))
                        S.op(SY, lambda: SY.dma_start(out=new_conv_sample[l, :, 1, ch * 128:(ch + 1) * 128], in_=tm[:L, :]), True)

    def load_lb(l, lbt, oml):
        if l == 0:
            S.op(V, lambda: V.memset(lbt[:], 0.0))
        else:
            with SBT("lb_a", [128, HG], F32) as a0, SBT("lb_b", [128, HG], F32) as a1:
                load_percol(a0[:, :], lb_logits[0:1, :], HG)
                load_percol(a1[:, :], lb_logits[1:2, :], HG)
                S.op(V, lambda: V.tensor_tensor(out=a1[:], in0=a1[:], in1=a0[:], op=ALU.subtract))
                S.op(A, lambda: A.activation(out=lbt[:], in_=a1[:], func=AF.Sigmoid))
        S.op(V, lambda: V.tensor_scalar(out=oml[:], in0=lbt[:], scalar1=-1.0, scalar2=1.0, op0=ALU.mult, op1=ALU.add))

    def stage_hgrn_prompt(l):
        L = LCH
        NCK = SEQ // L
        with ExitStack() as st:
            def t(name, shape, dt=F32):
                return st.enter_context(SBT(name, list(shape), dt))
            lbt = t("h_lb", [128, HG]); oml = t("h_oml", [128, HG])
            q = t("h_q", [128, SEQ]); z = t("h_z", [128, SEQ]); Aa = t("h_A", [128, SEQ]); kk = t("h_k", [128, SEQ]); ea = t("h_ea", [128, SEQ])
            qa = t("h_qa", [128, SEQ], BF16); kd = t("h_kd", [128, SEQ], BF16); ke = t("h_ke", [128, SEQ], BF16)
            al = t("h_al", [128, NCK]); el = t("h_el", [128, NCK]); ket = t("h_ket", [L, NCK, 128], BF16)
            vf = t("h_vf", [L, NCK, 128]); vb = t("h_vb", [L, NCK, 128], BF16)
            Sf = t("h_S", [128, 128]); Sb = t("h_Sb", [128, 128], BF16)
            GC = 4
            ptt4 = t("h_pt4", [L, GC, L], BF16); ot4 = t("h_o4", [L, GC, 128]); mask4 = t("h_m4", [L, GC, L])
            for j in range(GC):
                S.op(V, lambda j=j: V.tensor_copy(out=mask4[:, j, :], in_=mask64[0:L, 0:L]))
            load_lb(l, lbt, oml)
            for b in range(NSEQ):
                t0 = b * SEQ
                for h in range(HG):
                    S.op(SY, lambda: SY.dma_start(out=q[:], in_=PA[24 + h, :, t0:t0 + SEQ]), True)
                    S.op(SY, lambda: SY.dma_start(out=z[:], in_=PA[32 + h, :, t0:t0 + SEQ]), True)
                    S.op(SY, lambda: SY.dma_start(out=vf[:], in_=PB[t0:t0 + SEQ, h * 128:(h + 1) * 128].rearrange("(n p) v -> p n v", p=L)), True)
                    S.op(V, lambda: V.tensor_copy(out=vb[:], in_=vf[:]))
                    S.op(A, lambda: A.activation(out=z[:], in_=z[:], func=AF.Sigmoid))
                    S.op(V, lambda: V.tensor_scalar(out=z[:], in0=z[:], scalar1=oml[:, h:h + 1], scalar2=lbt[:, h:h + 1], op0=ALU.mult, op1=ALU.add))
                    S.op(V, lambda: V.tensor_scalar(out=kk[:], in0=z[:], scalar1=-1.0, scalar2=1.0, op0=ALU.mult, op1=ALU.add))
                    S.op(V, lambda: V.tensor_scalar_max(out=z[:], in0=z[:], scalar1=F_MIN))
                    S.op(A, lambda: A.activation(out=z[:], in_=z[:], func=AF.Ln))
                    S.op(V, lambda: V.tensor_tensor_scan(out=Aa[:], data0=scanm[:], data1=z[:], initial=0.0, op0=ALU.mult, op1=ALU.add))
                    S.op(V, lambda: V.tensor_copy(out=al[:], in_=Aa[:].rearrange("p (n l) -> p n l", l=L)[:, :, L - 1]))
                    S.op(A, lambda: A.activation(out=el[:], in_=al[:], func=AF.Exp))
                    S.op(A, lambda: A.activation(out=ea[:], in_=Aa[:], func=AF.Exp))
                    S.op(V, lambda: V.tensor_tensor(out=qa[:], in0=q[:], in1=ea[:], op=ALU.mult))
                    S.op(V, lambda: V.tensor_tensor(out=ea[:].rearrange("p (n l) -> p n l", l=L), in0=Aa[:].rearrange("p (n l) -> p n l", l=L),
                                                    in1=al[:].unsqueeze(2).to_broadcast([128, NCK, L]), op=ALU.subtract))
                    S.op(A, lambda: A.activation(out=ea[:], in_=ea[:], func=AF.Exp, scale=-1.0))
                    S.op(V, lambda: V.tensor_tensor(out=ke[:], in0=kk[:], in1=ea[:], op=ALU.mult))
                    S.op(V, lambda: V.tensor_scalar(out=ea[:], in0=Aa[:], scalar1=-1.0, scalar2=80.0, op0=ALU.mult, op1=ALU.min))
                    S.op(A, lambda: A.activation(out=ea[:], in_=ea[:], func=AF.Exp))
                    S.op(V, lambda: V.tensor_tensor(out=kd[:], in0=kk[:], in1=ea[:], op=ALU.mult))
                    for c0 in range(0, NCK, 16):
                        nck = min(16, NCK - c0)
                        S.grp(PE, [(lambda j=j: PE.transpose(out=pt[0:L, j * 128:(j + 1) * 128], in_=ke[:, (c0 + j) * L:(c0 + j + 1) * L], identity=ident_b[:]))
                                   for j in range(nck)])
                        S.op(V, lambda: V.tensor_copy(out=ket[:, c0:c0 + nck, :], in_=pt[0:L, 0:nck * 128].rearrange("p (n d) -> p n d", d=128)))
                    S.op(V, lambda: V.memset(Sf[:], 0.0))
                    S.op(V, lambda: V.memset(Sb[:], 0.0))
                    for c0 in range(0, NCK, GC):
                        fns = []
                        for j in range(GC):
                            cs = slice((c0 + j) * L, (c0 + j + 1) * L)
                            fns.append(lambda j=j, cs=cs: PE.matmul(ps[0:L, 0, j * 128:j * 128 + L], lhsT=kd[:, cs], rhs=qa[:, cs], start=True, stop=True))
                        for j in range(GC):
                            fns.append(lambda j=j, ck=c0 + j: PE.matmul(ps[:, 1, j * 128:(j + 1) * 128], lhsT=ket[:, ck, :], rhs=vb[:, ck, :], start=True, stop=True))
                        S.grp(PE, fns)
                        S.op(V, lambda: V.tensor_tensor(out=ptt4[:], in0=ps[0:L, 0, 0:GC * 128].rearrange("p (j l) -> p j l", l=128)[:, :, 0:L], in1=mask4[:], op=ALU.mult))
                        for j in range(GC):
                            ck = c0 + j
                            cs = slice(ck * L, (ck + 1) * L)
                            S.grp(PE, [lambda j=j, ck=ck: PE.matmul(pf[0:L, 1, 0:128], lhsT=ptt4[:, j, :], rhs=vb[:, ck, :], start=True, stop=False),
                                       lambda cs=cs: PE.matmul(pf[0:L, 1, 0:128], lhsT=qa[:, cs], rhs=Sb[:], start=False, stop=True)])
                            S.op(V, lambda j=j: V.tensor_copy(out=ot4[:, j, :], in_=pf[0:L, 1, 0:128]))
                            S.op(V, lambda j=j, ck=ck: V.scalar_tensor_tensor(out=Sf[:], in0=Sf[:], scalar=el[:, ck:ck + 1], in1=ps[:, 1, j * 128:(j + 1) * 128],
                                                                              op0=ALU.mult, op1=ALU.add))
                            S.op(V, lambda: V.tensor_copy(out=Sb[:], in_=Sf[:]))
                        S.op(SY, lambda c0=c0: SY.dma_start(out=PO[t0 + c0 * L:t0 + (c0 + GC) * L, h * 128:(h + 1) * 128].rearrange("(n p) v -> p n v", p=L), in_=ot4[:]), True)
                    S.op(SY, lambda: SY.dma_start(out=new_hgrn_prompt[l, b, h, :, :], in_=Sf[:]), True)

    def stage_hgrn_sample(l):
        with ExitStack() as st:
            def t(name, shape, dt=F32):
                return st.enter_context(SBT(name, list(shape), dt))
            lbt = t("s_lb", [128, HG]); oml = t("s_oml", [128, HG])
            q = t("s_q", [128, HG, TS]); f = t("s_f", [128, HG, TS]); k = t("s_k", [128, HG, TS])
            so = t("s_so", [128, HG, 128]); sn = t("s_sn", [128, HG, 128]); vB = t("s_vB", [128, HG, 128]); orow = t("s_or", [1, 1024])
            load_lb(l, lbt, oml)
            for h in range(HG):
                S.op(SY, lambda h=h: SY.dma_start(out=q[:, h, :], in_=PA[24 + h, :, TP:TP + TS]), True)
                S.op(SY, lambda h=h: SY.dma_start(out=f[:, h, :], in_=PA[32 + h, :, TP:TP + TS]), True)
                S.op(A, lambda h=h: A.activation(out=f[:, h, :], in_=f[:, h, :], func=AF.Sigmoid))
                S.op(V, lambda h=h: V.tensor_scalar(out=f[:, h, :], in0=f[:, h, :], scalar1=oml[:, h:h + 1], scalar2=lbt[:, h:h + 1], op0=ALU.mult, op1=ALU.add))
                S.op(V, lambda h=h: V.tensor_scalar(out=k[:, h, :], in0=f[:, h, :], scalar1=-1.0, scalar2=1.0, op0=ALU.mult, op1=ALU.add))
                S.op(V, lambda h=h: V.tensor_scalar_max(out=f[:, h, :], in0=f[:, h, :], scalar1=F_MIN))
            for b in range(TS):
                S.op(SY, lambda: SY.dma_start(out=so[:], in_=state_hgrn[l, b].rearrange("h d v -> d h v")), True)
                S.op(SY, lambda: SY.dma_start(out=vB[:].rearrange("p h v -> p (h v)"), in_=row_bcast(PB[TP + b:TP + b + 1, 0:1024], 128)), True)
                kb = k[:, :, b].unsqueeze(2).to_broadcast([128, HG, 128])
                fb = f[:, :, b].unsqueeze(2).to_broadcast([128, HG, 128])
                S.op(V, lambda: V.tensor_tensor(out=vB[:], in0=vB[:], in1=kb, op=ALU.mult))
                S.op(V, lambda: V.tensor_tensor(out=so[:], in0=so[:], in1=fb, op=ALU.mult))
                S.op(V, lambda: V.tensor_tensor(out=sn[:], in0=so[:], in1=vB[:], op=ALU.add))
                S.grp(PE, [(lambda h=h: PE.matmul(pf[0:1, h // 4, (h % 4) * 128:(h % 4 + 1) * 128], lhsT=q[:, h, b:b + 1], rhs=sn[:, h, :], start=True, stop=True)) for h in range(HG)])
                S.op(V, lambda: V.tensor_copy(out=orow[:].rearrange("p (a c) -> p a c", a=2), in_=pf[0:1, :, :]))
                S.op(SY, lambda: SY.dma_start(out=PO[TP + b:TP + b + 1, :], in_=orow[:]), True)
                S.op(SY, lambda: SY.dma_start(out=new_hgrn_sample[l, b].rearrange("h d v -> d h v"), in_=sn[:]), True)

    def stage_hgrn_post(l):
        with ExitStack() as st:
            def t(name, shape, dt=F32):
                return st.enter_context(SBT(name, list(shape), dt))
            o = t("p_o", [128, 1024]); og = t("p_og", [128, 1024]); sq = t("p_sq", [128, 1024]); hn = t("p_hn", [128, 1024])
            ms = t("p_ms", [128, HG]); ob = t("p_ob", [128, 1024], BF16)
            S.op(SY, lambda: SY.dma_start(out=hn[:], in_=row_bcast(hgrn_norm[l:l + 1, :], 128)), True)
            for (t0, tn) in tiles(TP) + [(TP, TS)]:
                S.op(SY, lambda: SY.dma_start(out=o[:tn, :], in_=PO[t0:t0 + tn, :]), True)
                S.op(SY, lambda: SY.dma_start(out=og[:tn, :], in_=PB[t0:t0 + tn, 1024:2048]), True)
                S.op(V, lambda: V.tensor_tensor(out=sq[:tn, :], in0=o[:tn, :], in1=o[:tn, :], op=ALU.mult))
                S.op(V, lambda: V.reduce_sum(out=ms[:tn, :], in_=sq[:tn, :].rearrange("p (h v) -> p h v", v=128), axis=AX.X))
                S.op(V, lambda: V.tensor_scalar(out=ms[:tn, :], in0=ms[:tn, :], scalar1=1.0 / 128, scalar2=EPS, op0=ALU.mult, op1=ALU.add))
                S.op(A, lambda: A.sqrt(ms[:tn, :], ms[:tn, :]))
                S.op(V, lambda: V.reciprocal(out=ms[:tn, :], in_=ms[:tn, :]))
                S.op(V, lambda: V.tensor_tensor(out=o[:tn, :].rearrange("p (h v) -> p h v", v=128), in0=o[:tn, :].rearrange("p (h v) -> p h v", v=128),
                                                in1=ms[:tn, :].unsqueeze(2).to_broadcast([tn, HG, 128]), op=ALU.mult))
                S.op(V, lambda: V.tensor_tensor(out=o[:tn, :], in0=o[:tn, :], in1=hn[:tn, :], op=ALU.mult))
                S.op(A, lambda: A.activation(out=og[:tn, :], in_=og[:tn, :], func=AF.Silu))
                S.op(V, lambda: V.tensor_tensor(out=ob[:tn, :], in0=o[:tn, :], in1=og[:tn, :], op=ALU.mult))
                transpose_store(ob, tn, MIXT, t0, 8, c0=8)

    def stage_ffn(W1, W3, W2, dff, INT=None, Tn=None, tls=None, DSTB=None, odt=F32):
        KC = dff // 128
        INT = HT if INT is None else INT
        Tn = T if Tn is None else Tn
        tls = (tiles(TP) + [(TP, TS)]) if tls is None else tls
        gemm_a([W1, W3], INT, DC, dff, Tn, "swiglu", AT)
        gemm_b(W2, AT, KC, D, tls, FO if DSTB is None else DSTB, odt)

    stage_mod()
    for l in range(DEPTH):
        stage_normmod(l, 0)
        stage_win(l)
        stage_conv(l)
        stage_hgrn_prompt(l)
        stage_hgrn_sample(l)
        stage_hgrn_post(l)
        gemm_b(w_out[l], MIXT, DC, D, tiles(TP) + [(TP, TS)], FO)
        stage_epilogue(l, 0)
        if l % 2 == 0:
            stage_normmod(l, 1)
            stage_ffn(ffn_w1[l // 2], ffn_w3[l // 2], ffn_w2[l // 2], c.dff)
        else:
            stage_normmod(l, 1, router=True)
            stage_he_transpose()
            for e in range(NE):
                stage_ffn(moe_w1[l // 2, e], moe_w3[l // 2, e], moe_w2[l // 2, e], c.dffe, INT=HE[e], Tn=TE, tls=tiles(TE), DSTB=YE[e * TE:(e + 1) * TE, :], odt=BF16)
            stage_combine()
        stage_epilogue(l, 1, final=(l == DEPTH - 1))
    S.fin(SY)
    es.close()
    return nc


_NAMES = ["x_prompt", "x_sample", "state_conv", "state_hgrn", "c_prompt", "c_sample", "norm_pre", "norm_post", "w_mod", "b_mod",
          "w_in", "conv_w", "conv_norm", "lb_logits", "hgrn_norm", "w_out", "ffn_w1", "ffn_w3", "ffn_w2", "router_w", "router_b",
          "moe_w1", "moe_w3", "moe_w2"]
_OUTS = ["y_prompt", "y_sample", "new_conv_prompt", "new_hgrn_prompt", "new_conv_sample", "new_hgrn_sample"]


def kernel(**inputs):
    nseq, seq = inputs["x_prompt"].shape[0], inputs["x_prompt"].shape[1]
    cfg = Cfg(nseq=nseq, seq=seq, ts=inputs["x_sample"].shape[0], dff=inputs["ffn_w1"].shape[2], dffe=inputs["moe_w1"].shape[3])
    nc = build(cfg)
    in_map = {k: np.ascontiguousarray(np.asarray(inputs[k], dtype=np.float32)) for k in _NAMES}
    res = run_bass_kernel_spmd(nc, [in_map], core_ids=[0])
    r = res.results[0]
    return tuple(np.asarray(r[k], dtype=np.float32) for k in _OUTS)
```
